# Optimizing a Trainium2 kernel written in Bass

```python
import math
import jax, jax.numpy as jnp
from jax import lax
import numpy as np

D_MODEL = 2048
BATCH = 4
SEQ = 2048
DEPTH = 2
DEC_BATCH = 128
DEC_SEQ = 4
PAST_LEN = 16384
PAGE_SIZE = 128

N_MIXERS = 2
N_GDN_LAYERS = (DEPTH + 1) // 2
N_SCONV_LAYERS = DEPTH // 2
GDN_HEAD_K = 128
GDN_HEAD_V = 128
GDN_K_HEADS = D_MODEL // GDN_HEAD_K
GDN_V_HEADS = 2 * GDN_K_HEADS
GDN_K_DIM = GDN_K_HEADS * GDN_HEAD_K
GDN_V_DIM = GDN_V_HEADS * GDN_HEAD_V
GDN_CONV_DIM = 2 * GDN_K_DIM + GDN_V_DIM
GDN_IN_DIM = GDN_CONV_DIM + GDN_V_DIM + 2 * GDN_V_HEADS
GDN_CONV = 4
GDN_CHUNK = 64
SCONV_WIDTH = 3
FFN_DIM = 5632
N_EXPERTS = 8
TOP_K = 2
EXPERT_DIM = 7 * D_MODEL // 2
NORM_EPS = 1e-6

kernel_name = 'hybrid_gdn_shortconv_moe_step'


def _rmsnorm(x, gain):
    xf = x.astype(jnp.float32)
    y = xf * lax.rsqrt(jnp.mean(xf * xf, axis=-1, keepdims=True) + NORM_EPS)
    return (y * gain.astype(jnp.float32)).astype(x.dtype)


def _l2norm(x):
    xf = x.astype(jnp.float32)
    return xf * lax.rsqrt(jnp.sum(xf * xf, axis=-1, keepdims=True) + NORM_EPS)


def _modulation(c, w_ada, b_ada):
    m = jax.nn.silu(c) @ w_ada + b_ada
    return jnp.split(m[:, None, :], 6, axis=-1)


def _causal_dwconv(u, buf, w):
    width = w.shape[0]
    t = u.shape[1]
    full = jnp.concatenate([buf.astype(u.dtype), u], axis=1)
    y = full[:, 0:t] * w[0]
    for j in range(1, width):
        y = y + full[:, j:j + t] * w[j]
    return y, full[:, t:]


def _gated_delta_rule(q, k, v, beta, g, s0):
    b, t, h, dk = q.shape
    dv = v.shape[-1]
    c = min(GDN_CHUNK, t)
    n = -(-t // c)
    pad = n * c - t

    def blocks(a):
        a = jnp.pad(a.astype(jnp.float32), [(0, 0), (0, pad)] + [(0, 0)] * (a.ndim - 2))
        a = jnp.moveaxis(a, 1, 2)
        return a.reshape((b, h, n, c) + a.shape[3:])

    q = blocks(q) * (dk ** -0.5)
    k, v, beta, g = blocks(k), blocks(v), blocks(beta), blocks(g)
    g = jnp.cumsum(g, axis=-1)
    causal = jnp.tril(jnp.ones((c, c), dtype=bool))
    decay = jnp.exp(jnp.where(causal, g[..., :, None] - g[..., None, :], -jnp.inf))
    kb = k * beta[..., None]
    eye = jnp.eye(c, dtype=jnp.float32)
    lower = jnp.einsum('bhnid,bhnjd->bhnij', kb, k) * decay * (1.0 - eye)
    tinv = lax.linalg.triangular_solve(eye + lower, jnp.broadcast_to(eye, lower.shape),
                                       left_side=True, lower=True, unit_diagonal=True)
    u = jnp.einsum('bhnij,bhnjd->bhnid', tinv, v * beta[..., None])
    w = jnp.einsum('bhnij,bhnjd->bhnid', tinv, kb * jnp.exp(g)[..., None])
    a_intra = jnp.einsum('bhnid,bhnjd->bhnij', q, k) * decay
    q_dec = q * jnp.exp(g)[..., None]
    k_dec = k * jnp.exp(g[..., -1:] - g)[..., None]
    g_tot = jnp.exp(g[..., -1])

    def step(s, xs):
        q_i, k_i, u_i, w_i, a_i, gt_i = xs
        v_new = u_i - jnp.einsum('bhcd,bhde->bhce', w_i, s)
        o_i = jnp.einsum('bhcd,bhde->bhce', q_i, s) + jnp.einsum('bhij,bhje->bhie', a_i, v_new)
        s = s * gt_i[..., None, None] + jnp.einsum('bhcd,bhce->bhde', k_i, v_new)
        return s, o_i

    xs = tuple(jnp.moveaxis(a, 2, 0) for a in (q_dec, k_dec, u, w, a_intra, g_tot))
    s_fin, o = lax.scan(step, s0.astype(jnp.float32), xs)
    o = jnp.transpose(o, (1, 0, 3, 2, 4)).reshape(b, n * c, h, dv)[:, :t]
    return o, s_fin


def _gdn_mixer(h, s0, conv_buf, w_in, conv_w, a_log, dt_bias, g_onorm, w_out):
    b, t, _ = h.shape
    proj = h @ w_in
    qkv, z, bt, a = jnp.split(proj, [GDN_CONV_DIM, GDN_CONV_DIM + GDN_V_DIM,
                                     GDN_CONV_DIM + GDN_V_DIM + GDN_V_HEADS], axis=-1)
    qkv, new_buf = _causal_dwconv(qkv, conv_buf, conv_w)
    qkv = jax.nn.silu(qkv)
    q, k, v = jnp.split(qkv, [GDN_K_DIM, 2 * GDN_K_DIM], axis=-1)
    rep = GDN_V_HEADS // GDN_K_HEADS
    q = jnp.repeat(_l2norm(q.reshape(b, t, GDN_K_HEADS, GDN_HEAD_K)), rep, axis=2)
    k = jnp.repeat(_l2norm(k.reshape(b, t, GDN_K_HEADS, GDN_HEAD_K)), rep, axis=2)
    v = v.reshape(b, t, GDN_V_HEADS, GDN_HEAD_V)
    beta = jax.nn.sigmoid(bt.astype(jnp.float32))
    g = -jnp.exp(a_log.astype(jnp.float32)) * jax.nn.softplus(a.astype(jnp.float32) + dt_bias.astype(jnp.float32))
    o, s_new = _gated_delta_rule(q, k, v, beta, g, s0)
    zf = z.reshape(b, t, GDN_V_HEADS, GDN_HEAD_V).astype(jnp.float32)
    o = _rmsnorm(o, g_onorm) * jax.nn.silu(zf)
    y = o.reshape(b, t, GDN_V_DIM).astype(h.dtype) @ w_out
    return y, s_new.astype(s0.dtype), new_buf.astype(conv_buf.dtype)


def _shortconv_mixer(h, buf, w_in, conv_w, w_out):
    bg, cg, xin = jnp.split(h @ w_in, 3, axis=-1)
    y, new_buf = _causal_dwconv(cg * xin, buf, conv_w)
    return (bg * y) @ w_out, new_buf.astype(buf.dtype)


def _swiglu(h, w_up, w_down):
    gate, up = jnp.split(h @ w_up, 2, axis=-1)
    return (jax.nn.silu(gate) * up) @ w_down


def _moe(h, w_router, b_router, w_up, w_down):
    logits = (h @ w_router).astype(jnp.float32) + b_router.astype(jnp.float32)
    top_v, top_i = lax.top_k(logits, TOP_K)
    gates = jax.nn.softmax(top_v, axis=-1)
    dense_gate = jnp.einsum('btk,btke->bte', gates, jax.nn.one_hot(top_i, N_EXPERTS, dtype=jnp.float32))
    out = jnp.zeros(h.shape, jnp.float32)
    for e in range(N_EXPERTS):
        out = out + dense_gate[..., e:e + 1] * _swiglu(h, w_up[e], w_down[e]).astype(jnp.float32)
    return out.astype(h.dtype)


def _trunk(x, c, s_gdn, s_gconv, s_sconv, w_ada, b_ada, g_norm_mix, g_norm_ffn, g_norm_out,
           gdn_w_in, gdn_conv_w, gdn_a_log, gdn_dt_bias, gdn_g_onorm, gdn_w_out,
           sc_w_in, sc_conv_w, sc_w_out, ffn_w_up, ffn_w_down,
           moe_w_router, moe_b_router, moe_w_up, moe_w_down):
    new_gdn, new_gconv, new_sconv = [], [], []
    for i in range(DEPTH):
        j = i // N_MIXERS
        sh1, sc1, ga1, sh2, sc2, ga2 = _modulation(c, w_ada[i], b_ada[i])
        h = _rmsnorm(x, g_norm_mix[i]) * (1.0 + sc1) + sh1
        if i % N_MIXERS == 0:
            y, s_new, cb_new = _gdn_mixer(h, s_gdn[j], s_gconv[j], gdn_w_in[j], gdn_conv_w[j],
                                          gdn_a_log[j], gdn_dt_bias[j], gdn_g_onorm[j], gdn_w_out[j])
            new_gdn.append(s_new)
            new_gconv.append(cb_new)
        else:
            y, cb_new = _shortconv_mixer(h, s_sconv[j], sc_w_in[j], sc_conv_w[j], sc_w_out[j])
            new_sconv.append(cb_new)
        x = x + ga1 * y
        h = _rmsnorm(x, g_norm_ffn[i]) * (1.0 + sc2) + sh2
        if i % 2 == 0:
            f = _swiglu(h, ffn_w_up[j], ffn_w_down[j])
        else:
            f = _moe(h, moe_w_router[j], moe_b_router[j], moe_w_up[j], moe_w_down[j])
        x = x + ga2 * f
    return _rmsnorm(x, g_norm_out), jnp.stack(new_gdn), jnp.stack(new_gconv), jnp.stack(new_sconv)


def setup_inputs(seed: int = 0) -> dict:
    key = jax.random.key(seed)
    ks = iter(jax.random.split(key, 40))
    f32 = jnp.float32

    def nrm(shape, scale):
        return jax.random.normal(next(ks), shape, f32) * scale

    d = D_MODEL
    x_prompt = nrm((BATCH, SEQ, d), 1.0)
    x_sample = nrm((DEC_BATCH, DEC_SEQ, d), 1.0)
    c_prompt = nrm((BATCH, d), 1.0)
    c_sample = nrm((DEC_BATCH, d), 1.0)
    state_gdn = nrm((N_GDN_LAYERS, DEC_BATCH, GDN_V_HEADS, GDN_HEAD_K, GDN_HEAD_V), 0.1)
    state_gdn_conv = nrm((N_GDN_LAYERS, DEC_BATCH, GDN_CONV - 1, GDN_CONV_DIM), 1.0)
    state_sconv = nrm((N_SCONV_LAYERS, DEC_BATCH, SCONV_WIDTH - 1, d), 1.0)
    w_ada = nrm((DEPTH, d, 6 * d), 0.5 * d ** -0.5)
    b_ada = nrm((DEPTH, 6 * d), 0.02)
    g_norm_mix = 1.0 + nrm((DEPTH, d), 0.02)
    g_norm_ffn = 1.0 + nrm((DEPTH, d), 0.02)
    g_norm_out = 1.0 + nrm((d,), 0.02)
    gdn_w_in = nrm((N_GDN_LAYERS, d, GDN_IN_DIM), d ** -0.5)
    gdn_conv_w = nrm((N_GDN_LAYERS, GDN_CONV, GDN_CONV_DIM), GDN_CONV ** -0.5)
    gdn_a_log = jnp.log(jax.random.uniform(next(ks), (N_GDN_LAYERS, GDN_V_HEADS), f32, 1.0, 16.0))
    dt = jnp.exp(jax.random.uniform(next(ks), (N_GDN_LAYERS, GDN_V_HEADS), f32,
                                    math.log(1e-3), math.log(1e-1)))
    gdn_dt_bias = dt + jnp.log(-jnp.expm1(-dt))
    gdn_g_onorm = 1.0 + nrm((N_GDN_LAYERS, GDN_HEAD_V), 0.02)
    gdn_w_out = nrm((N_GDN_LAYERS, GDN_V_DIM, d), GDN_V_DIM ** -0.5)
    sc_w_in = nrm((N_SCONV_LAYERS, d, 3 * d), d ** -0.5)
    sc_conv_w = nrm((N_SCONV_LAYERS, SCONV_WIDTH, d), SCONV_WIDTH ** -0.5)
    sc_w_out = nrm((N_SCONV_LAYERS, d, d), d ** -0.5)
    ffn_w_up = nrm((N_GDN_LAYERS, d, 2 * FFN_DIM), d ** -0.5)
    ffn_w_down = nrm((N_GDN_LAYERS, FFN_DIM, d), FFN_DIM ** -0.5)
    moe_w_router = nrm((N_SCONV_LAYERS, d, N_EXPERTS), d ** -0.5)
    moe_b_router = nrm((N_SCONV_LAYERS, N_EXPERTS), 0.01)
    moe_w_up = nrm((N_SCONV_LAYERS, N_EXPERTS, d, 2 * EXPERT_DIM), d ** -0.5)
    moe_w_down = nrm((N_SCONV_LAYERS, N_EXPERTS, EXPERT_DIM, d), EXPERT_DIM ** -0.5)
    return {'x_prompt': x_prompt, 'x_sample': x_sample, 'c_prompt': c_prompt, 'c_sample': c_sample,
            'state_gdn': state_gdn, 'state_gdn_conv': state_gdn_conv, 'state_sconv': state_sconv,
            'w_ada': w_ada, 'b_ada': b_ada, 'g_norm_mix': g_norm_mix, 'g_norm_ffn': g_norm_ffn,
            'g_norm_out': g_norm_out, 'gdn_w_in': gdn_w_in, 'gdn_conv_w': gdn_conv_w,
            'gdn_a_log': gdn_a_log, 'gdn_dt_bias': gdn_dt_bias, 'gdn_g_onorm': gdn_g_onorm,
            'gdn_w_out': gdn_w_out, 'sc_w_in': sc_w_in, 'sc_conv_w': sc_conv_w, 'sc_w_out': sc_w_out,
            'ffn_w_up': ffn_w_up, 'ffn_w_down': ffn_w_down, 'moe_w_router': moe_w_router,
            'moe_b_router': moe_b_router, 'moe_w_up': moe_w_up, 'moe_w_down': moe_w_down}


def reference(x_prompt, x_sample, c_prompt, c_sample, state_gdn, state_gdn_conv, state_sconv,
              w_ada, b_ada, g_norm_mix, g_norm_ffn, g_norm_out, gdn_w_in, gdn_conv_w,
              gdn_a_log, gdn_dt_bias, gdn_g_onorm, gdn_w_out, sc_w_in, sc_conv_w, sc_w_out,
              ffn_w_up, ffn_w_down, moe_w_router, moe_b_router, moe_w_up, moe_w_down):
    bp = x_prompt.shape[0]
    p_gdn = jnp.zeros((state_gdn.shape[0], bp) + state_gdn.shape[2:], state_gdn.dtype)
    p_gconv = jnp.zeros((state_gdn_conv.shape[0], bp) + state_gdn_conv.shape[2:], state_gdn_conv.dtype)
    p_sconv = jnp.zeros((state_sconv.shape[0], bp) + state_sconv.shape[2:], state_sconv.dtype)
    y_prompt, gdn_p, gconv_p, sconv_p = _trunk(
        x_prompt, c_prompt, p_gdn, p_gconv, p_sconv, w_ada, b_ada, g_norm_mix, g_norm_ffn, g_norm_out,
        gdn_w_in, gdn_conv_w, gdn_a_log, gdn_dt_bias, gdn_g_onorm, gdn_w_out,
        sc_w_in, sc_conv_w, sc_w_out, ffn_w_up, ffn_w_down,
        moe_w_router, moe_b_router, moe_w_up, moe_w_down)
    y_sample, gdn_s, gconv_s, sconv_s = _trunk(
        x_sample, c_sample, state_gdn, state_gdn_conv, state_sconv, w_ada, b_ada, g_norm_mix, g_norm_ffn,
        g_norm_out, gdn_w_in, gdn_conv_w, gdn_a_log, gdn_dt_bias, gdn_g_onorm, gdn_w_out,
        sc_w_in, sc_conv_w, sc_w_out, ffn_w_up, ffn_w_down,
        moe_w_router, moe_b_router, moe_w_up, moe_w_down)
    return (y_prompt, y_sample, gdn_p, gconv_p, sconv_p, gdn_s, gconv_s, sconv_s)
```

```python
import contextlib
import numpy as np
import concourse.bass as bass
import concourse.mybir as mybir
from concourse.bass_utils import run_bass_kernel_spmd

F32 = mybir.dt.float32
BF16 = mybir.dt.bfloat16
AF = mybir.ActivationFunctionType
ALU = mybir.AluOpType
AX = mybir.AxisListType

SEM_ROT = 12000
D = 2048
KC = 16
NPR = 1024
NSQ = 16
NSM = 64
NHALO = 4
NT = NPR + NSM + NHALO
NMOD = 18
GIN = 12352
FFN = 5632
EXD = 7168
NEXP = 8
EPS = 1e-6


class Buf:
    __slots__ = ("name", "w", "r", "war")

    def __init__(self, name):
        self.name = name
        self.w = None
        self.r = []
        self.war = set()


class Prog:
    ENGS = ("pe", "act", "dve", "pool", "sp")

    def __init__(self, nc):
        self.nc = nc
        self.ins = []
        self.nb = 0
        self.out_dmas = []
        self.last = {}
        self.fence_deps = {}

    def buf(self, name=None):
        self.nb += 1
        return Buf(f"{name or 'b'}{self.nb}")

    def fence(self):
        allast = set(self.last.values())
        for e in self.ENGS:
            self.fence_deps[e] = set(allast)

    def add(self, eng, fn, reads=(), writes=(), dma=False, join=False, out=False):
        i = len(self.ins)
        deps = set()
        for b in reads:
            if b.w:
                deps.update(b.w.values())
        for b in writes:
            if b.w and not join:
                deps.update(b.w.values())
            for r in b.r:
                deps.add(r)
            if join and b.w:
                deps.update(b.war)
        for b in reads:
            b.r.append(i)
        ek = (eng, dma)
        for b in writes:
            if join and b.w:
                b.w[ek] = i
                b.war = b.war | set(b.r)
            else:
                b.w = {ek: i}
                b.war = set(b.r)
            b.r = []
        if eng in self.fence_deps:
            deps |= self.fence_deps.pop(eng)
        deps.discard(i)
        dsem = None
        if dma:
            dsem = writes[0].name if writes else reads[0].name
        self.ins.append(dict(eng=eng, fn=fn, deps=deps, dma=dma, dsem=dsem))
        self.last[eng] = i
        if out:
            self.out_dmas.append(i)
        return i

    def emit(self, stack):
        nc = self.nc
        ins = self.ins
        n = len(ins)
        needed = [False] * n
        for it in ins:
            for d in it["deps"]:
                if ins[d]["eng"] == "pe" and it["eng"] == "pe" and not ins[d]["dma"] and not it["dma"]:
                    continue
                needed[d] = True
        for i, it in enumerate(ins):
            if it["dma"]:
                needed[i] = True
        cnt = [0]

        def newsem(tag):
            cnt[0] += 1
            return stack.enter_context(nc.semaphore(f"s{tag}{cnt[0]}"))

        eng_sem, eng_cnt, dma_sems = {}, {}, {}
        tok = [None] * n
        for i, it in enumerate(ins):
            if not needed[i]:
                continue
            if it["dma"]:
                key = it["dsem"]
                if key not in dma_sems:
                    dma_sems[key] = [newsem("d"), 0]
                ds = dma_sems[key]
                ds[1] += 16
                tok[i] = (ds[0], ds[1], id(ds[0]))
                if ds[1] >= SEM_ROT * 16:
                    del dma_sems[key]
            else:
                e = it["eng"]
                if e not in eng_sem or eng_cnt[e] >= SEM_ROT:
                    eng_sem[e] = newsem(e)
                    eng_cnt[e] = 0
                eng_cnt[e] += 1
                tok[i] = (eng_sem[e], eng_cnt[e], id(eng_sem[e]))
        self.nsem = cnt[0]
        per = {e: [] for e in self.ENGS}
        for i, it in enumerate(ins):
            per[it["eng"]].append(i)
        final_waits = [tok[i] for i in self.out_dmas]

        def run_engine(ename, eh):
            known = {}
            for i in per[ename]:
                it = ins[i]
                waits = {}
                for d in it["deps"]:
                    if tok[d] is None:
                        continue
                    if ename == "pe" and ins[d]["eng"] == "pe" and not ins[d]["dma"] and not it["dma"]:
                        continue
                    s, v, k = tok[d]
                    if k not in waits or waits[k][1] < v:
                        waits[k] = (s, v)
                for k, (s, v) in waits.items():
                    if known.get(k, 0) < v:
                        eh.wait_ge(s, v)
                        known[k] = v
                r = it["fn"](eh)
                if tok[i] is not None:
                    s, v, k = tok[i]
                    r.then_inc(s, 16 if it["dma"] else 1)
            if ename == "sp":
                fw = {}
                for s, v, k in final_waits:
                    if k not in fw or fw[k][1] < v:
                        fw[k] = (s, v)
                for k, (s, v) in fw.items():
                    eh.wait_ge(s, v)

        with nc.Block() as block:
            @block.sync
            def _(e):
                run_engine("sp", e)

            @block.tensor
            def _(e):
                run_engine("pe", e)

            @block.scalar
            def _(e):
                run_engine("act", e)

            @block.vector
            def _(e):
                run_engine("dve", e)

            @block.gpsimd
            def _(e):
                run_engine("pool", e)


class TT:
    def __init__(self, t, b):
        self.t = t
        self.b = b

    def __getitem__(self, k):
        return self.t[k]


class _Stop(Exception):
    pass


def build_program(dbg=None):
    nc = bass.Bass("TRN2", target_bir_lowering=False)
    P = Prog(nc)
    dbg = dbg or {}

    def dump(tt, name, shape, dt=F32):
        if name not in dbg.get('dumps', ()):
            return
        dd = nc.dram_tensor('dbg_' + name, list(shape), dt, kind='ExternalOutput').ap()
        P.add('sp', lambda e: e.dma_start(out=dd, in_=tt[:]), reads=[tt.b], dma=True, out=True)

    @contextlib.contextmanager
    def scope():
        with contextlib.ExitStack() as sc_:
            yield sc_
        P.fence()

    def stop_at(tag):
        if dbg.get('stop') == tag:
            raise _Stop()

    def din(name, shape, dt=F32):
        return nc.dram_tensor(name, list(shape), dt, kind="ExternalInput").ap()

    def dout(name, shape, dt=F32):
        return nc.dram_tensor(name, list(shape), dt, kind="ExternalOutput").ap()

    xo_d = din("xo", [NT, D])
    xp_d = din("xp", [NPR, D])
    cvec_d = din("cvec", [NMOD, D])
    sgdn_d = din("sgdn", [NSQ, 32, 128, 128])
    sgconv_d = din("sgconv", [NSQ * 3, 8192])
    ssconv_d = din("ssconv", [NSQ * 2, D])
    f1_d = din("f1", [128, 1])
    vecs_d = din("vecs", [640, 128])
    hvec_d = din("hvec", [1, 72])
    ident_d = din("ident", [128, 128])
    masks_d = din("masks", [64, 4, 64])
    sel_d = din("sel", [8, 8, 128])
    w_ada_d = din("w_ada", [2, D, 6 * D])
    w_gin_d = din("gdn_w_in", [D, GIN])
    w_gout_d = din("gdn_w_out", [4096, D])
    w_scin_d = din("sc_w_in", [D, 3 * D])
    w_scout_d = din("sc_w_out", [D, D])
    w_fup_d = din("ffn_w_up", [D, 2 * FFN])
    w_fdn_d = din("ffn_w_down", [FFN, D])
    w_rt_d = din("moe_w_router", [D, NEXP])
    w_mup_d = din("moe_w_up", [NEXP, D, 2 * EXD])
    w_mdn_d = din("moe_w_down", [NEXP, EXD, D])

    yo_d = dout("yo", [NT, D])
    gdnp_d = dout("gdn_p", [32, 128, 128])
    gconvp_d = dout("gconv_p", [3, 8192])
    sconvp_d = dout("sconv_p", [2, D])
    gdns_d = dout("gdn_s", [NSQ, 32, 128, 128])
    gconvs_d = dout("gconv_s", [NSQ * 3, 8192])
    sconvs_d = dout("sconv_s", [NSQ * 2, D])

    sscr_d = nc.dram_tensor("sscr", [32, 128, 128], F32).ap()
    oscr_d = nc.dram_tensor("oscr", [32, 128, NT], BF16).ap()
    bscr = [P.buf("sscr") for _ in range(32)]
    boscr = [P.buf("oscr") for _ in range(32)]

    top = contextlib.ExitStack()
    uid = [0]

    def sb(shape, dt=F32, stack=None, name=None):
        uid[0] += 1
        nm = f"{name or 't'}{uid[0]}"
        t = (stack or top).enter_context(nc.sbuf_tensor(nm, list(shape), dt))
        return TT(t, P.buf(nm))

    banks = []
    for i in range(8):
        t = top.enter_context(nc.psum_tensor(f"psb{i}", [128, 512], F32))
        banks.append(TT(t, P.buf(f"psb{i}")))
    brr = [0, 0]

    def bank():
        b = banks[brr[0] % 5]
        brr[0] += 1
        return b

    def obank():
        b = banks[5 + brr[1] % 2]
        brr[1] += 1
        return b
    rbank = banks[7]

    rr = {"ev": 0, "cast": 0}

    def ev_eng():
        rr["ev"] += 1
        return "act" if rr["ev"] % 2 else "dve"

    def copy_op(eng, out, in_):
        if eng == "act":
            return lambda e: e.activation(out=out, in_=in_, func=AF.Copy)
        return lambda e: e.tensor_copy(out=out, in_=in_)

    ident = sb([128, 128], name="ident")
    P.add("sp", lambda e: e.dma_start(out=ident[:], in_=ident_d), writes=[ident.b], dma=True)
    masks = sb([64, 4, 64], name="masks")
    P.add("sp", lambda e: e.dma_start(out=masks[:], in_=masks_d), writes=[masks.b], dma=True)
    sel = sb([8, 8, 128], name="sel")
    P.add("sp", lambda e: e.dma_start(out=sel[:], in_=sel_d), writes=[sel.b], dma=True)
    f1 = sb([128, 1], name="f1")
    P.add("sp", lambda e: e.dma_start(out=f1[:], in_=f1_d), writes=[f1.b], dma=True)
    hvec = sb([128, 72], name="hvec")
    P.add("sp", lambda e: e.dma_start(out=hvec[:], in_=hvec_d.partition_broadcast(128)), writes=[hvec.b], dma=True)
    ones_f = sb([128, 128], name="ones_f")
    P.add("pool", lambda e: e.memset(ones_f[:], 1.0), writes=[ones_f.b])
    ones_b = sb([128, 128], BF16, name="ones_b")
    P.add("pool", lambda e: e.memset(ones_b[:], 1.0), writes=[ones_b.b])
    epst = sb([128, 1], name="eps")
    P.add("pool", lambda e: e.memset(epst[:], EPS), writes=[epst.b])

    vecT = sb([128, 640], name="vecT")
    mod = sb([128, 2, 96, NMOD], name="mod")
    gmod = sb([128, 2, 2, 16, NMOD], name="gmod")
    cT = sb([128, KC, NMOD], BF16, name="cT")
    HT = sb([128, KC, NT], BF16, name="HT")
    nexpa = sb([128, 32], name="nexpa")
    ba_w = sb([128, KC, 64], BF16, name="ba_w")
    tails = sb([128, 4, 16, 3], name="tails")

    NSTG = 2
    stg = [sb([128, 2048], name="stg") for _ in range(NSTG)]
    wbp = [sb([128, 2048], BF16, name="wb") for _ in range(NSTG)]
    wrr = [0]

    def stage_load(src_ap, kc, cw):
        i = wrr[0] % NSTG
        wrr[0] += 1
        s = stg[i]
        sv = s[:, 0:kc * cw].rearrange("p (k c) -> p k c", k=kc)
        P.add("sp", lambda e: e.dma_start(out=sv, in_=src_ap.rearrange("(k p) c -> p k c", p=128)),
              writes=[s.b], dma=True)
        return i, s, sv

    def cast_eng():
        rr["cast"] += 1
        return ("act", "dve", "pool")[rr["cast"] % 3]

    def load_w(src_ap, kc, cw):
        i, s, sv = stage_load(src_ap, kc, cw)
        w = wbp[i]
        wv = w[:, 0:kc * cw].rearrange("p (k c) -> p k c", k=kc)
        ce = cast_eng()
        P.add(ce, copy_op(ce, w[:, 0:kc * cw], s[:, 0:kc * cw]), reads=[s.b], writes=[w.b])
        return w, wv

    fin = contextlib.ExitStack()
    try:
        with scope() as sc:
            vs = sb([128, 5, 128], stack=sc, name="vs")
            P.add("sp", lambda e: e.dma_start(out=vs[:], in_=vecs_d.rearrange("(t p) c -> p t c", p=128)),
                  writes=[vs.b], dma=True)
            for half in range(2):
                bk = bank()
                nt_ = 4 if half == 0 else 1

                def tr(e, half=half, bk=bk, nt_=nt_):
                    for j in range(nt_):
                        r = e.transpose(bk[:, j * 128:(j + 1) * 128], vs[:, half * 4 + j, :], ident[:])
                    return r
                P.add("pe", tr, reads=[vs.b, ident.b], writes=[bk.b])
                P.add("dve", copy_op("dve", vecT[:, half * 512:half * 512 + nt_ * 128], bk[:, 0:nt_ * 128]),
                      reads=[bk.b], writes=[vecT.b], join=(half > 0))
            cs = sb([NMOD, D], stack=sc, name="cs")
            P.add("sp", lambda e: e.dma_start(out=cs[:], in_=cvec_d), writes=[cs.b], dma=True)
            P.add("act", lambda e: e.activation(out=cs[:], in_=cs[:], func=AF.Silu), reads=[cs.b], writes=[cs.b])
            for g4 in range(4):
                bk = bank()

                def tr(e, g4=g4, bk=bk):
                    for j in range(4):
                        k = g4 * 4 + j
                        r = e.transpose(bk[:, j * 32:j * 32 + NMOD], cs[:, k * 128:(k + 1) * 128], ident[0:NMOD, 0:NMOD])
                    return r
                P.add("pe", tr, reads=[cs.b, ident.b], writes=[bk.b])
                P.add("dve", copy_op("dve", cT[:, g4 * 4:(g4 + 1) * 4, :],
                                     bk[:, 0:128].rearrange("p (a b) -> p a b", a=4)[:, :, 0:NMOD]),
                      reads=[bk.b], writes=[cT.b], join=(g4 > 0))
            P.add("act", lambda e: e.activation(out=nexpa[:], in_=hvec[:, 0:32], func=AF.Exp),
                  reads=[hvec.b], writes=[nexpa.b])
            P.add("dve", lambda e: e.tensor_scalar(out=nexpa[:], in0=nexpa[:], scalar1=-1.0, scalar2=None, op0=ALU.mult),
                  reads=[nexpa.b], writes=[nexpa.b])
            _, s_, sv_ = stage_load(w_gin_d[:, 12288:12352], KC, 64)
            P.add("dve", copy_op("dve", ba_w[:], sv_), reads=[s_.b], writes=[ba_w.b])
            for l in range(2):
                for cc in range(96):
                    w, wv = load_w(w_ada_d[l, :, cc * 128:(cc + 1) * 128], KC, 128)
                    bk = bank()

                    def mm(e, wv=wv, bk=bk):
                        for k in range(KC):
                            r = e.matmul(bk[:, 0:NMOD], lhsT=wv[:, k, :], rhs=cT[:, k, :], start=(k == 0), stop=(k == KC - 1))
                        return r
                    P.add("pe", mm, reads=[w.b, cT.b], writes=[bk.b])
                    P.add("dve", lambda e, bk=bk, l=l, cc=cc: e.tensor_scalar(
                        out=mod[:, l, cc, :], in0=bk[:, 0:NMOD],
                        scalar1=vecT[:, l * 96 + cc:l * 96 + cc + 1], scalar2=None, op0=ALU.add),
                        reads=[bk.b, vecT.b], writes=[mod.b], join=True)
            for l in range(2):
                for s in range(2):
                    gcol = 192 + s * 32 + l * 16
                    for k in range(KC):
                        P.add("dve", lambda e, l=l, s=s, k=k, gcol=gcol: e.tensor_scalar(
                            out=gmod[:, l, s, k, :], in0=mod[:, l, (1 + 3 * s) * 16 + k, :],
                            scalar1=1.0, scalar2=vecT[:, gcol + k:gcol + k + 1], op0=ALU.add, op1=ALU.mult),
                            reads=[mod.b, vecT.b], writes=[gmod.b], join=True)
        P.fence()
        dump(vecT, 'vecT', [128, 640])
        dump(mod, 'mod', [128, 2, 96, NMOD])
        dump(gmod, 'gmod', [128, 2, 2, 16, NMOD])
        dump(cT, 'cT', [128, KC, NMOD], BF16)
        stop_at('setup')

        def load_xT(XT, src_d, ntok, stack):
            xs = [sb([128, D], stack=stack, name="xs") for _ in range(2)]
            nt_ = (ntok + 127) // 128
            for t in range(nt_):
                r0 = t * 128
                rows = min(128, ntok - r0)
                s = xs[t % 2]
                P.add("sp", lambda e, s=s, r0=r0, rows=rows: e.dma_start(out=s[0:rows, :], in_=src_d[r0:r0 + rows, :]),
                      writes=[s.b], dma=True)
                for g4 in range(4):
                    bk = bank()

                    def tr(e, s=s, bk=bk, g4=g4, rows=rows):
                        for j in range(4):
                            k = g4 * 4 + j
                            r = e.transpose(bk[:, j * 128:j * 128 + rows], s[0:rows, k * 128:(k + 1) * 128],
                                            ident[0:rows, 0:rows])
                        return r
                    P.add("pe", tr, reads=[s.b, ident.b], writes=[bk.b])
                    en = ev_eng()
                    P.add(en, copy_op(en, XT[:, g4 * 4:(g4 + 1) * 4, r0:r0 + rows],
                                      bk[:, 0:512].rearrange("p (a b) -> p a b", a=4)[:, :, 0:rows]),
                          reads=[bk.b], writes=[XT.b], join=True)

        def tok_tiles(n):
            res = []
            t0 = 0
            while t0 < n:
                res.append((t0, min(512, n - t0)))
                t0 += 512
            return res

        def rstd_bc(XT, ntok, stack, out_rs):
            sq = [sb([128, 512], BF16, stack=stack, name="sq") for _ in range(2)]
            for (t0, tn) in tok_tiles(ntok):
                bk = bank()
                for k in range(KC):
                    s = sq[k % 2]
                    if k % 2 == 0:
                        P.add("act", lambda e, s=s, k=k, t0=t0, tn=tn: e.activation(
                            out=s[:, 0:tn], in_=XT[:, k, t0:t0 + tn], func=AF.Square), reads=[XT.b], writes=[s.b])
                    else:
                        P.add("pool", lambda e, s=s, k=k, t0=t0, tn=tn: e.tensor_tensor(
                            out=s[:, 0:tn], in0=XT[:, k, t0:t0 + tn], in1=XT[:, k, t0:t0 + tn], op=ALU.mult),
                            reads=[XT.b], writes=[s.b])
                    P.add("pe", lambda e, s=s, k=k, bk=bk, tn=tn: e.matmul(
                        bk[:, 0:tn], lhsT=ones_b[:], rhs=s[:, 0:tn], start=(k == 0), stop=(k == KC - 1)),
                        reads=[s.b, ones_b.b], writes=[bk.b], join=(k > 0))
                P.add("act", lambda e, bk=bk, t0=t0, tn=tn: e.activation(
                    out=out_rs[:, t0:t0 + tn], in_=bk[:, 0:tn], func=AF.Sqrt, scale=1.0 / D, bias=epst[:, 0:1]),
                    reads=[bk.b, epst.b], writes=[out_rs.b], join=True)
            P.add("dve", lambda e: e.reciprocal(out=out_rs[:, 0:ntok], in_=out_rs[:, 0:ntok]),
                  reads=[out_rs.b], writes=[out_rs.b])

        def norm_mod(XT, npr, nsm, l, s, stack, router=None):
            ntok = npr + nsm
            rs = sb([128, NT], stack=stack, name="rs")
            rstd_bc(XT, ntok, stack, rs)
            tmp = [sb([128, NT], stack=stack, name="ntmp") for _ in range(2)]
            shb = (0 + 3 * s) * 16
            ns = nsm // 4
            for k in range(KC):
                tm = tmp[k % 2]
                P.add("dve", lambda e, tm=tm, k=k: e.tensor_tensor(
                    out=tm[:, 0:ntok], in0=XT[:, k, 0:ntok], in1=rs[:, 0:ntok], op=ALU.mult),
                    reads=[XT.b, rs.b], writes=[tm.b])
                P.add("pool", lambda e, tm=tm, k=k: e.tensor_scalar(
                    out=tm[:, 0:npr], in0=tm[:, 0:npr],
                    scalar1=gmod[:, l, s, k, 0:1], scalar2=mod[:, l, shb + k, 0:1], op0=ALU.mult, op1=ALU.add),
                    reads=[tm.b, gmod.b, mod.b], writes=[tm.b])
                if nsm:
                    P.add("dve", lambda e, tm=tm, k=k: e.tensor_tensor(
                        out=tm[:, npr:ntok].rearrange("p (s t) -> p s t", t=4),
                        in0=tm[:, npr:ntok].rearrange("p (s t) -> p s t", t=4),
                        in1=gmod[:, l, s, k, 1:1 + ns].unsqueeze(2).to_broadcast([128, ns, 4]), op=ALU.mult),
                        reads=[tm.b, gmod.b], writes=[tm.b])
                    P.add("dve", lambda e, tm=tm, k=k: e.tensor_tensor(
                        out=tm[:, npr:ntok].rearrange("p (s t) -> p s t", t=4),
                        in0=tm[:, npr:ntok].rearrange("p (s t) -> p s t", t=4),
                        in1=mod[:, l, shb + k, 1:1 + ns].unsqueeze(2).to_broadcast([128, ns, 4]), op=ALU.add),
                        reads=[tm.b, mod.b], writes=[tm.b])
                P.add("act", lambda e, tm=tm, k=k: e.activation(out=HT[:, k, 0:ntok], in_=tm[:, 0:ntok], func=AF.Copy),
                      reads=[tm.b], writes=[HT.b], join=True)
                if router is not None:
                    wrt = router

                    def rmm(e, tm=tm, k=k):
                        for t in range(9):
                            tn = min(128, ntok - t * 128)
                            r = e.matmul(rbank[0:tn, t * 8:(t + 1) * 8], lhsT=tm[:, t * 128:t * 128 + tn],
                                         rhs=wrt[:, k, :], start=(k == 0 and t == 0), stop=(k == KC - 1),
                                         skip_group_check=True)
                        return r
                    P.add("pe", rmm, reads=[tm.b, wrt.b], writes=[rbank.b], join=(k > 0))

        def linear(w_d, col0, ncc, kc, rhs_fn, tiles, epilogue, rhs_bufs):
            cw = 256 if kc * 256 <= 2048 else 128
            per = cw // 128
            cc = 0
            while cc < ncc:
                np_ = min(per, ncc - cc)
                w, wv = load_w(w_d[:, col0 + cc * 128: col0 + (cc + np_) * 128], kc, np_ * 128)
                for j in range(np_):
                    for (t0, tn) in tiles:
                        bk = bank()

                        def mm(e, wv=wv, j=j, t0=t0, tn=tn, bk=bk):
                            for k in range(kc):
                                r = e.matmul(bk[:, 0:tn], lhsT=wv[:, k, j * 128:(j + 1) * 128], rhs=rhs_fn(k, t0, tn),
                                             start=(k == 0), stop=(k == kc - 1))
                            return r
                        P.add("pe", mm, reads=[w.b] + rhs_bufs, writes=[bk.b])
                        epilogue(cc + j, t0, tn, bk)
                cc += np_

        def gdn_pass(stack, blocks, first, last):
            wgrp = sb([128, 6, KC, 128], BF16, stack=stack, name="wgrp")
            totch = sum(b["nch"] for b in blocks)
            BA = sb([64, 8, 64], stack=stack, name="BA")
            gtmp_ = sb([64, 16, 32], stack=stack, name="gtmp")
            pre = {nm: sb([64, totch, 32], stack=stack, name=nm) for nm in ("beta", "g", "esuf", "nbeg", "nbeta")}
            pc = 0
            for blk in blocks:
                C, nch = blk["C"], blk["nch"]
                blk["pc0"] = pc
                for c8 in range(0, nch, 8):
                    bk = bank()

                    def mm(e, C=C, c8=c8, bk=bk, blk=blk):
                        for c in range(8):
                            t0 = blk["tok0"] + (c8 + c) * C
                            for k in range(KC):
                                r = e.matmul(bk[0:C, c * 64:(c + 1) * 64], lhsT=HT[:, k, t0:t0 + C], rhs=ba_w[:, k, :],
                                             start=(k == 0), stop=(k == KC - 1))
                        return r
                    P.add("pe", mm, reads=[HT.b, ba_w.b], writes=[bk.b])
                    P.add("dve", copy_op("dve", BA[0:C, :, :], bk[0:C, 0:512].rearrange("p (a b) -> p a b", a=8)),
                          reads=[bk.b], writes=[BA.b])
                    sl = slice(pc + c8, pc + c8 + 8)
                    be, g_, es, nbg, nbe = (pre[k_][0:C, sl, :] for k_ in ("beta", "g", "esuf", "nbeg", "nbeta"))
                    P.add("act", lambda e, be=be, C=C: e.activation(out=be, in_=BA[0:C, :, 0:32], func=AF.Sigmoid),
                          reads=[BA.b], writes=[pre["beta"].b], join=True)
                    P.add("dve", lambda e, g_=g_, C=C: e.tensor_tensor(
                        out=g_, in0=BA[0:C, :, 32:64], in1=hvec[0:C, 32:64].unsqueeze(1).to_broadcast([C, 8, 32]),
                        op=ALU.add), reads=[BA.b, hvec.b], writes=[pre["g"].b], join=True)
                    P.add("act", lambda e, g_=g_: e.activation(out=g_, in_=g_, func=AF.Exp),
                          reads=[pre["g"].b], writes=[pre["g"].b])
                    P.add("act", lambda e, g_=g_: e.activation(out=g_, in_=g_, func=AF.Ln, bias=1.0),
                          reads=[pre["g"].b], writes=[pre["g"].b])
                    P.add("dve", lambda e, g_=g_, C=C: e.tensor_tensor(
                        out=g_, in0=g_, in1=nexpa[0:C, :].unsqueeze(1).to_broadcast([C, 8, 32]), op=ALU.mult),
                        reads=[pre["g"].b, nexpa.b], writes=[pre["g"].b])
                    P.add("dve", lambda e, nbe=nbe, be=be: e.tensor_scalar(out=nbe, in0=be, scalar1=-1.0, scalar2=None, op0=ALU.mult),
                          reads=[pre["beta"].b], writes=[pre["nbeta"].b], join=True)
                    gflat = pre["g"][0:C, :, :].rearrange("p a b -> p (a b)")[:, (pc + c8) * 32:(pc + c8 + 8) * 32]
                    bk1 = bank()
                    P.add("pe", lambda e, bk1=bk1, C=C, gflat=gflat: e.matmul(
                        bk1[0:C, 0:256], lhsT=masks[0:C, 0, 0:C], rhs=gflat, start=True, stop=True),
                        reads=[masks.b, pre["g"].b], writes=[bk1.b])
                    P.add("act", lambda e, bk1=bk1, C=C: e.activation(
                        out=gtmp_[0:C, 0:8, :].rearrange("p a b -> p (a b)"), in_=bk1[0:C, 0:256], func=AF.Exp),
                        reads=[bk1.b], writes=[gtmp_.b])
                    P.add("dve", lambda e, nbg=nbg, nbe=nbe, C=C: e.tensor_tensor(out=nbg, in0=nbe, in1=gtmp_[0:C, 0:8, :], op=ALU.mult),
                          reads=[pre["nbeta"].b, gtmp_.b], writes=[pre["nbeg"].b], join=True)
                    bk2 = bank()
                    P.add("pe", lambda e, bk2=bk2, C=C, gflat=gflat: e.matmul(
                        bk2[0:C, 0:256], lhsT=masks[0:C, 1, 0:C], rhs=gflat, start=True, stop=True),
                        reads=[masks.b, pre["g"].b], writes=[bk2.b])
                    P.add("act", lambda e, bk2=bk2, C=C, es=es: e.activation(
                        out=es, in_=bk2[0:C, 0:256].rearrange("p (a b) -> p a b", a=8), func=AF.Exp),
                        reads=[bk2.b], writes=[pre["esuf"].b], join=True)
                pc += nch

            QKVZ = sb([128, 6, 512], stack=stack, name="QKVZ")
            Fb = sb([128, 515], stack=stack, name="F")
            cacc = sb([128, 512], stack=stack, name="cacc")
            sqt = sb([128, 512], stack=stack, name="sqt")
            S = [sb([128, 128], stack=stack, name="S") for _ in range(4)]
            og = sb([128, 2, 512], BF16, stack=stack, name="og")
            sgc = sb([48, 512], stack=stack, name="sgc")
            sgo = sb([48, 512], stack=stack, name="sgo")
            tl3 = sb([128, 4, 48], stack=stack, name="tl3")
            WM = 512

            def t2(nm):
                return sb([64, WM], stack=stack, name=nm)
            d = dict(rgt=t2("rgt"), rle=t2("rle"), draw=t2("draw"), dtril=t2("dtril"), dstr=t2("dstr"),
                     p0=t2("p0"), pt0=t2("pt0"), rt=t2("rt"), a=t2("a"),
                     egcb=sb([128, WM], stack=stack, name="egcb"), qd=sb([128, WM], stack=stack, name="qd"),
                     wt=sb([128, WM], stack=stack, name="wt"),
                     vtok=sb([64, 1024], stack=stack, name="vtok"), ktok=sb([64, 512], stack=stack, name="ktok"),
                     kd=sb([64, 1024], stack=stack, name="kd"), otok=sb([64, 1024], stack=stack, name="otok"),
                     ors=sb([64, 8], stack=stack, name="ors"),
                     vn=[sb([64, 128], stack=stack, name="vn") for _ in range(4)])
            d["tb"] = d["rgt"]
            d["tg"] = d["rle"]
            d["at"] = d["draw"]
            d["p1"] = d["dstr"]
            d["pt1"] = d["dtril"]
            d["osq"] = d["kd"]
            nprompt = len([b for b in blocks if not b["sample"]])

            for g in range(16):
                cols = [g * 128, 2048 + g * 128, 4096 + 2 * g * 128, 4096 + (2 * g + 1) * 128,
                        8192 + 2 * g * 128, 8192 + (2 * g + 1) * 128]
                anyqz = any(b["qz"] for b in blocks)
                for j in range(6):
                    if j in (0, 4, 5) and not anyqz:
                        continue
                    _, s_, sv_ = stage_load(w_gin_d[:, cols[j]:cols[j] + 128], KC, 128)
                    ce = cast_eng()
                    P.add(ce, copy_op(ce, wgrp[:, j, :, :], sv_), reads=[s_.b], writes=[wgrp.b], join=True)

                for bi, blk in enumerate(blocks):
                    C, nch, nseq, T = blk["C"], blk["nch"], blk["nseq"], blk["T"]
                    NB = nseq * T
                    tok0 = blk["tok0"]
                    sample = blk["sample"]
                    pc0 = blk["pc0"]
                    if sample:
                        for j in range(4):
                            P.add("sp", lambda e, j=j, c0=cols[j]: e.dma_start(
                                out=sgc[:, j * 128:(j + 1) * 128], in_=sgconv_d[:, c0:c0 + 128]),
                                writes=[sgc.b], dma=True, join=(j > 0))
                        bk = bank()

                        def tr(e, bk=bk):
                            for j in range(4):
                                r = e.transpose(bk[:, j * 48:(j + 1) * 48], sgc[:, j * 128:(j + 1) * 128], ident[0:48, 0:48])
                            return r
                        P.add("pe", tr, reads=[sgc.b, ident.b], writes=[bk.b])
                        P.add("dve", copy_op("dve", tl3[:], bk[:, 0:192].rearrange("p (a b) -> p a b", a=4)),
                              reads=[bk.b], writes=[tl3.b])
                    for j in range(6):
                        if j in (0, 4, 5) and not blk["qz"]:
                            continue
                        bk = bank()

                        def mm(e, j=j, bk=bk, tok0=tok0, NB=NB):
                            for k in range(KC):
                                r = e.matmul(bk[:, 0:NB], lhsT=wgrp[:, j, k, :], rhs=HT[:, k, tok0:tok0 + NB],
                                             start=(k == 0), stop=(k == KC - 1))
                            return r
                        P.add("pe", mm, reads=[wgrp.b, HT.b], writes=[bk.b])
                        if j >= 4:
                            P.add("act", lambda e, j=j, bk=bk, NB=NB: e.activation(
                                out=QKVZ[:, j, 0:NB], in_=bk[:, 0:NB], func=AF.Silu), reads=[bk.b], writes=[QKVZ.b], join=True)
                            continue
                        F = Fb
                        Fv = F[:, 0:nseq * (T + 3)].rearrange("p (s t) -> p s t", s=nseq)
                        P.add("act", lambda e, Fv=Fv, bk=bk, NB=NB, nseq=nseq: e.activation(
                            out=Fv[:, :, 3:], in_=bk[:, 0:NB].rearrange("p (s t) -> p s t", s=nseq), func=AF.Copy),
                            reads=[bk.b], writes=[F.b])
                        if sample:
                            P.add("pool", lambda e, Fv=Fv, j=j: e.tensor_copy(
                                out=Fv[:, :, 0:3], in_=tl3[:, j, :].rearrange("p (s r) -> p s r", r=3)),
                                reads=[tl3.b], writes=[F.b], join=True)
                        elif first and bi == 0:
                            P.add("pool", lambda e, Fv=Fv: e.memset(Fv[:, 0, 0:3], 0.0), writes=[F.b], join=True)
                        elif (not first) and bi == 0:
                            P.add("pool", lambda e, Fv=Fv, j=j, g=g: e.tensor_scalar(
                                out=Fv[:, 0, 0:3], in0=tails[:, j, g, :], scalar1=f1[:, 0:1], scalar2=None, op0=ALU.mult),
                                reads=[tails.b, f1.b], writes=[F.b], join=True)
                        else:
                            P.add("pool", lambda e, Fv=Fv, j=j, g=g: e.tensor_copy(out=Fv[:, 0, 0:3], in_=tails[:, j, g, :]),
                                  reads=[tails.b], writes=[F.b], join=True)
                        ca = cacc
                        cav = ca[:, 0:NB].rearrange("p (s t) -> p s t", s=nseq)
                        wcol = 272 + cols[j] // 128
                        en = "dve"
                        P.add(en, lambda e, cav=cav, Fv=Fv, wcol=wcol, T=T: e.tensor_scalar(
                            out=cav, in0=Fv[:, :, 0:T], scalar1=vecT[:, wcol:wcol + 1], scalar2=None, op0=ALU.mult),
                            reads=[F.b, vecT.b], writes=[ca.b])
                        for tp in range(1, 4):
                            P.add(en, lambda e, cav=cav, Fv=Fv, wcol=wcol, T=T, tp=tp: e.scalar_tensor_tensor(
                                out=cav, in0=Fv[:, :, tp:tp + T], scalar=vecT[:, wcol + 64 * tp:wcol + 64 * tp + 1],
                                in1=cav, op0=ALU.mult, op1=ALU.add), reads=[F.b, vecT.b, ca.b], writes=[ca.b])
                        if sample:
                            P.add("pool", lambda e, Fv=Fv, j=j: e.tensor_copy(
                                out=tl3[:, j, :].rearrange("p (s r) -> p s r", r=3), in_=Fv[:, :, 4:7]),
                                reads=[F.b], writes=[tl3.b])
                        else:
                            P.add("pool", lambda e, Fv=Fv, j=j, g=g, T=T: e.tensor_copy(
                                out=tails[:, j, g, :], in_=Fv[:, 0, T:T + 3]), reads=[F.b], writes=[tails.b])
                        if j >= 2:
                            P.add("act", lambda e, j=j, ca=ca, NB=NB: e.activation(
                                out=QKVZ[:, j, 0:NB], in_=ca[:, 0:NB], func=AF.Silu), reads=[ca.b], writes=[QKVZ.b], join=True)
                        else:
                            P.add("act", lambda e, ca=ca, NB=NB: e.activation(out=ca[:, 0:NB], in_=ca[:, 0:NB], func=AF.Silu),
                                  reads=[ca.b], writes=[ca.b])
                            P.add("act", lambda e, ca=ca, NB=NB: e.activation(
                                out=sqt[:, 0:NB], in_=ca[:, 0:NB], func=AF.Square), reads=[ca.b], writes=[sqt.b])
                            bk2 = bank()
                            P.add("pe", lambda e, bk2=bk2, NB=NB: e.matmul(
                                bk2[:, 0:NB], lhsT=ones_f[:], rhs=sqt[:, 0:NB], start=True, stop=True),
                                reads=[sqt.b, ones_f.b], writes=[bk2.b])
                            P.add("act", lambda e, bk2=bk2, NB=NB: e.activation(
                                out=sqt[:, 0:NB], in_=bk2[:, 0:NB], func=AF.Sqrt, bias=epst[:, 0:1]),
                                reads=[bk2.b, epst.b], writes=[sqt.b])
                            P.add("dve", lambda e, NB=NB: e.reciprocal(out=sqt[:, 0:NB], in_=sqt[:, 0:NB]),
                                  reads=[sqt.b], writes=[sqt.b])
                            sc_ = (128.0 ** -0.5) if j == 0 else 1.0
                            P.add("dve", lambda e, j=j, ca=ca, NB=NB, sc_=sc_: e.scalar_tensor_tensor(
                                out=QKVZ[:, j, 0:NB], in0=ca[:, 0:NB], scalar=sc_, in1=sqt[:, 0:NB],
                                op0=ALU.mult, op1=ALU.mult), reads=[ca.b, sqt.b], writes=[QKVZ.b], join=True)
                    if sample or (last and bi == nprompt - 1):
                        nr = 48 if sample else 3
                        bk = bank()

                        def tr(e, bk=bk, nr=nr, sample=sample, g=g):
                            for j in range(4):
                                src = tl3[:, j, :] if sample else tails[:, j, g, :]
                                r = e.transpose(bk[0:nr, j * 128:(j + 1) * 128], src, ident[:])
                            return r
                        P.add("pe", tr, reads=[tl3.b if sample else tails.b, ident.b], writes=[bk.b])
                        P.add("dve", copy_op("dve", sgo[0:nr, :], bk[0:nr, :]), reads=[bk.b], writes=[sgo.b])
                        dd = gconvs_d if sample else gconvp_d
                        for j in range(4):
                            P.add("pool", lambda e, j=j, nr=nr, dd=dd, c0=cols[j]: e.dma_start(
                                out=dd[:, c0:c0 + 128], in_=sgo[0:nr, j * 128:(j + 1) * 128]),
                                reads=[sgo.b], dma=True, out=True)

                    n = 4
                    for c0 in range(0, nch, n):
                        need_o = blk["o_from"] is not None and c0 >= blk["o_from"]
                        W = n * 2 * C
                        HC = n * 2
                        st0 = c0 * C
                        Mle = masks[0:C, 0, 0:C]
                        Mgt = masks[0:C, 1, 0:C]
                        Mtril = masks[0:C, 2, 0:C]
                        Mstr = masks[0:C, 3, 0:C]
                        psl = slice(pc0 + c0, pc0 + c0 + n)
                        hsl = slice(2 * g, 2 * g + 2)
                        gsl = pre["g"][0:C, psl, hsl]

                        def v4(t, C=C, W=W):
                            return t[0:C, 0:W].rearrange("p (a h c) -> p a h c", a=n, h=2)

                        def v3(t, C=C, W=W, HC=HC):
                            return t[0:C, 0:W].rearrange("p (a c) -> p a c", a=HC)

                        def f2(t, C=C, W=W):
                            return t[0:C, 0:W]

                        def bc_hc(ap2, C=C):
                            return ap2.unsqueeze(3).to_broadcast([C, n, 2, C])

                        def bc_m(m, C=C):
                            return m.unsqueeze(1).unsqueeze(1).to_broadcast([C, n, 2, C])
                        P.add("dve", lambda e, v4=v4, bc_hc=bc_hc, bc_m=bc_m, gsl=gsl, Mgt=Mgt: e.tensor_tensor(
                            out=v4(d["rgt"]), in0=bc_m(Mgt), in1=bc_hc(gsl), op=ALU.mult),
                            reads=[masks.b, pre["g"].b], writes=[d["rgt"].b])
                        P.add("pool", lambda e, v4=v4, bc_hc=bc_hc, bc_m=bc_m, gsl=gsl, Mle=Mle: e.tensor_tensor(
                            out=v4(d["rle"]), in0=bc_m(Mle), in1=bc_hc(gsl), op=ALU.mult),
                            reads=[masks.b, pre["g"].b], writes=[d["rle"].b])
                        bkG = bank()
                        P.add("pe", lambda e, bkG=bkG, Mle=Mle, W=W, C=C, f2=f2: e.matmul(
                            bkG[0:C, 0:W], lhsT=Mle, rhs=f2(d["rgt"]), start=True, stop=True),
                            reads=[masks.b, d["rgt"].b], writes=[bkG.b])
                        P.add("act", lambda e, bkG=bkG, W=W, C=C, f2=f2: e.activation(
                            out=f2(d["draw"]), in_=bkG[0:C, 0:W], func=AF.Exp), reads=[bkG.b], writes=[d["draw"].b])
                        P.add("pool", lambda e, v4=v4, bc_m=bc_m, Mtril=Mtril: e.tensor_tensor(
                            out=v4(d["dtril"]), in0=v4(d["draw"]), in1=bc_m(Mtril), op=ALU.mult),
                            reads=[d["draw"].b, masks.b], writes=[d["dtril"].b])
                        P.add("dve", lambda e, v4=v4, bc_m=bc_m, Mstr=Mstr: e.tensor_tensor(
                            out=v4(d["dstr"]), in0=v4(d["draw"]), in1=bc_m(Mstr), op=ALU.mult),
                            reads=[d["draw"].b, masks.b], writes=[d["dstr"].b])
                        bkE = bank()
                        P.add("pe", lambda e, bkE=bkE, W=W, C=C, f2=f2: e.matmul(
                            bkE[:, 0:W], lhsT=ones_f[0:C, :], rhs=f2(d["rle"]), start=True, stop=True),
                            reads=[ones_f.b, d["rle"].b], writes=[bkE.b])
                        P.add("act", lambda e, bkE=bkE, W=W: e.activation(
                            out=d["egcb"][:, 0:W], in_=bkE[:, 0:W], func=AF.Exp), reads=[bkE.b], writes=[d["egcb"].b])
                        qsl = QKVZ[:, 0, st0:st0 + n * C].rearrange("p (a c) -> p a c", a=n)
                        ksl = QKVZ[:, 1, st0:st0 + n * C].rearrange("p (a c) -> p a c", a=n)
                        if need_o:
                            P.add("dve", lambda e, qsl=qsl, W=W, C=C: e.tensor_tensor(
                                out=d["qd"][:, 0:W].rearrange("p (a h c) -> p a h c", a=n, h=2),
                                in0=qsl.unsqueeze(2).to_broadcast([128, n, 2, C]),
                                in1=d["egcb"][:, 0:W].rearrange("p (a h c) -> p a h c", a=n, h=2), op=ALU.mult),
                                reads=[QKVZ.b, d["egcb"].b], writes=[d["qd"].b])
                        bkK = bank()

                        def mmk(e, bkK=bkK, qsl=qsl, ksl=ksl, C=C, need_o=need_o):
                            for c in range(n):
                                r = e.matmul(bkK[0:C, (2 * c) * C:(2 * c + 1) * C], lhsT=ksl[:, c, :], rhs=ksl[:, c, :],
                                             start=True, stop=True)
                                if need_o:
                                    r = e.matmul(bkK[0:C, (2 * c + 1) * C:(2 * c + 2) * C], lhsT=qsl[:, c, :], rhs=ksl[:, c, :],
                                                 start=True, stop=True)
                            return r
                        P.add("pe", mmk, reads=[QKVZ.b], writes=[bkK.b])
                        kkv = bkK[0:C, 0:W].rearrange("p (a h c) -> p a h c", a=n, h=2)
                        P.add("dve", lambda e, v4=v4, kkv=kkv, C=C: e.tensor_tensor(
                            out=v4(d["p0"]), in0=v4(d["dstr"]),
                            in1=kkv[:, :, 0:1, :].to_broadcast([C, n, 2, C]), op=ALU.mult),
                            reads=[d["dstr"].b, bkK.b], writes=[d["p0"].b])
                        nbs = pre["nbeta"][0:C, psl, hsl]
                        P.add("dve", lambda e, v4=v4, bc_hc=bc_hc, nbs=nbs: e.tensor_tensor(
                            out=v4(d["p0"]), in0=v4(d["p0"]), in1=bc_hc(nbs), op=ALU.mult),
                            reads=[d["p0"].b, pre["nbeta"].b], writes=[d["p0"].b])
                        if need_o:
                            P.add("dve", lambda e, v4=v4, kkv=kkv, C=C: e.tensor_tensor(
                                out=v4(d["a"]), in0=v4(d["dtril"]),
                                in1=kkv[:, :, 1:2, :].to_broadcast([C, n, 2, C]), op=ALU.mult),
                                reads=[d["dtril"].b, bkK.b], writes=[d["a"].b])

                        def transpose_hc(src, dst, eng_ev, C=C, W=W, HC=HC):
                            bkT = bank()

                            def tr(e, src=src, bkT=bkT):
                                for hc in range(HC):
                                    r = e.transpose(bkT[0:C, hc * C:(hc + 1) * C], src[0:C, hc * C:(hc + 1) * C], ident[0:C, 0:C])
                                return r
                            P.add("pe", tr, reads=[src.b, ident.b], writes=[bkT.b])
                            P.add(eng_ev, copy_op(eng_ev, dst[0:C, 0:W], bkT[0:C, 0:W]), reads=[bkT.b], writes=[dst.b])
                        transpose_hc(d["p0"], d["pt0"], "act")
                        if need_o:
                            transpose_hc(d["a"], d["at"], "act")
                        P.add("dve", lambda e, v3=v3, HC=HC, C=C: e.tensor_tensor(
                            out=v3(d["rt"]), in0=v3(d["pt0"]),
                            in1=ident[0:C, 0:C].unsqueeze(1).to_broadcast([C, HC, C]), op=ALU.add),
                            reads=[d["pt0"].b, ident.b], writes=[d["rt"].b])
                        nsteps = {64: 5, 4: 1}[C]
                        cur = 0
                        for stp in range(nsteps):
                            Pc, PTc = d["p%d" % cur], d["pt%d" % cur]
                            Pn, PTn = d["p%d" % (1 - cur)], d["pt%d" % (1 - cur)]
                            lastst = (stp == nsteps - 1)
                            bkP = bank()

                            def mmp(e, Pc=Pc, PTc=PTc, bkP=bkP, C=C, HC=HC):
                                for hc in range(HC):
                                    sl = slice(hc * C, (hc + 1) * C)
                                    r = e.matmul(bkP[0:C, sl], lhsT=PTc[0:C, sl], rhs=Pc[0:C, sl], start=True, stop=True)
                                return r
                            P.add("pe", mmp, reads=[Pc.b, PTc.b], writes=[bkP.b])
                            if not lastst:
                                bkQ = bank()

                                def mmq(e, Pc=Pc, PTc=PTc, bkQ=bkQ, C=C, HC=HC):
                                    for hc in range(HC):
                                        sl = slice(hc * C, (hc + 1) * C)
                                        r = e.matmul(bkQ[0:C, sl], lhsT=Pc[0:C, sl], rhs=PTc[0:C, sl], start=True, stop=True)
                                    return r
                                P.add("pe", mmq, reads=[Pc.b, PTc.b], writes=[bkQ.b])
                            P.add("act", copy_op("act", Pn[0:C, 0:W], bkP[0:C, 0:W]), reads=[bkP.b], writes=[Pn.b])
                            if not lastst:
                                P.add("dve", copy_op("dve", PTn[0:C, 0:W], bkQ[0:C, 0:W]), reads=[bkQ.b], writes=[PTn.b])
                            bkR = bank()

                            def mmr(e, Pn=Pn, bkR=bkR, C=C, HC=HC):
                                for hc in range(HC):
                                    sl = slice(hc * C, (hc + 1) * C)
                                    r = e.matmul(bkR[0:C, sl], lhsT=Pn[0:C, sl], rhs=d["rt"][0:C, sl], start=True, stop=True)
                                return r
                            P.add("pe", mmr, reads=[Pn.b, d["rt"].b], writes=[bkR.b])
                            P.add("dve", lambda e, bkR=bkR, W=W, C=C, f2=f2: e.tensor_tensor(
                                out=f2(d["rt"]), in0=f2(d["rt"]), in1=bkR[0:C, 0:W], op=ALU.add),
                                reads=[d["rt"].b, bkR.b], writes=[d["rt"].b])
                            cur = 1 - cur
                        bsl = pre["beta"][0:C, psl, hsl]
                        ngs = pre["nbeg"][0:C, psl, hsl]
                        P.add("pool", lambda e, v4=v4, bc_hc=bc_hc, bsl=bsl: e.tensor_tensor(
                            out=v4(d["tb"]), in0=v4(d["rt"]), in1=bc_hc(bsl), op=ALU.mult),
                            reads=[d["rt"].b, pre["beta"].b], writes=[d["tb"].b])
                        P.add("dve", lambda e, v4=v4, bc_hc=bc_hc, ngs=ngs: e.tensor_tensor(
                            out=v4(d["tg"]), in0=v4(d["rt"]), in1=bc_hc(ngs), op=ALU.mult),
                            reads=[d["rt"].b, pre["nbeg"].b], writes=[d["tg"].b])
                        for q4 in range(0, HC, 4):
                            bkV = bank()

                            def trv(e, bkV=bkV, q4=q4, C=C, st0=st0):
                                for x in range(4):
                                    hc = q4 + x
                                    c, h = hc // 2, hc % 2
                                    r = e.transpose(bkV[0:C, x * 128:(x + 1) * 128],
                                                    QKVZ[:, 2 + h, st0 + c * C:st0 + (c + 1) * C], ident[:])
                                return r
                            P.add("pe", trv, reads=[QKVZ.b, ident.b], writes=[bkV.b])
                            en = ev_eng()
                            P.add(en, copy_op(en, d["vtok"][0:C, q4 * 128:(q4 + 4) * 128], bkV[0:C, 0:512]),
                                  reads=[bkV.b], writes=[d["vtok"].b], join=(q4 > 0))
                        bkV = bank()

                        def trk(e, bkV=bkV, C=C, st0=st0):
                            for c in range(4):
                                r = e.transpose(bkV[0:C, c * 128:(c + 1) * 128],
                                                QKVZ[:, 1, st0 + c * C:st0 + (c + 1) * C], ident[:])
                            return r
                        P.add("pe", trk, reads=[QKVZ.b, ident.b], writes=[bkV.b])
                        en = ev_eng()
                        P.add(en, copy_op(en, d["ktok"][0:C, 0:512], bkV[0:C, 0:512]), reads=[bkV.b], writes=[d["ktok"].b])
                        ess = pre["esuf"][0:C, psl, hsl]
                        P.add("pool", lambda e, ess=ess, C=C: e.tensor_tensor(
                            out=d["kd"][0:C, :].rearrange("p (a h c) -> p a h c", a=n, h=2),
                            in0=d["ktok"][0:C, :].rearrange("p (a c) -> p a c", a=n).unsqueeze(2).to_broadcast([C, n, 2, 128]),
                            in1=ess.unsqueeze(3).to_broadcast([C, n, 2, 128]), op=ALU.mult),
                            reads=[d["ktok"].b, pre["esuf"].b], writes=[d["kd"].b])
                        bkW = bank()

                        def mmw(e, bkW=bkW, C=C, HC=HC):
                            for hc in range(HC):
                                c = hc // 2
                                r = e.matmul(bkW[:, hc * C:(hc + 1) * C], lhsT=d["ktok"][0:C, c * 128:(c + 1) * 128],
                                             rhs=d["tg"][0:C, hc * C:(hc + 1) * C], start=True, stop=True)
                            return r
                        P.add("pe", mmw, reads=[d["ktok"].b, d["tg"].b], writes=[bkW.b])
                        P.add("act", copy_op("act", d["wt"][:, 0:W], bkW[:, 0:W]), reads=[bkW.b], writes=[d["wt"].b])
                        bkO = None
                        for c in range(n):
                            for h in range(2):
                                hc = c * 2 + h
                                head = 2 * g + h
                                sl = slice(hc * C, (hc + 1) * C)
                                vsl = slice(hc * 128, (hc + 1) * 128)
                                if sample:
                                    St = S[hc % 4]
                                    P.add("sp", lambda e, St=St, sq_=c0 + c, head=head: e.dma_start(
                                        out=St[:], in_=sgdn_d[sq_, head]), writes=[St.b], dma=True)
                                else:
                                    St = S[h]
                                    if bi == 0 and c0 == 0 and c == 0:
                                        if first:
                                            P.add("pool", lambda e, St=St: e.memset(St[:], 0.0), writes=[St.b])
                                        else:
                                            P.add("sp", lambda e, St=St, head=head: e.dma_start(out=St[:], in_=sscr_d[head]),
                                                  reads=[bscr[head]], writes=[St.b], dma=True)
                                            P.add("dve", lambda e, St=St: e.tensor_scalar(
                                                out=St[:], in0=St[:], scalar1=f1[:, 0:1], scalar2=None, op0=ALU.mult),
                                                reads=[St.b, f1.b], writes=[St.b])
                                bkVn = bank()
                                vn = d["vn"][hc % 4]

                                def mm1(e, bkVn=bkVn, sl=sl, vsl=vsl, St=St, C=C):
                                    e.matmul(bkVn[0:C, 0:128], lhsT=d["tb"][0:C, sl], rhs=d["vtok"][0:C, vsl], start=True, stop=False)
                                    return e.matmul(bkVn[0:C, 0:128], lhsT=d["wt"][:, sl], rhs=St[:], start=False, stop=True)
                                P.add("pe", mm1, reads=[d["tb"].b, d["vtok"].b, d["wt"].b, St.b], writes=[bkVn.b])
                                P.add("act", copy_op("act", vn[0:C, :], bkVn[0:C, 0:128]), reads=[bkVn.b], writes=[vn.b])
                                if need_o:
                                    if hc % 4 == 0:
                                        bkO = obank()

                                    def mm2(e, bkO=bkO, sl=sl, St=St, vn=vn, x=hc % 4, C=C):
                                        e.matmul(bkO[0:C, x * 128:(x + 1) * 128], lhsT=d["qd"][:, sl], rhs=St[:], start=True, stop=False)
                                        return e.matmul(bkO[0:C, x * 128:(x + 1) * 128], lhsT=d["at"][0:C, sl], rhs=vn[0:C, :],
                                                        start=False, stop=True)
                                    P.add("pe", mm2, reads=[d["qd"].b, d["at"].b, St.b, vn.b], writes=[bkO.b], join=(hc % 4 != 0))
                                    if hc % 4 == 3:
                                        q4 = hc - 3
                                        P.add("dve", copy_op("dve", d["otok"][0:C, q4 * 128:(q4 + 4) * 128], bkO[0:C, 0:512]),
                                              reads=[bkO.b], writes=[d["otok"].b], join=(q4 > 0))
                                bkS = bank()
                                P.add("pe", lambda e, bkS=bkS, vsl=vsl, vn=vn, C=C: e.matmul(
                                    bkS[:, 0:128], lhsT=d["kd"][0:C, vsl], rhs=vn[0:C, :], start=True, stop=True),
                                    reads=[d["kd"].b, vn.b], writes=[bkS.b])
                                gcol = hc * C + C - 1
                                P.add("dve", lambda e, bkS=bkS, St=St, gcol=gcol: e.scalar_tensor_tensor(
                                    out=St[:], in0=St[:], scalar=d["egcb"][:, gcol:gcol + 1], in1=bkS[:, 0:128],
                                    op0=ALU.mult, op1=ALU.add), reads=[St.b, d["egcb"].b, bkS.b], writes=[St.b])
                                if sample:
                                    P.add("pool", lambda e, St=St, sq_=c0 + c, head=head: e.dma_start(
                                        out=gdns_d[sq_, head], in_=St[:]), reads=[St.b], dma=True, out=True)
                                elif bi == nprompt - 1 and c0 + n == nch and c == n - 1:
                                    if last:
                                        P.add("pool", lambda e, St=St, head=head: e.dma_start(out=gdnp_d[head], in_=St[:]),
                                              reads=[St.b], dma=True, out=True)
                                    else:
                                        P.add("pool", lambda e, St=St, head=head: e.dma_start(out=sscr_d[head], in_=St[:]),
                                              reads=[St.b], writes=[bscr[head]], dma=True)
                        if need_o:
                            ot_ = d["otok"][0:C, :]
                            P.add("act", lambda e, ot_=ot_, C=C: e.activation(out=d["osq"][0:C, :], in_=ot_, func=AF.Square),
                                  reads=[d["otok"].b], writes=[d["osq"].b])
                            P.add("dve", lambda e, HC=HC, C=C: e.tensor_reduce(
                                out=d["ors"][0:C, 0:HC], in_=d["osq"][0:C, :].rearrange("p (a c) -> p a c", a=HC),
                                axis=AX.X, op=ALU.add), reads=[d["osq"].b], writes=[d["ors"].b])
                            P.add("act", lambda e, HC=HC, C=C: e.activation(
                                out=d["ors"][0:C, 0:HC], in_=d["ors"][0:C, 0:HC], func=AF.Sqrt, scale=1.0 / 128, bias=epst[0:C, 0:1]),
                                reads=[d["ors"].b, epst.b], writes=[d["ors"].b])
                            P.add("dve", lambda e, HC=HC, C=C: e.reciprocal(out=d["ors"][0:C, 0:HC], in_=d["ors"][0:C, 0:HC]),
                                  reads=[d["ors"].b], writes=[d["ors"].b])
                            P.add("dve", lambda e, HC=HC, C=C, ot_=ot_: e.tensor_tensor(
                                out=ot_.rearrange("p (a c) -> p a c", a=HC), in0=ot_.rearrange("p (a c) -> p a c", a=HC),
                                in1=d["ors"][0:C, 0:HC].unsqueeze(2).to_broadcast([C, HC, 128]), op=ALU.mult),
                                reads=[d["otok"].b, d["ors"].b], writes=[d["otok"].b])
                            bkOT = bank()

                            def tro(e, bkOT=bkOT, C=C, HC=HC):
                                for hc in range(HC):
                                    r = e.transpose(bkOT[:, hc * C:(hc + 1) * C], d["otok"][0:C, hc * 128:(hc + 1) * 128],
                                                    ident[0:C, 0:C])
                                return r
                            P.add("pe", tro, reads=[d["otok"].b, ident.b], writes=[bkOT.b])
                            for h in range(2):
                                P.add("dve", lambda e, h=h, bkOT=bkOT, C=C, st0=st0: e.scalar_tensor_tensor(
                                    out=og[:, h, st0:st0 + n * C].rearrange("p (a c) -> p a c", a=n),
                                    in0=bkOT[:, 0:n * 2 * C].rearrange("p (a h c) -> p a h c", a=n, h=2)[:, :, h, :],
                                    scalar=vecT[:, 576:577],
                                    in1=QKVZ[:, 4 + h, st0:st0 + n * C].rearrange("p (a c) -> p a c", a=n),
                                    op0=ALU.mult, op1=ALU.mult), reads=[bkOT.b, vecT.b, QKVZ.b], writes=[og.b], join=True)
                        if dbg.get('stop') == 'sub' and g == dbg.get('g', 0) and bi == dbg.get('bi', 0) and c0 == dbg.get('c0', 0) and first == dbg.get('first', True):
                            for nm_ in ('beta', 'g', 'esuf', 'nbeg'):
                                dump(pre[nm_], 'pre_' + nm_, [64, totch, 32])
                            dump(QKVZ, 'QKVZ', [128, 6, 512])
                            for nm_ in ('rt', 'tb', 'tg', 'at', 'p0', 'pt0'):
                                dump(d[nm_], nm_, [64, 512])
                            for nm_ in ('egcb', 'qd', 'wt'):
                                dump(d[nm_], nm_, [128, 512])
                            for nm_ in ('vtok', 'kd', 'otok'):
                                dump(d[nm_], nm_, [64, 1024])
                            dump(d['ktok'], 'ktok', [64, 512])
                            dump(og, 'og', [128, 2, 512], BF16)
                            dump(S[0], 'S0', [128, 128])
                            dump(S[1], 'S1', [128, 128])
                            dbg['_halt'] = True
                            return
                    if blk["o_from"] is not None:
                        ot0 = blk["otok0"]
                        lo = blk.get("olo", 0)
                        for h in range(2):
                            head = 2 * g + h
                            P.add("pool", lambda e, h=h, head=head, ot0=ot0, NB=NB, lo=lo: e.dma_start(
                                out=oscr_d[head, :, ot0:ot0 + NB - lo], in_=og[:, h, lo:NB]),
                                reads=[og.b], writes=[boscr[head]], dma=True, join=True)

        with scope() as sc:
            XT = sb([128, KC, NT], stack=sc, name="XTp")
            load_xT(XT, xp_d, NPR, sc)
            norm_mod(XT, NPR, 0, 0, 0, sc)
            dump(XT, 'XT_P', [128, KC, NT])
        P.fence()
        dump(HT, 'HT_P', [128, KC, NT], BF16)
        stop_at('normP')
        blocksP = [dict(C=64, nch=8, tok0=0, nseq=1, T=512, sample=False, otok0=0, qz=bool(dbg.get('p_full')), o_from=(0 if dbg.get('p_full') else None)),
                   dict(C=64, nch=8, tok0=512, nseq=1, T=512, sample=False, otok0=NPR + NSM, qz=True, o_from=4, olo=508)]
        with scope() as sc:
            gdn_pass(sc, blocksP, first=True, last=False)
        P.fence()
        if dbg.get('_halt'):
            raise _Stop()
        with scope() as sc:
            XT = sb([128, KC, NT], stack=sc, name="XTo")
            load_xT(XT, xo_d, NPR + NSM, sc)
            norm_mod(XT, NPR, NSM, 0, 0, sc)
        P.fence()
        blocksO = [dict(C=64, nch=8, tok0=0, nseq=1, T=512, sample=False, otok0=0, qz=True, o_from=0),
                   dict(C=64, nch=8, tok0=512, nseq=1, T=512, sample=False, otok0=512, qz=True, o_from=0),
                   dict(C=4, nch=16, tok0=1024, nseq=16, T=4, sample=True, otok0=1024, qz=True, o_from=0)]
        with scope() as sc:
            gdn_pass(sc, blocksO, first=False, last=True)
        P.fence()
        if 'oscr' in dbg.get('dumps', ()):
            dd_ = nc.dram_tensor('dbg_oscr', [32, 128, NT], BF16, kind='ExternalOutput').ap()
            P.add('sp', lambda e: e.dma_start(out=dd_, in_=oscr_d), reads=boscr, dma=True, out=True)
        stop_at('gdnO')
        if dbg.get('_halt'):
            raise _Stop()

        XT = sb([128, KC, NT], stack=fin, name="XT")
        with scope() as sc:
            load_xT(XT, xo_d, NT, sc)
        tiles = tok_tiles(NT)
        NSA = (NT - NPR) // 4
        ACT_FS = 8
        AT_ = sb([128, ACT_FS, NT], BF16, stack=fin, name="AT")
        gtmp = sb([128, 128], stack=fin, name="gtmp")

        def resid_epilogue(l, s):
            gab = (2 + 3 * s) * 16

            def ep(cc, t0, tn, bk):
                if t0 < NPR:
                    P.add("dve", lambda e: e.scalar_tensor_tensor(
                        out=XT[:, cc, t0:t0 + tn], in0=bk[:, 0:tn], scalar=mod[:, l, gab + cc, 0:1],
                        in1=XT[:, cc, t0:t0 + tn], op0=ALU.mult, op1=ALU.add),
                        reads=[bk.b, mod.b, XT.b], writes=[XT.b])
                else:
                    ns = tn // 4
                    P.add("dve", lambda e: e.tensor_tensor(
                        out=gtmp[:, 0:tn].rearrange("p (s t) -> p s t", t=4),
                        in0=bk[:, 0:tn].rearrange("p (s t) -> p s t", t=4),
                        in1=mod[:, l, gab + cc, 1:1 + ns].unsqueeze(2).to_broadcast([128, ns, 4]), op=ALU.mult),
                        reads=[bk.b, mod.b], writes=[gtmp.b])
                    P.add("dve", lambda e: e.tensor_tensor(
                        out=XT[:, cc, t0:t0 + tn], in0=XT[:, cc, t0:t0 + tn], in1=gtmp[:, 0:tn], op=ALU.add),
                        reads=[gtmp.b, XT.b], writes=[XT.b])
            return ep

        at_rhs = lambda k, t0, tn: AT_[:, k, t0:t0 + tn]
        ht_rhs = lambda k, t0, tn: HT[:, k, t0:t0 + tn]

        ep = resid_epilogue(0, 0)
        for hg in range(4):
            for hh in range(8):
                head = hg * 8 + hh
                P.add("sp", lambda e, hh=hh, head=head: e.dma_start(out=AT_[:, hh, :], in_=oscr_d[head]),
                      reads=[boscr[head]], writes=[AT_.b], dma=True, join=(hh > 0))
            linear(w_gout_d[hg * 1024:(hg + 1) * 1024, :], 0, 16, 8, at_rhs, tiles, ep, [AT_.b])

        dump(XT, 'XT_gout', [128, KC, NT])

        def glu_mlp(w_up_d, w_dn_d, hid, l, gate_bc=None):
            nhc = hid // 128
            ep = resid_epilogue(l, 1)
            for f0 in range(0, nhc, ACT_FS):
                fs = min(ACT_FS, nhc - f0)

                def ep_gate(cc, t0, tn, bk):
                    P.add("act", lambda e: e.activation(out=AT_[:, cc, t0:t0 + tn], in_=bk[:, 0:tn], func=AF.Silu),
                          reads=[bk.b], writes=[AT_.b], join=True)

                def ep_up(cc, t0, tn, bk):
                    P.add("dve", lambda e: e.tensor_tensor(out=AT_[:, cc, t0:t0 + tn], in0=AT_[:, cc, t0:t0 + tn],
                                                          in1=bk[:, 0:tn], op=ALU.mult),
                          reads=[bk.b, AT_.b], writes=[AT_.b])
                    if gate_bc is not None:
                        P.add("pool", lambda e: e.tensor_tensor(out=AT_[:, cc, t0:t0 + tn], in0=AT_[:, cc, t0:t0 + tn],
                                                               in1=gate_bc[:, t0:t0 + tn], op=ALU.mult),
                              reads=[gate_bc.b, AT_.b], writes=[AT_.b])
                linear(w_up_d, f0 * 128, fs, KC, ht_rhs, tiles, ep_gate, [HT.b])
                linear(w_up_d, hid + f0 * 128, fs, KC, ht_rhs, tiles, ep_up, [HT.b])
                linear(w_dn_d[f0 * 128:(f0 + fs) * 128, :], 0, 16, fs, at_rhs, tiles, ep, [AT_.b])

        with scope() as sc:
            norm_mod(XT, NPR, NT - NPR, 0, 1, sc)
        glu_mlp(w_fup_d, w_fdn_d, FFN, 0)
        dump(XT, 'XT_ffn', [128, KC, NT])

        with scope() as sc:
            norm_mod(XT, NPR, NT - NPR, 1, 0, sc)
        with scope() as sc:
            F = sb([128, NPR + 2 + NSA * 6], stack=sc, name="Fs")
            Fp = F[:, 0:NPR + 2]
            Fsm = F[:, NPR + 2:NPR + 2 + NSA * 6].rearrange("p (s t) -> p s t", t=6)
            cgt = [sb([128, 512], stack=sc, name="cgt") for _ in range(2)]
            yv = sb([128, NT], stack=sc, name="yv")
            sst = sb([32, 512], stack=sc, name="sst")
            ssT = sb([128, KC, 32], stack=sc, name="ssT")
            sso = sb([128, KC, 34], stack=sc, name="sso")
            sot = sb([34, 512], stack=sc, name="sot")
            for g4 in range(4):
                P.add("sp", lambda e, g4=g4: e.dma_start(out=sst[:], in_=ssconv_d[:, g4 * 512:(g4 + 1) * 512]),
                      writes=[sst.b], dma=True)
                bk = bank()

                def tr(e, g4=g4, bk=bk):
                    for j in range(4):
                        r = e.transpose(bk[:, j * 32:(j + 1) * 32], sst[:, j * 128:(j + 1) * 128], ident[0:32, 0:32])
                    return r
                P.add("pe", tr, reads=[sst.b, ident.b], writes=[bk.b])
                P.add("dve", copy_op("dve", ssT[:, g4 * 4:(g4 + 1) * 4, :], bk[:, 0:128].rearrange("p (a b) -> p a b", a=4)),
                      reads=[bk.b], writes=[ssT.b], join=(g4 > 0))
            P.add("pool", lambda e: e.memset(F[:], 0.0), writes=[F.b])
            ep_res = resid_epilogue(1, 0)
            for half in range(2):
                for c8 in range(8):
                    cc = half * 8 + c8
                    wts = [load_w(w_scin_d[:, o * 2048 + cc * 128:o * 2048 + (cc + 1) * 128], KC, 128) for o in (1, 2)]
                    for (t0, tn) in tiles:
                        cg = cgt[(t0 // 512) % 2]
                        for o in range(2):
                            wt_, wvv = wts[o]
                            bk = bank()

                            def mm(e, wvv=wvv, t0=t0, tn=tn, bk=bk):
                                for k in range(KC):
                                    r = e.matmul(bk[:, 0:tn], lhsT=wvv[:, k, :], rhs=HT[:, k, t0:t0 + tn],
                                                 start=(k == 0), stop=(k == KC - 1))
                                return r
                            P.add("pe", mm, reads=[wt_.b, HT.b], writes=[bk.b])
                            if o == 0:
                                P.add("act", copy_op("act", cg[:, 0:tn], bk[:, 0:tn]), reads=[bk.b], writes=[cg.b])
                            elif t0 < NPR:
                                P.add("dve", lambda e, cg=cg, bk=bk, t0=t0, tn=tn: e.tensor_tensor(
                                    out=Fp[:, 2 + t0:2 + t0 + tn], in0=cg[:, 0:tn], in1=bk[:, 0:tn], op=ALU.mult),
                                    reads=[cg.b, bk.b], writes=[F.b])
                            else:
                                P.add("dve", lambda e, cg=cg, bk=bk, tn=tn: e.tensor_tensor(
                                    out=Fsm[:, :, 2:6], in0=cg[:, 0:tn].rearrange("p (s t) -> p s t", t=4),
                                    in1=bk[:, 0:tn].rearrange("p (s t) -> p s t", t=4), op=ALU.mult),
                                    reads=[cg.b, bk.b], writes=[F.b])
                    P.add("pool", lambda e, cc=cc: e.tensor_copy(
                        out=Fsm[:, 0:NSQ, 0:2], in_=ssT[:, cc, :].rearrange("p (s r) -> p s r", r=2)),
                        reads=[ssT.b], writes=[F.b])
                    P.add("pool", lambda e: e.tensor_scalar(out=Fp[:, 0:2], in0=Fsm[:, NSQ, 4:6], scalar1=f1[:, 0:1],
                                                           scalar2=None, op0=ALU.mult), reads=[F.b, f1.b], writes=[F.b])
                    wcol = 528 + cc
                    en = "dve"
                    yvs = yv[:, NPR:NT].rearrange("p (s t) -> p s t", t=4)
                    P.add(en, lambda e, wcol=wcol: e.tensor_scalar(
                        out=yv[:, 0:NPR], in0=Fp[:, 0:NPR], scalar1=vecT[:, wcol:wcol + 1], scalar2=None, op0=ALU.mult),
                        reads=[F.b, vecT.b], writes=[yv.b])
                    P.add(en, lambda e, wcol=wcol, yvs=yvs: e.tensor_scalar(
                        out=yvs, in0=Fsm[:, :, 0:4], scalar1=vecT[:, wcol:wcol + 1], scalar2=None, op0=ALU.mult),
                        reads=[F.b, vecT.b], writes=[yv.b], join=True)
                    for tp in (1, 2):
                        wc2 = wcol + 16 * tp
                        P.add(en, lambda e, wc2=wc2, tp=tp: e.scalar_tensor_tensor(
                            out=yv[:, 0:NPR], in0=Fp[:, tp:tp + NPR], scalar=vecT[:, wc2:wc2 + 1], in1=yv[:, 0:NPR],
                            op0=ALU.mult, op1=ALU.add), reads=[F.b, vecT.b, yv.b], writes=[yv.b])
                        P.add(en, lambda e, wc2=wc2, tp=tp, yvs=yvs: e.scalar_tensor_tensor(
                            out=yvs, in0=Fsm[:, :, tp:tp + 4], scalar=vecT[:, wc2:wc2 + 1], in1=yvs,
                            op0=ALU.mult, op1=ALU.add), reads=[F.b, vecT.b, yv.b], writes=[yv.b])
                    P.add("pool", lambda e, cc=cc: e.tensor_copy(out=sso[:, cc, 0:2], in_=Fp[:, NPR:NPR + 2]),
                          reads=[F.b], writes=[sso.b], join=True)
                    P.add("pool", lambda e, cc=cc: e.tensor_copy(
                        out=sso[:, cc, 2:34].rearrange("p (s r) -> p s r", r=2), in_=Fsm[:, 0:NSQ, 4:6]),
                        reads=[F.b], writes=[sso.b], join=True)
                    wb_, wbv = load_w(w_scin_d[:, cc * 128:(cc + 1) * 128], KC, 128)
                    for (t0, tn) in tiles:
                        bk = bank()

                        def mm(e, wbv=wbv, t0=t0, tn=tn, bk=bk):
                            for k in range(KC):
                                r = e.matmul(bk[:, 0:tn], lhsT=wbv[:, k, :], rhs=HT[:, k, t0:t0 + tn],
                                             start=(k == 0), stop=(k == KC - 1))
                            return r
                        P.add("pe", mm, reads=[wb_.b, HT.b], writes=[bk.b])
                        P.add("dve", lambda e, c8=c8, bk=bk, t0=t0, tn=tn: e.tensor_tensor(
                            out=AT_[:, c8, t0:t0 + tn], in0=bk[:, 0:tn], in1=yv[:, t0:t0 + tn], op=ALU.mult),
                            reads=[bk.b, yv.b], writes=[AT_.b], join=True)
                linear(w_scout_d[half * 1024:(half + 1) * 1024, :], 0, 16, 8, at_rhs, tiles, ep_res, [AT_.b])
            for g4 in range(4):
                bk = bank()

                def tr(e, g4=g4, bk=bk):
                    for j in range(4):
                        k = g4 * 4 + j
                        r = e.transpose(bk[0:34, j * 128:(j + 1) * 128], sso[:, k, :], ident[:])
                    return r
                P.add("pe", tr, reads=[sso.b, ident.b], writes=[bk.b])
                P.add("dve", copy_op("dve", sot[:, :], bk[0:34, 0:512]), reads=[bk.b], writes=[sot.b])
                P.add("pool", lambda e, g4=g4: e.dma_start(out=sconvp_d[:, g4 * 512:(g4 + 1) * 512], in_=sot[0:2, :]),
                      reads=[sot.b], dma=True, out=True)
                P.add("pool", lambda e, g4=g4: e.dma_start(out=sconvs_d[:, g4 * 512:(g4 + 1) * 512], in_=sot[2:34, :]),
                      reads=[sot.b], dma=True, out=True)

        dump(XT, 'XT_sc', [128, KC, NT])
        gT = sb([8, NT], stack=fin, name="gT")
        gbc = sb([128, NT], stack=fin, name="gbc")
        with scope() as sc:
            wrs = sb([128, KC, NEXP], stack=sc, name="wrs")
            P.add("sp", lambda e: e.dma_start(out=wrs[:], in_=w_rt_d.rearrange("(k p) c -> p k c", p=128)),
                  writes=[wrs.b], dma=True)
            norm_mod(XT, NPR, NT - NPR, 1, 1, sc, router=wrs)
        with scope() as sc:
            lg = sb([128, 9, 8], stack=sc, name="lg")
            m1 = sb([128, 9], stack=sc, name="m1")
            m2 = sb([128, 9], stack=sc, name="m2")
            k1 = sb([128, 9, 8], stack=sc, name="k1")
            k2 = sb([128, 9, 8], stack=sc, name="k2")
            l2 = sb([128, 9, 8], stack=sc, name="l2")
            LT = NT - 1024
            P.add("pool", lambda e: e.memset(lg[:], 0.0), writes=[lg.b])
            P.add("dve", lambda e: e.tensor_tensor(
                out=lg[:, 0:8, :], in0=rbank[:, 0:64].rearrange("p (a b) -> p a b", a=8),
                in1=hvec[:, 64:72].unsqueeze(1).to_broadcast([128, 8, 8]), op=ALU.add),
                reads=[rbank.b, hvec.b, lg.b], writes=[lg.b])
            P.add("dve", lambda e: e.tensor_tensor(out=lg[0:LT, 8, :], in0=rbank[0:LT, 64:72], in1=hvec[0:LT, 64:72], op=ALU.add),
                  reads=[rbank.b, hvec.b, lg.b], writes=[lg.b])
            b98 = lambda ap: ap.unsqueeze(2).to_broadcast([128, 9, 8])
            P.add("dve", lambda e: e.tensor_reduce(out=m1[:], in_=lg[:], axis=AX.X, op=ALU.max), reads=[lg.b], writes=[m1.b])
            P.add("dve", lambda e: e.tensor_tensor(out=k1[:], in0=lg[:], in1=b98(m1[:]), op=ALU.is_equal),
                  reads=[lg.b, m1.b], writes=[k1.b])
            P.add("dve", lambda e: e.scalar_tensor_tensor(out=l2[:], in0=k1[:], scalar=-1e30, in1=lg[:], op0=ALU.mult, op1=ALU.add),
                  reads=[k1.b, lg.b], writes=[l2.b])
            P.add("dve", lambda e: e.tensor_reduce(out=m2[:], in_=l2[:], axis=AX.X, op=ALU.max), reads=[l2.b], writes=[m2.b])
            P.add("dve", lambda e: e.tensor_tensor(out=k2[:], in0=l2[:], in1=b98(m2[:]), op=ALU.is_equal),
                  reads=[l2.b, m2.b], writes=[k2.b])
            P.add("dve", lambda e: e.tensor_tensor(out=m1[:], in0=m1[:], in1=m2[:], op=ALU.subtract),
                  reads=[m1.b, m2.b], writes=[m1.b])
            P.add("act", lambda e: e.activation(out=m1[:], in_=m1[:], func=AF.Exp), reads=[m1.b], writes=[m1.b])
            P.add("dve", lambda e: e.tensor_scalar(out=m1[:], in0=m1[:], scalar1=1.0, scalar2=None, op0=ALU.add),
                  reads=[m1.b], writes=[m1.b])
            P.add("dve", lambda e: e.reciprocal(out=m2[:], in_=m1[:]), reads=[m1.b], writes=[m2.b])
            P.add("dve", lambda e: e.tensor_scalar(out=m1[:], in0=m2[:], scalar1=-1.0, scalar2=1.0, op0=ALU.mult, op1=ALU.add),
                  reads=[m2.b], writes=[m1.b])
            P.add("dve", lambda e: e.tensor_tensor(out=k1[:], in0=k1[:], in1=b98(m1[:]), op=ALU.mult),
                  reads=[k1.b, m1.b], writes=[k1.b])
            P.add("dve", lambda e: e.tensor_tensor(out=k2[:], in0=k2[:], in1=b98(m2[:]), op=ALU.mult),
                  reads=[k2.b, m2.b], writes=[k2.b])
            P.add("dve", lambda e: e.tensor_tensor(out=k1[:], in0=k1[:], in1=k2[:], op=ALU.add),
                  reads=[k1.b, k2.b], writes=[k1.b])
            for t3 in range(3):
                bk = bank()
                nt3 = 4 if t3 < 2 else 1

                def trg(e, bk=bk, t3=t3, nt3=nt3):
                    for x in range(nt3):
                        t = t3 * 4 + x
                        tn = min(128, NT - t * 128)
                        r = e.transpose(bk[0:8, x * 128:x * 128 + tn], k1[0:tn, t, :], ident[0:tn, 0:tn])
                    return r
                P.add("pe", trg, reads=[k1.b, ident.b], writes=[bk.b])
                wd = 512 if t3 < 2 else LT
                P.add("dve", copy_op("dve", gT[:, t3 * 512:t3 * 512 + wd], bk[0:8, 0:wd]), reads=[bk.b], writes=[gT.b],
                      join=(t3 > 0))
        for ex in range(NEXP):
            for (t0, tn) in tiles:
                bk = bank()
                P.add("pe", lambda e, ex=ex, t0=t0, tn=tn, bk=bk: e.matmul(
                    bk[:, 0:tn], lhsT=sel[:, ex, :], rhs=gT[:, t0:t0 + tn], start=True, stop=True),
                    reads=[sel.b, gT.b], writes=[bk.b])
                P.add("act", copy_op("act", gbc[:, t0:t0 + tn], bk[:, 0:tn]), reads=[bk.b], writes=[gbc.b], join=(t0 > 0))
            glu_mlp(w_mup_d[ex], w_mdn_d[ex], EXD, 1, gate_bc=gbc)

        dump(XT, 'XT_moe', [128, KC, NT])
        dump(gT, 'gT', [8, NT])
        with scope() as sc:
            rs = sb([128, NT], stack=sc, name="rsf")
            rstd_bc(XT, NT, sc, rs)
            for k in range(KC):
                P.add("dve", lambda e, k=k: e.scalar_tensor_tensor(
                    out=XT[:, k, :], in0=XT[:, k, :], scalar=vecT[:, 256 + k:257 + k], in1=rs[:, :], op0=ALU.mult, op1=ALU.mult),
                    reads=[XT.b, vecT.b, rs.b], writes=[XT.b])
            y_ = sb([128, D], stack=sc, name="ys")
            for t in range(9):
                r0 = t * 128
                rows = min(128, NT - r0)
                for g4 in range(4):
                    bk = bank()

                    def tr(e, g4=g4, bk=bk, r0=r0, rows=rows):
                        for j in range(4):
                            k = g4 * 4 + j
                            r = e.transpose(bk[0:rows, j * 128:(j + 1) * 128], XT[:, k, r0:r0 + rows], ident[:])
                        return r
                    P.add("pe", tr, reads=[XT.b, ident.b], writes=[bk.b])
                    en = ev_eng()
                    P.add(en, copy_op(en, y_[0:rows, g4 * 512:(g4 + 1) * 512], bk[0:rows, 0:512]), reads=[bk.b], writes=[y_.b],
                          join=(g4 > 0))
                P.add("pool", lambda e, r0=r0, rows=rows: e.dma_start(out=yo_d[r0:r0 + rows, :], in_=y_[0:rows, :]),
                      reads=[y_.b], dma=True, out=True)

    except _Stop:
        pass
    sems = contextlib.ExitStack()
    P.emit(sems)
    sems.close()
    fin.close()
    top.close()
    return nc, P


_CACHE = {}


def _consts():
    ident = np.eye(128, dtype=np.float32)
    masks = np.zeros((64, 4, 64), np.float32)
    t = np.arange(64)[:, None]
    i = np.arange(64)[None, :]
    masks[:, 0] = (t <= i)
    masks[:, 1] = (t > i)
    masks[:, 2] = (t >= i)
    masks[:, 3] = (t > i)
    sel = np.zeros((8, 8, 128), np.float32)
    for e in range(8):
        sel[e, e, :] = 1.0
    return ident, masks, sel


def kernel(x_prompt, x_sample, c_prompt, c_sample, state_gdn, state_gdn_conv, state_sconv,
           w_ada, b_ada, g_norm_mix, g_norm_ffn, g_norm_out, gdn_w_in, gdn_conv_w,
           gdn_a_log, gdn_dt_bias, gdn_g_onorm, gdn_w_out, sc_w_in, sc_conv_w, sc_w_out,
           ffn_w_up, ffn_w_down, moe_w_router, moe_b_router, moe_w_up, moe_w_down):
    f = lambda a: np.ascontiguousarray(np.asarray(a, dtype=np.float32))
    x_prompt, x_sample, c_prompt, c_sample = f(x_prompt), f(x_sample), f(c_prompt), f(c_sample)
    state_gdn, state_gdn_conv, state_sconv = f(state_gdn), f(state_gdn_conv), f(state_sconv)
    ident, masks, sel = _consts()
    vecs = np.zeros((640, 128), np.float32)
    vecs[0:192] = f(b_ada).reshape(192, 128)
    vecs[192:224] = f(g_norm_mix).reshape(32, 128)
    vecs[224:256] = f(g_norm_ffn).reshape(32, 128)
    vecs[256:272] = f(g_norm_out).reshape(16, 128)
    vecs[272:528] = f(gdn_conv_w).reshape(256, 128)
    vecs[528:576] = f(sc_conv_w).reshape(48, 128)
    vecs[576] = f(gdn_g_onorm).reshape(128)
    hvec = np.concatenate([f(gdn_a_log).reshape(-1), f(gdn_dt_bias).reshape(-1), f(moe_b_router).reshape(-1)])[None, :]
    shared = {
        "vecs": vecs, "hvec": np.ascontiguousarray(hvec), "ident": ident, "masks": masks, "sel": sel,
        "w_ada": f(w_ada), "gdn_w_in": f(gdn_w_in)[0], "gdn_w_out": f(gdn_w_out)[0], "sc_w_in": f(sc_w_in)[0],
        "sc_w_out": f(sc_w_out)[0], "ffn_w_up": f(ffn_w_up)[0], "ffn_w_down": f(ffn_w_down)[0],
        "moe_w_router": f(moe_w_router)[0], "moe_w_up": f(moe_w_up)[0], "moe_w_down": f(moe_w_down)[0],
    }
    in_maps = []
    for c in range(8):
        s, m = c // 2, c % 2
        xs_ = x_sample[16 * c:16 * c + 16].reshape(64, D)
        xo = np.concatenate([x_prompt[s, m * 1024:(m + 1) * 1024], xs_, x_prompt[s, 1020:1024]], axis=0)
        xp = x_prompt[s, 0:1024]
        cvec = np.concatenate([c_prompt[s:s + 1], c_sample[16 * c:16 * c + 16], c_prompt[s:s + 1]], axis=0)
        d = dict(shared)
        d.update({
            "xo": np.ascontiguousarray(xo), "xp": np.ascontiguousarray(xp), "cvec": np.ascontiguousarray(cvec),
            "sgdn": np.ascontiguousarray(state_gdn[0, 16 * c:16 * c + 16]),
            "sgconv": np.ascontiguousarray(state_gdn_conv[0, 16 * c:16 * c + 16].reshape(48, 8192)),
            "ssconv": np.ascontiguousarray(state_sconv[0, 16 * c:16 * c + 16].reshape(32, D)),
            "f1": np.full((128, 1), float(m), np.float32),
        })
        in_maps.append(d)
    if _CACHE.get("dbg_hook") is not None:
        return _CACHE["dbg_hook"](in_maps)
    if "nc" not in _CACHE:
        _CACHE["nc"] = build_program()[0]
    nc = _CACHE["nc"]
    res = run_bass_kernel_spmd(nc, in_maps, core_ids=list(range(8)))
    R = res.results
    y_prompt = np.zeros((4, 2048, D), np.float32)
    y_sample = np.zeros((128, 4, D), np.float32)
    gdn_p = np.zeros((1, 4, 32, 128, 128), np.float32)
    gconv_p = np.zeros((1, 4, 3, 8192), np.float32)
    sconv_p = np.zeros((1, 4, 2, D), np.float32)
    gdn_s = np.zeros((1, 128, 32, 128, 128), np.float32)
    gconv_s = np.zeros((1, 128, 3, 8192), np.float32)
    sconv_s = np.zeros((1, 128, 2, D), np.float32)
    for c in range(8):
        s, m = c // 2, c % 2
        r = R[c]
        y_prompt[s, m * 1024:(m + 1) * 1024] = r["yo"][0:1024]
        y_sample[16 * c:16 * c + 16] = r["yo"][1024:1088].reshape(16, 4, D)
        if m == 1:
            gdn_p[0, s] = r["gdn_p"]
            gconv_p[0, s] = r["gconv_p"]
            sconv_p[0, s] = r["sconv_p"]
        gdn_s[0, 16 * c:16 * c + 16] = r["gdn_s"]
        gconv_s[0, 16 * c:16 * c + 16] = r["gconv_s"].reshape(16, 3, 8192)
        sconv_s[0, 16 * c:16 * c + 16] = r["sconv_s"].reshape(16, 2, D)
    return (y_prompt, y_sample, gdn_p, gconv_p, sconv_p, gdn_s, gconv_s, sconv_s)
```

```python
import contextlib
import numpy as np
import concourse.bass as bass
import concourse.mybir as mybir
from concourse.bass_utils import run_bass_kernel_spmd

F32 = mybir.dt.float32
BF16 = mybir.dt.bfloat16
AF = mybir.ActivationFunctionType
ALU = mybir.AluOpType
AX = mybir.AxisListType

SEM_ROT = 12000
D = 2048
KC = 16
NPR = 1024
NSQ = 16
NSM = 64
NHALO = 4
NT = NPR + NSM + NHALO
NMOD = 18
GIN = 12352
FFN = 5632
EXD = 7168
NEXP = 8
EPS = 1e-6


class Buf:
    __slots__ = ("name", "w", "r", "war")

    def __init__(self, name):
        self.name = name
        self.w = None
        self.r = []
        self.war = set()


class Prog:
    ENGS = ("pe", "act", "dve", "pool", "sp")

    def __init__(self, nc):
        self.nc = nc
        self.ins = []
        self.nb = 0
        self.out_dmas = []
        self.last = {}
        self.fence_deps = {}

    def buf(self, name=None):
        self.nb += 1
        return Buf(f"{name or 'b'}{self.nb}")

    def fence(self):
        allast = set(self.last.values())
        for e in self.ENGS:
            self.fence_deps[e] = set(allast)

    def add(self, eng, fn, reads=(), writes=(), dma=False, join=False, out=False):
        i = len(self.ins)
        deps = set()
        for b in reads:
            if b.w:
                deps.update(b.w.values())
        for b in writes:
            if b.w and not join:
                deps.update(b.w.values())
            for r in b.r:
                deps.add(r)
            if join and b.w:
                deps.update(b.war)
        for b in reads:
            b.r.append(i)
        ek = (eng, dma)
        for b in writes:
            if join and b.w:
                b.w[ek] = i
                b.war = b.war | set(b.r)
            else:
                b.w = {ek: i}
                b.war = set(b.r)
            b.r = []
        if eng in self.fence_deps:
            deps |= self.fence_deps.pop(eng)
        deps.discard(i)
        dsem = None
        if dma:
            dsem = writes[0].name if writes else reads[0].name
        self.ins.append(dict(eng=eng, fn=fn, deps=deps, dma=dma, dsem=dsem))
        self.last[eng] = i
        if out:
            self.out_dmas.append(i)
        return i

    def emit(self, stack):
        nc = self.nc
        ins = self.ins
        n = len(ins)
        needed = [False] * n
        for it in ins:
            for d in it["deps"]:
                if ins[d]["eng"] == "pe" and it["eng"] == "pe" and not ins[d]["dma"] and not it["dma"]:
                    continue
                needed[d] = True
        for i, it in enumerate(ins):
            if it["dma"]:
                needed[i] = True
        cnt = [0]

        def newsem(tag):
            cnt[0] += 1
            return stack.enter_context(nc.semaphore(f"s{tag}{cnt[0]}"))

        eng_sem, eng_cnt, dma_sems = {}, {}, {}
        tok = [None] * n
        for i, it in enumerate(ins):
            if not needed[i]:
                continue
            if it["dma"]:
                key = it["dsem"]
                if key not in dma_sems:
                    dma_sems[key] = [newsem("d"), 0]
                ds = dma_sems[key]
                ds[1] += 16
                tok[i] = (ds[0], ds[1], id(ds[0]))
                if ds[1] >= SEM_ROT * 16:
                    del dma_sems[key]
            else:
                e = it["eng"]
                if e not in eng_sem or eng_cnt[e] >= SEM_ROT:
                    eng_sem[e] = newsem(e)
                    eng_cnt[e] = 0
                eng_cnt[e] += 1
                tok[i] = (eng_sem[e], eng_cnt[e], id(eng_sem[e]))
        self.nsem = cnt[0]
        per = {e: [] for e in self.ENGS}
        for i, it in enumerate(ins):
            per[it["eng"]].append(i)
        final_waits = [tok[i] for i in self.out_dmas]

        def run_engine(ename, eh):
            known = {}
            for i in per[ename]:
                it = ins[i]
                waits = {}
                for d in it["deps"]:
                    if tok[d] is None:
                        continue
                    if ename == "pe" and ins[d]["eng"] == "pe" and not ins[d]["dma"] and not it["dma"]:
                        continue
                    s, v, k = tok[d]
                    if k not in waits or waits[k][1] < v:
                        waits[k] = (s, v)
                for k, (s, v) in waits.items():
                    if known.get(k, 0) < v:
                        eh.wait_ge(s, v)
                        known[k] = v
                r = it["fn"](eh)
                if tok[i] is not None:
                    s, v, k = tok[i]
                    r.then_inc(s, 16 if it["dma"] else 1)
            if ename == "sp":
                fw = {}
                for s, v, k in final_waits:
                    if k not in fw or fw[k][1] < v:
                        fw[k] = (s, v)
                for k, (s, v) in fw.items():
                    eh.wait_ge(s, v)

        with nc.Block() as block:
            @block.sync
            def _(e):
                run_engine("sp", e)

            @block.tensor
            def _(e):
                run_engine("pe", e)

            @block.scalar
            def _(e):
                run_engine("act", e)

            @block.vector
            def _(e):
                run_engine("dve", e)

            @block.gpsimd
            def _(e):
                run_engine("pool", e)


class TT:
    def __init__(self, t, b):
        self.t = t
        self.b = b

    def __getitem__(self, k):
        return self.t[k]


class _Stop(Exception):
    pass


def build_program(dbg=None):
    nc = bass.Bass("TRN2", target_bir_lowering=False)
    P = Prog(nc)
    dbg = dbg or {}

    def dump(tt, name, shape, dt=F32):
        if name not in dbg.get('dumps', ()):
            return
        dd = nc.dram_tensor('dbg_' + name, list(shape), dt, kind='ExternalOutput').ap()
        P.add('sp', lambda e: e.dma_start(out=dd, in_=tt[:]), reads=[tt.b], dma=True, out=True)

    @contextlib.contextmanager
    def scope():
        with contextlib.ExitStack() as sc_:
            yield sc_
        P.fence()

    def stop_at(tag):
        if dbg.get('stop') == tag:
            raise _Stop()

    def din(name, shape, dt=F32):
        return nc.dram_tensor(name, list(shape), dt, kind="ExternalInput").ap()

    def dout(name, shape, dt=F32):
        return nc.dram_tensor(name, list(shape), dt, kind="ExternalOutput").ap()

    xo_d = din("xo", [NT, D])
    xp_d = din("xp", [NPR, D])
    cvec_d = din("cvec", [NMOD, D])
    sgdn_d = din("sgdn", [NSQ, 32, 128, 128])
    sgconv_d = din("sgconv", [NSQ * 3, 8192])
    ssconv_d = din("ssconv", [NSQ * 2, D])
    f1_d = din("f1", [128, 1])
    vecs_d = din("vecs", [640, 128])
    hvec_d = din("hvec", [1, 72])
    ident_d = din("ident", [128, 128])
    masks_d = din("masks", [64, 4, 64])
    sel_d = din("sel", [8, 8, 128])
    w_ada_d = din("w_ada", [2, D, 6 * D])
    w_gin_d = din("gdn_w_in", [D, GIN])
    w_gout_d = din("gdn_w_out", [4096, D])
    w_scin_d = din("sc_w_in", [D, 3 * D])
    w_scout_d = din("sc_w_out", [D, D])
    w_fup_d = din("ffn_w_up", [D, 2 * FFN])
    w_fdn_d = din("ffn_w_down", [FFN, D])
    w_rt_d = din("moe_w_router", [D, NEXP])
    w_mup_d = din("moe_w_up", [NEXP, D, 2 * EXD])
    w_mdn_d = din("moe_w_down", [NEXP, EXD, D])

    yo_d = dout("yo", [NT, D])
    gdnp_d = dout("gdn_p", [32, 128, 128])
    gconvp_d = dout("gconv_p", [3, 8192])
    sconvp_d = dout("sconv_p", [2, D])
    gdns_d = dout("gdn_s", [NSQ, 32, 128, 128])
    gconvs_d = dout("gconv_s", [NSQ * 3, 8192])
    sconvs_d = dout("sconv_s", [NSQ * 2, D])

    sscr_d = nc.dram_tensor("sscr", [32, 128, 128], F32).ap()
    oscr_d = nc.dram_tensor("oscr", [32, 128, NT], BF16).ap()
    bscr = [P.buf("sscr") for _ in range(32)]
    boscr = [P.buf("oscr") for _ in range(32)]

    top = contextlib.ExitStack()
    uid = [0]

    def sb(shape, dt=F32, stack=None, name=None):
        uid[0] += 1
        nm = f"{name or 't'}{uid[0]}"
        t = (stack or top).enter_context(nc.sbuf_tensor(nm, list(shape), dt))
        return TT(t, P.buf(nm))

    banks = []
    for i in range(8):
        t = top.enter_context(nc.psum_tensor(f"psb{i}", [128, 512], F32))
        banks.append(TT(t, P.buf(f"psb{i}")))
    brr = [0, 0]

    def bank():
        b = banks[brr[0] % 5]
        brr[0] += 1
        return b

    def obank():
        b = banks[5 + brr[1] % 2]
        brr[1] += 1
        return b
    rbank = banks[7]

    rr = {"ev": 0, "cast": 0}

    def ev_eng():
        rr["ev"] += 1
        return "act" if rr["ev"] % 2 else "dve"

    def copy_op(eng, out, in_):
        if eng == "act":
            return lambda e: e.activation(out=out, in_=in_, func=AF.Copy)
        return lambda e: e.tensor_copy(out=out, in_=in_)

    ident = sb([128, 128], name="ident")
    P.add("sp", lambda e: e.dma_start(out=ident[:], in_=ident_d), writes=[ident.b], dma=True)
    masks = sb([64, 4, 64], name="masks")
    P.add("sp", lambda e: e.dma_start(out=masks[:], in_=masks_d), writes=[masks.b], dma=True)
    sel = sb([8, 8, 128], name="sel")
    P.add("sp", lambda e: e.dma_start(out=sel[:], in_=sel_d), writes=[sel.b], dma=True)
    f1 = sb([128, 1], name="f1")
    P.add("sp", lambda e: e.dma_start(out=f1[:], in_=f1_d), writes=[f1.b], dma=True)
    hvec = sb([128, 72], name="hvec")
    P.add("sp", lambda e: e.dma_start(out=hvec[:], in_=hvec_d.partition_broadcast(128)), writes=[hvec.b], dma=True)
    ones_f = sb([128, 128], name="ones_f")
    P.add("pool", lambda e: e.memset(ones_f[:], 1.0), writes=[ones_f.b])
    ones_b = sb([128, 128], BF16, name="ones_b")
    P.add("pool", lambda e: e.memset(ones_b[:], 1.0), writes=[ones_b.b])
    epst = sb([128, 1], name="eps")
    P.add("pool", lambda e: e.memset(epst[:], EPS), writes=[epst.b])

    vecT = sb([128, 640], name="vecT")
    mod = sb([128, 2, 96, NMOD], name="mod")
    gmod = sb([128, 2, 2, 16, NMOD], name="gmod")
    cT = sb([128, KC, NMOD], BF16, name="cT")
    HT = sb([128, KC, NT], BF16, name="HT")
    nexpa = sb([128, 32], name="nexpa")
    ba_w = sb([128, KC, 64], BF16, name="ba_w")
    tails = sb([128, 4, 16, 3], name="tails")

    NSTG = 2
    stg = [sb([128, 2048], name="stg") for _ in range(NSTG)]
    wbp = [sb([128, 2048], BF16, name="wb") for _ in range(NSTG)]
    wrr = [0]

    def stage_load(src_ap, kc, cw):
        i = wrr[0] % NSTG
        wrr[0] += 1
        s = stg[i]
        sv = s[:, 0:kc * cw].rearrange("p (k c) -> p k c", k=kc)
        P.add("sp", lambda e: e.dma_start(out=sv, in_=src_ap.rearrange("(k p) c -> p k c", p=128)),
              writes=[s.b], dma=True)
        return i, s, sv

    def cast_eng():
        rr["cast"] += 1
        return ("act", "dve", "pool")[rr["cast"] % 3]

    def load_w(src_ap, kc, cw):
        i, s, sv = stage_load(src_ap, kc, cw)
        w = wbp[i]
        wv = w[:, 0:kc * cw].rearrange("p (k c) -> p k c", k=kc)
        ce = cast_eng()
        P.add(ce, copy_op(ce, w[:, 0:kc * cw], s[:, 0:kc * cw]), reads=[s.b], writes=[w.b])
        return w, wv

    fin = contextlib.ExitStack()
    try:
        with scope() as sc:
            vs = sb([128, 5, 128], stack=sc, name="vs")
            P.add("sp", lambda e: e.dma_start(out=vs[:], in_=vecs_d.rearrange("(t p) c -> p t c", p=128)),
                  writes=[vs.b], dma=True)
            for half in range(2):
                bk = bank()
                nt_ = 4 if half == 0 else 1

                def tr(e, half=half, bk=bk, nt_=nt_):
                    for j in range(nt_):
                        r = e.transpose(bk[:, j * 128:(j + 1) * 128], vs[:, half * 4 + j, :], ident[:])
                    return r
                P.add("pe", tr, reads=[vs.b, ident.b], writes=[bk.b])
                P.add("dve", copy_op("dve", vecT[:, half * 512:half * 512 + nt_ * 128], bk[:, 0:nt_ * 128]),
                      reads=[bk.b], writes=[vecT.b], join=(half > 0))
            cs = sb([NMOD, D], stack=sc, name="cs")
            P.add("sp", lambda e: e.dma_start(out=cs[:], in_=cvec_d), writes=[cs.b], dma=True)
            P.add("act", lambda e: e.activation(out=cs[:], in_=cs[:], func=AF.Silu), reads=[cs.b], writes=[cs.b])
            for g4 in range(4):
                bk = bank()

                def tr(e, g4=g4, bk=bk):
                    for j in range(4):
                        k = g4 * 4 + j
                        r = e.transpose(bk[:, j * 32:j * 32 + NMOD], cs[:, k * 128:(k + 1) * 128], ident[0:NMOD, 0:NMOD])
                    return r
                P.add("pe", tr, reads=[cs.b, ident.b], writes=[bk.b])
                P.add("dve", copy_op("dve", cT[:, g4 * 4:(g4 + 1) * 4, :],
                                     bk[:, 0:128].rearrange("p (a b) -> p a b", a=4)[:, :, 0:NMOD]),
                      reads=[bk.b], writes=[cT.b], join=(g4 > 0))
            P.add("act", lambda e: e.activation(out=nexpa[:], in_=hvec[:, 0:32], func=AF.Exp),
                  reads=[hvec.b], writes=[nexpa.b])
            P.add("dve", lambda e: e.tensor_scalar(out=nexpa[:], in0=nexpa[:], scalar1=-1.0, scalar2=None, op0=ALU.mult),
                  reads=[nexpa.b], writes=[nexpa.b])
            _, s_, sv_ = stage_load(w_gin_d[:, 12288:12352], KC, 64)
            P.add("dve", copy_op("dve", ba_w[:], sv_), reads=[s_.b], writes=[ba_w.b])
            for l in range(2):
                for cc in range(96):
                    w, wv = load_w(w_ada_d[l, :, cc * 128:(cc + 1) * 128], KC, 128)
                    bk = bank()

                    def mm(e, wv=wv, bk=bk):
                        for k in range(KC):
                            r = e.matmul(bk[:, 0:NMOD], lhsT=wv[:, k, :], rhs=cT[:, k, :], start=(k == 0), stop=(k == KC - 1))
                        return r
                    P.add("pe", mm, reads=[w.b, cT.b], writes=[bk.b])
                    P.add("dve", lambda e, bk=bk, l=l, cc=cc: e.tensor_scalar(
                        out=mod[:, l, cc, :], in0=bk[:, 0:NMOD],
                        scalar1=vecT[:, l * 96 + cc:l * 96 + cc + 1], scalar2=None, op0=ALU.add),
                        reads=[bk.b, vecT.b], writes=[mod.b], join=True)
            for l in range(2):
                for s in range(2):
                    gcol = 192 + s * 32 + l * 16
                    for k in range(KC):
                        P.add("dve", lambda e, l=l, s=s, k=k, gcol=gcol: e.tensor_scalar(
                            out=gmod[:, l, s, k, :], in0=mod[:, l, (1 + 3 * s) * 16 + k, :],
                            scalar1=1.0, scalar2=vecT[:, gcol + k:gcol + k + 1], op0=ALU.add, op1=ALU.mult),
                            reads=[mod.b, vecT.b], writes=[gmod.b], join=True)
        P.fence()
        dump(vecT, 'vecT', [128, 640])
        dump(mod, 'mod', [128, 2, 96, NMOD])
        dump(gmod, 'gmod', [128, 2, 2, 16, NMOD])
        dump(cT, 'cT', [128, KC, NMOD], BF16)
        stop_at('setup')

        def load_xT(XT, src_d, ntok, stack):
            xs = [sb([128, D], stack=stack, name="xs") for _ in range(2)]
            nt_ = (ntok + 127) // 128
            for t in range(nt_):
                r0 = t * 128
                rows = min(128, ntok - r0)
                s = xs[t % 2]
                P.add("sp", lambda e, s=s, r0=r0, rows=rows: e.dma_start(out=s[0:rows, :], in_=src_d[r0:r0 + rows, :]),
                      writes=[s.b], dma=True)
                for g4 in range(4):
                    bk = bank()

                    def tr(e, s=s, bk=bk, g4=g4, rows=rows):
                        for j in range(4):
                            k = g4 * 4 + j
                            r = e.transpose(bk[:, j * 128:j * 128 + rows], s[0:rows, k * 128:(k + 1) * 128],
                                            ident[0:rows, 0:rows])
                        return r
                    P.add("pe", tr, reads=[s.b, ident.b], writes=[bk.b])
                    en = ev_eng()
                    P.add(en, copy_op(en, XT[:, g4 * 4:(g4 + 1) * 4, r0:r0 + rows],
                                      bk[:, 0:512].rearrange("p (a b) -> p a b", a=4)[:, :, 0:rows]),
                          reads=[bk.b], writes=[XT.b], join=True)

        def tok_tiles(n):
            res = []
            t0 = 0
            while t0 < n:
                res.append((t0, min(512, n - t0)))
                t0 += 512
            return res

        def rstd_bc(XT, ntok, stack, out_rs):
            sq = [sb([128, 512], BF16, stack=stack, name="sq") for _ in range(2)]
            for (t0, tn) in tok_tiles(ntok):
                bk = bank()
                for k in range(KC):
                    s = sq[k % 2]
                    if k % 2 == 0:
                        P.add("act", lambda e, s=s, k=k, t0=t0, tn=tn: e.activation(
                            out=s[:, 0:tn], in_=XT[:, k, t0:t0 + tn], func=AF.Square), reads=[XT.b], writes=[s.b])
                    else:
                        P.add("pool", lambda e, s=s, k=k, t0=t0, tn=tn: e.tensor_tensor(
                            out=s[:, 0:tn], in0=XT[:, k, t0:t0 + tn], in1=XT[:, k, t0:t0 + tn], op=ALU.mult),
                            reads=[XT.b], writes=[s.b])
                    P.add("pe", lambda e, s=s, k=k, bk=bk, tn=tn: e.matmul(
                        bk[:, 0:tn], lhsT=ones_b[:], rhs=s[:, 0:tn], start=(k == 0), stop=(k == KC - 1)),
                        reads=[s.b, ones_b.b], writes=[bk.b], join=(k > 0))
                P.add("act", lambda e, bk=bk, t0=t0, tn=tn: e.activation(
                    out=out_rs[:, t0:t0 + tn], in_=bk[:, 0:tn], func=AF.Sqrt, scale=1.0 / D, bias=epst[:, 0:1]),
                    reads=[bk.b, epst.b], writes=[out_rs.b], join=True)
            P.add("dve", lambda e: e.reciprocal(out=out_rs[:, 0:ntok], in_=out_rs[:, 0:ntok]),
                  reads=[out_rs.b], writes=[out_rs.b])

        def norm_mod(XT, npr, nsm, l, s, stack, router=None):
            ntok = npr + nsm
            rs = sb([128, NT], stack=stack, name="rs")
            rstd_bc(XT, ntok, stack, rs)
            tmp = [sb([128, NT], stack=stack, name="ntmp") for _ in range(2)]
            shb = (0 + 3 * s) * 16
            ns = nsm // 4
            for k in range(KC):
                tm = tmp[k % 2]
                P.add("dve", lambda e, tm=tm, k=k: e.tensor_tensor(
                    out=tm[:, 0:ntok], in0=XT[:, k, 0:ntok], in1=rs[:, 0:ntok], op=ALU.mult),
                    reads=[XT.b, rs.b], writes=[tm.b])
                P.add("pool", lambda e, tm=tm, k=k: e.tensor_scalar(
                    out=tm[:, 0:npr], in0=tm[:, 0:npr],
                    scalar1=gmod[:, l, s, k, 0:1], scalar2=mod[:, l, shb + k, 0:1], op0=ALU.mult, op1=ALU.add),
                    reads=[tm.b, gmod.b, mod.b], writes=[tm.b])
                if nsm:
                    P.add("dve", lambda e, tm=tm, k=k: e.tensor_tensor(
                        out=tm[:, npr:ntok].rearrange("p (s t) -> p s t", t=4),
                        in0=tm[:, npr:ntok].rearrange("p (s t) -> p s t", t=4),
                        in1=gmod[:, l, s, k, 1:1 + ns].unsqueeze(2).to_broadcast([128, ns, 4]), op=ALU.mult),
                        reads=[tm.b, gmod.b], writes=[tm.b])
                    P.add("dve", lambda e, tm=tm, k=k: e.tensor_tensor(
                        out=tm[:, npr:ntok].rearrange("p (s t) -> p s t", t=4),
                        in0=tm[:, npr:ntok].rearrange("p (s t) -> p s t", t=4),
                        in1=mod[:, l, shb + k, 1:1 + ns].unsqueeze(2).to_broadcast([128, ns, 4]), op=ALU.add),
                        reads=[tm.b, mod.b], writes=[tm.b])
                P.add("act", lambda e, tm=tm, k=k: e.activation(out=HT[:, k, 0:ntok], in_=tm[:, 0:ntok], func=AF.Copy),
                      reads=[tm.b], writes=[HT.b], join=True)
                if router is not None:
                    wrt = router

                    def rmm(e, tm=tm, k=k):
                        for t in range(9):
                            tn = min(128, ntok - t * 128)
                            r = e.matmul(rbank[0:tn, t * 8:(t + 1) * 8], lhsT=tm[:, t * 128:t * 128 + tn],
                                         rhs=wrt[:, k, :], start=(k == 0 and t == 0), stop=(k == KC - 1),
                                         skip_group_check=True)
                        return r
                    P.add("pe", rmm, reads=[tm.b, wrt.b], writes=[rbank.b], join=(k > 0))

        def linear(w_d, col0, ncc, kc, rhs_fn, tiles, epilogue, rhs_bufs):
            cw = 256 if kc * 256 <= 2048 else 128
            per = cw // 128
            cc = 0
            while cc < ncc:
                np_ = min(per, ncc - cc)
                w, wv = load_w(w_d[:, col0 + cc * 128: col0 + (cc + np_) * 128], kc, np_ * 128)
                for j in range(np_):
                    for (t0, tn) in tiles:
                        bk = bank()

                        def mm(e, wv=wv, j=j, t0=t0, tn=tn, bk=bk):
                            for k in range(kc):
                                r = e.matmul(bk[:, 0:tn], lhsT=wv[:, k, j * 128:(j + 1) * 128], rhs=rhs_fn(k, t0, tn),
                                             start=(k == 0), stop=(k == kc - 1))
                            return r
                        P.add("pe", mm, reads=[w.b] + rhs_bufs, writes=[bk.b])
                        epilogue(cc + j, t0, tn, bk)
                cc += np_

        def gdn_pass(stack, blocks, first, last):
            wgrp = sb([128, 6, KC, 128], BF16, stack=stack, name="wgrp")
            totch = sum(b["nch"] for b in blocks)
            BA = sb([64, 8, 64], stack=stack, name="BA")
            gtmp_ = sb([64, 16, 32], stack=stack, name="gtmp")
            pre = {nm: sb([64, totch, 32], stack=stack, name=nm) for nm in ("beta", "g", "esuf", "nbeg", "nbeta")}
            pc = 0
            for blk in blocks:
                C, nch = blk["C"], blk["nch"]
                blk["pc0"] = pc
                for c8 in range(0, nch, 8):
                    bk = bank()

                    def mm(e, C=C, c8=c8, bk=bk, blk=blk):
                        for c in range(8):
                            t0 = blk["tok0"] + (c8 + c) * C
                            for k in range(KC):
                                r = e.matmul(bk[0:C, c * 64:(c + 1) * 64], lhsT=HT[:, k, t0:t0 + C], rhs=ba_w[:, k, :],
                                             start=(k == 0), stop=(k == KC - 1))
                        return r
                    P.add("pe", mm, reads=[HT.b, ba_w.b], writes=[bk.b])
                    P.add("dve", copy_op("dve", BA[0:C, :, :], bk[0:C, 0:512].rearrange("p (a b) -> p a b", a=8)),
                          reads=[bk.b], writes=[BA.b])
                    sl = slice(pc + c8, pc + c8 + 8)
                    be, g_, es, nbg, nbe = (pre[k_][0:C, sl, :] for k_ in ("beta", "g", "esuf", "nbeg", "nbeta"))
                    P.add("act", lambda e, be=be, C=C: e.activation(out=be, in_=BA[0:C, :, 0:32], func=AF.Sigmoid),
                          reads=[BA.b], writes=[pre["beta"].b], join=True)
                    P.add("dve", lambda e, g_=g_, C=C: e.tensor_tensor(
                        out=g_, in0=BA[0:C, :, 32:64], in1=hvec[0:C, 32:64].unsqueeze(1).to_broadcast([C, 8, 32]),
                        op=ALU.add), reads=[BA.b, hvec.b], writes=[pre["g"].b], join=True)
                    P.add("act", lambda e, g_=g_: e.activation(out=g_, in_=g_, func=AF.Exp),
                          reads=[pre["g"].b], writes=[pre["g"].b])
                    P.add("act", lambda e, g_=g_: e.activation(out=g_, in_=g_, func=AF.Ln, bias=1.0),
                          reads=[pre["g"].b], writes=[pre["g"].b])
                    P.add("dve", lambda e, g_=g_, C=C: e.tensor_tensor(
                        out=g_, in0=g_, in1=nexpa[0:C, :].unsqueeze(1).to_broadcast([C, 8, 32]), op=ALU.mult),
                        reads=[pre["g"].b, nexpa.b], writes=[pre["g"].b])
                    P.add("dve", lambda e, nbe=nbe, be=be: e.tensor_scalar(out=nbe, in0=be, scalar1=-1.0, scalar2=None, op0=ALU.mult),
                          reads=[pre["beta"].b], writes=[pre["nbeta"].b], join=True)
                    gflat = pre["g"][0:C, :, :].rearrange("p a b -> p (a b)")[:, (pc + c8) * 32:(pc + c8 + 8) * 32]
                    bk1 = bank()
                    P.add("pe", lambda e, bk1=bk1, C=C, gflat=gflat: e.matmul(
                        bk1[0:C, 0:256], lhsT=masks[0:C, 0, 0:C], rhs=gflat, start=True, stop=True),
                        reads=[masks.b, pre["g"].b], writes=[bk1.b])
                    P.add("act", lambda e, bk1=bk1, C=C: e.activation(
                        out=gtmp_[0:C, 0:8, :].rearrange("p a b -> p (a b)"), in_=bk1[0:C, 0:256], func=AF.Exp),
                        reads=[bk1.b], writes=[gtmp_.b])
                    P.add("dve", lambda e, nbg=nbg, nbe=nbe, C=C: e.tensor_tensor(out=nbg, in0=nbe, in1=gtmp_[0:C, 0:8, :], op=ALU.mult),
                          reads=[pre["nbeta"].b, gtmp_.b], writes=[pre["nbeg"].b], join=True)
                    bk2 = bank()
                    P.add("pe", lambda e, bk2=bk2, C=C, gflat=gflat: e.matmul(
                        bk2[0:C, 0:256], lhsT=masks[0:C, 1, 0:C], rhs=gflat, start=True, stop=True),
                        reads=[masks.b, pre["g"].b], writes=[bk2.b])
                    P.add("act", lambda e, bk2=bk2, C=C, es=es: e.activation(
                        out=es, in_=bk2[0:C, 0:256].rearrange("p (a b) -> p a b", a=8), func=AF.Exp),
                        reads=[bk2.b], writes=[pre["esuf"].b], join=True)
                pc += nch

            QKVZ = sb([128, 6, 512], stack=stack, name="QKVZ")
            Fb = sb([128, 515], stack=stack, name="F")
            cacc = sb([128, 512], stack=stack, name="cacc")
            sqt = sb([128, 512], stack=stack, name="sqt")
            S = [sb([128, 128], stack=stack, name="S") for _ in range(4)]
            og = sb([128, 2, 512], BF16, stack=stack, name="og")
            sgc = sb([48, 512], stack=stack, name="sgc")
            sgo = sb([48, 512], stack=stack, name="sgo")
            tl3 = sb([128, 4, 48], stack=stack, name="tl3")
            WM = 512

            def t2(nm):
                return sb([64, WM], stack=stack, name=nm)
            d = dict(rgt=t2("rgt"), rle=t2("rle"), draw=t2("draw"), dtril=t2("dtril"), dstr=t2("dstr"),
                     p0=t2("p0"), pt0=t2("pt0"), rt=t2("rt"), a=t2("a"),
                     egcb=sb([128, WM], stack=stack, name="egcb"), qd=sb([128, WM], stack=stack, name="qd"),
                     wt=sb([128, WM], stack=stack, name="wt"),
                     vtok=sb([64, 1024], stack=stack, name="vtok"), ktok=sb([64, 512], stack=stack, name="ktok"),
                     kd=sb([64, 1024], stack=stack, name="kd"), otok=sb([64, 1024], stack=stack, name="otok"),
                     ors=sb([64, 8], stack=stack, name="ors"),
                     vn=[sb([64, 128], stack=stack, name="vn") for _ in range(4)])
            d["tb"] = d["rgt"]
            d["tg"] = d["rle"]
            d["at"] = d["draw"]
            d["p1"] = d["dstr"]
            d["pt1"] = d["dtril"]
            d["osq"] = d["kd"]
            nprompt = len([b for b in blocks if not b["sample"]])

            for g in range(16):
                cols = [g * 128, 2048 + g * 128, 4096 + 2 * g * 128, 4096 + (2 * g + 1) * 128,
                        8192 + 2 * g * 128, 8192 + (2 * g + 1) * 128]
                anyqz = any(b["qz"] for b in blocks)
                for j in range(6):
                    if j in (0, 4, 5) and not anyqz:
                        continue
                    _, s_, sv_ = stage_load(w_gin_d[:, cols[j]:cols[j] + 128], KC, 128)
                    ce = cast_eng()
                    P.add(ce, copy_op(ce, wgrp[:, j, :, :], sv_), reads=[s_.b], writes=[wgrp.b], join=True)

                for bi, blk in enumerate(blocks):
                    C, nch, nseq, T = blk["C"], blk["nch"], blk["nseq"], blk["T"]
                    NB = nseq * T
                    tok0 = blk["tok0"]
                    sample = blk["sample"]
                    pc0 = blk["pc0"]
                    if sample:
                        for j in range(4):
                            P.add("sp", lambda e, j=j, c0=cols[j]: e.dma_start(
                                out=sgc[:, j * 128:(j + 1) * 128], in_=sgconv_d[:, c0:c0 + 128]),
                                writes=[sgc.b], dma=True, join=(j > 0))
                        bk = bank()

                        def tr(e, bk=bk):
                            for j in range(4):
                                r = e.transpose(bk[:, j * 48:(j + 1) * 48], sgc[:, j * 128:(j + 1) * 128], ident[0:48, 0:48])
                            return r
                        P.add("pe", tr, reads=[sgc.b, ident.b], writes=[bk.b])
                        P.add("dve", copy_op("dve", tl3[:], bk[:, 0:192].rearrange("p (a b) -> p a b", a=4)),
                              reads=[bk.b], writes=[tl3.b])
                    for j in range(6):
                        if j in (0, 4, 5) and not blk["qz"]:
                            continue
                        bk = bank()

                        def mm(e, j=j, bk=bk, tok0=tok0, NB=NB):
                            for k in range(KC):
                                r = e.matmul(bk[:, 0:NB], lhsT=wgrp[:, j, k, :], rhs=HT[:, k, tok0:tok0 + NB],
                                             start=(k == 0), stop=(k == KC - 1))
                            return r
                        P.add("pe", mm, reads=[wgrp.b, HT.b], writes=[bk.b])
                        if j >= 4:
                            P.add("act", lambda e, j=j, bk=bk, NB=NB: e.activation(
                                out=QKVZ[:, j, 0:NB], in_=bk[:, 0:NB], func=AF.Silu), reads=[bk.b], writes=[QKVZ.b], join=True)
                            continue
                        F = Fb
                        Fv = F[:, 0:nseq * (T + 3)].rearrange("p (s t) -> p s t", s=nseq)
                        P.add("act", lambda e, Fv=Fv, bk=bk, NB=NB, nseq=nseq: e.activation(
                            out=Fv[:, :, 3:], in_=bk[:, 0:NB].rearrange("p (s t) -> p s t", s=nseq), func=AF.Copy),
                            reads=[bk.b], writes=[F.b])
                        if sample:
                            P.add("pool", lambda e, Fv=Fv, j=j: e.tensor_copy(
                                out=Fv[:, :, 0:3], in_=tl3[:, j, :].rearrange("p (s r) -> p s r", r=3)),
                                reads=[tl3.b], writes=[F.b], join=True)
                        elif first and bi == 0:
                            P.add("pool", lambda e, Fv=Fv: e.memset(Fv[:, 0, 0:3], 0.0), writes=[F.b], join=True)
                        elif (not first) and bi == 0:
                            P.add("pool", lambda e, Fv=Fv, j=j, g=g: e.tensor_scalar(
                                out=Fv[:, 0, 0:3], in0=tails[:, j, g, :], scalar1=f1[:, 0:1], scalar2=None, op0=ALU.mult),
                                reads=[tails.b, f1.b], writes=[F.b], join=True)
                        else:
                            P.add("pool", lambda e, Fv=Fv, j=j, g=g: e.tensor_copy(out=Fv[:, 0, 0:3], in_=tails[:, j, g, :]),
                                  reads=[tails.b], writes=[F.b], join=True)
                        ca = cacc
                        cav = ca[:, 0:NB].rearrange("p (s t) -> p s t", s=nseq)
                        wcol = 272 + cols[j] // 128
                        en = "dve"
                        P.add(en, lambda e, cav=cav, Fv=Fv, wcol=wcol, T=T: e.tensor_scalar(
                            out=cav, in0=Fv[:, :, 0:T], scalar1=vecT[:, wcol:wcol + 1], scalar2=None, op0=ALU.mult),
                            reads=[F.b, vecT.b], writes=[ca.b])
                        for tp in range(1, 4):
                            P.add(en, lambda e, cav=cav, Fv=Fv, wcol=wcol, T=T, tp=tp: e.scalar_tensor_tensor(
                                out=cav, in0=Fv[:, :, tp:tp + T], scalar=vecT[:, wcol + 64 * tp:wcol + 64 * tp + 1],
                                in1=cav, op0=ALU.mult, op1=ALU.add), reads=[F.b, vecT.b, ca.b], writes=[ca.b])
                        if sample:
                            P.add("pool", lambda e, Fv=Fv, j=j: e.tensor_copy(
                                out=tl3[:, j, :].rearrange("p (s r) -> p s r", r=3), in_=Fv[:, :, 4:7]),
                                reads=[F.b], writes=[tl3.b])
                        else:
                            P.add("pool", lambda e, Fv=Fv, j=j, g=g, T=T: e.tensor_copy(
                                out=tails[:, j, g, :], in_=Fv[:, 0, T:T + 3]), reads=[F.b], writes=[tails.b])
                        if j >= 2:
                            P.add("act", lambda e, j=j, ca=ca, NB=NB: e.activation(
                                out=QKVZ[:, j, 0:NB], in_=ca[:, 0:NB], func=AF.Silu), reads=[ca.b], writes=[QKVZ.b], join=True)
                        else:
                            P.add("act", lambda e, ca=ca, NB=NB: e.activation(out=ca[:, 0:NB], in_=ca[:, 0:NB], func=AF.Silu),
                                  reads=[ca.b], writes=[ca.b])
                            P.add("act", lambda e, ca=ca, NB=NB: e.activation(
                                out=sqt[:, 0:NB], in_=ca[:, 0:NB], func=AF.Square), reads=[ca.b], writes=[sqt.b])
                            bk2 = bank()
                            P.add("pe", lambda e, bk2=bk2, NB=NB: e.matmul(
                                bk2[:, 0:NB], lhsT=ones_f[:], rhs=sqt[:, 0:NB], start=True, stop=True),
                                reads=[sqt.b, ones_f.b], writes=[bk2.b])
                            P.add("act", lambda e, bk2=bk2, NB=NB: e.activation(
                                out=sqt[:, 0:NB], in_=bk2[:, 0:NB], func=AF.Sqrt, bias=epst[:, 0:1]),
                                reads=[bk2.b, epst.b], writes=[sqt.b])
                            P.add("dve", lambda e, NB=NB: e.reciprocal(out=sqt[:, 0:NB], in_=sqt[:, 0:NB]),
                                  reads=[sqt.b], writes=[sqt.b])
                            sc_ = (128.0 ** -0.5) if j == 0 else 1.0
                            P.add("dve", lambda e, j=j, ca=ca, NB=NB, sc_=sc_: e.scalar_tensor_tensor(
                                out=QKVZ[:, j, 0:NB], in0=ca[:, 0:NB], scalar=sc_, in1=sqt[:, 0:NB],
                                op0=ALU.mult, op1=ALU.mult), reads=[ca.b, sqt.b], writes=[QKVZ.b], join=True)
                    if sample or (last and bi == nprompt - 1):
                        nr = 48 if sample else 3
                        bk = bank()

                        def tr(e, bk=bk, nr=nr, sample=sample, g=g):
                            for j in range(4):
                                src = tl3[:, j, :] if sample else tails[:, j, g, :]
                                r = e.transpose(bk[0:nr, j * 128:(j + 1) * 128], src, ident[:])
                            return r
                        P.add("pe", tr, reads=[tl3.b if sample else tails.b, ident.b], writes=[bk.b])
                        P.add("dve", copy_op("dve", sgo[0:nr, :], bk[0:nr, :]), reads=[bk.b], writes=[sgo.b])
                        dd = gconvs_d if sample else gconvp_d
                        for j in range(4):
                            P.add("pool", lambda e, j=j, nr=nr, dd=dd, c0=cols[j]: e.dma_start(
                                out=dd[:, c0:c0 + 128], in_=sgo[0:nr, j * 128:(j + 1) * 128]),
                                reads=[sgo.b], dma=True, out=True)

                    n = 4
                    for c0 in range(0, nch, n):
                        need_o = blk["o_from"] is not None and c0 >= blk["o_from"]
                        W = n * 2 * C
                        HC = n * 2
                        st0 = c0 * C
                        Mle = masks[0:C, 0, 0:C]
                        Mgt = masks[0:C, 1, 0:C]
                        Mtril = masks[0:C, 2, 0:C]
                        Mstr = masks[0:C, 3, 0:C]
                        psl = slice(pc0 + c0, pc0 + c0 + n)
                        hsl = slice(2 * g, 2 * g + 2)
                        gsl = pre["g"][0:C, psl, hsl]

                        def v4(t, C=C, W=W):
                            return t[0:C, 0:W].rearrange("p (a h c) -> p a h c", a=n, h=2)

                        def v3(t, C=C, W=W, HC=HC):
                            return t[0:C, 0:W].rearrange("p (a c) -> p a c", a=HC)

                        def f2(t, C=C, W=W):
                            return t[0:C, 0:W]

                        def bc_hc(ap2, C=C):
                            return ap2.unsqueeze(3).to_broadcast([C, n, 2, C])

                        def bc_m(m, C=C):
                            return m.unsqueeze(1).unsqueeze(1).to_broadcast([C, n, 2, C])
                        P.add("dve", lambda e, v4=v4, bc_hc=bc_hc, bc_m=bc_m, gsl=gsl, Mgt=Mgt: e.tensor_tensor(
                            out=v4(d["rgt"]), in0=bc_m(Mgt), in1=bc_hc(gsl), op=ALU.mult),
                            reads=[masks.b, pre["g"].b], writes=[d["rgt"].b])
                        P.add("pool", lambda e, v4=v4, bc_hc=bc_hc, bc_m=bc_m, gsl=gsl, Mle=Mle: e.tensor_tensor(
                            out=v4(d["rle"]), in0=bc_m(Mle), in1=bc_hc(gsl), op=ALU.mult),
                            reads=[masks.b, pre["g"].b], writes=[d["rle"].b])
                        bkG = bank()
                        P.add("pe", lambda e, bkG=bkG, Mle=Mle, W=W, C=C, f2=f2: e.matmul(
                            bkG[0:C, 0:W], lhsT=Mle, rhs=f2(d["rgt"]), start=True, stop=True),
                            reads=[masks.b, d["rgt"].b], writes=[bkG.b])
                        P.add("act", lambda e, bkG=bkG, W=W, C=C, f2=f2: e.activation(
                            out=f2(d["draw"]), in_=bkG[0:C, 0:W], func=AF.Exp), reads=[bkG.b], writes=[d["draw"].b])
                        P.add("pool", lambda e, v4=v4, bc_m=bc_m, Mtril=Mtril: e.tensor_tensor(
                            out=v4(d["dtril"]), in0=v4(d["draw"]), in1=bc_m(Mtril), op=ALU.mult),
                            reads=[d["draw"].b, masks.b], writes=[d["dtril"].b])
                        P.add("dve", lambda e, v4=v4, bc_m=bc_m, Mstr=Mstr: e.tensor_tensor(
                            out=v4(d["dstr"]), in0=v4(d["draw"]), in1=bc_m(Mstr), op=ALU.mult),
                            reads=[d["draw"].b, masks.b], writes=[d["dstr"].b])
                        bkE = bank()
                        P.add("pe", lambda e, bkE=bkE, W=W, C=C, f2=f2: e.matmul(
                            bkE[:, 0:W], lhsT=ones_f[0:C, :], rhs=f2(d["rle"]), start=True, stop=True),
                            reads=[ones_f.b, d["rle"].b], writes=[bkE.b])
                        P.add("act", lambda e, bkE=bkE, W=W: e.activation(
                            out=d["egcb"][:, 0:W], in_=bkE[:, 0:W], func=AF.Exp), reads=[bkE.b], writes=[d["egcb"].b])
                        qsl = QKVZ[:, 0, st0:st0 + n * C].rearrange("p (a c) -> p a c", a=n)
                        ksl = QKVZ[:, 1, st0:st0 + n * C].rearrange("p (a c) -> p a c", a=n)
                        if need_o:
                            P.add("dve", lambda e, qsl=qsl, W=W, C=C: e.tensor_tensor(
                                out=d["qd"][:, 0:W].rearrange("p (a h c) -> p a h c", a=n, h=2),
                                in0=qsl.unsqueeze(2).to_broadcast([128, n, 2, C]),
                                in1=d["egcb"][:, 0:W].rearrange("p (a h c) -> p a h c", a=n, h=2), op=ALU.mult),
                                reads=[QKVZ.b, d["egcb"].b], writes=[d["qd"].b])
                        bkK = bank()

                        def mmk(e, bkK=bkK, qsl=qsl, ksl=ksl, C=C, need_o=need_o):
                            for c in range(n):
                                r = e.matmul(bkK[0:C, (2 * c) * C:(2 * c + 1) * C], lhsT=ksl[:, c, :], rhs=ksl[:, c, :],
                                             start=True, stop=True)
                                if need_o:
                                    r = e.matmul(bkK[0:C, (2 * c + 1) * C:(2 * c + 2) * C], lhsT=qsl[:, c, :], rhs=ksl[:, c, :],
                                                 start=True, stop=True)
                            return r
                        P.add("pe", mmk, reads=[QKVZ.b], writes=[bkK.b])
                        kkv = bkK[0:C, 0:W].rearrange("p (a h c) -> p a h c", a=n, h=2)
                        P.add("dve", lambda e, v4=v4, kkv=kkv, C=C: e.tensor_tensor(
                            out=v4(d["p0"]), in0=v4(d["dstr"]),
                            in1=kkv[:, :, 0:1, :].to_broadcast([C, n, 2, C]), op=ALU.mult),
                            reads=[d["dstr"].b, bkK.b], writes=[d["p0"].b])
                        nbs = pre["nbeta"][0:C, psl, hsl]
                        P.add("dve", lambda e, v4=v4, bc_hc=bc_hc, nbs=nbs: e.tensor_tensor(
                            out=v4(d["p0"]), in0=v4(d["p0"]), in1=bc_hc(nbs), op=ALU.mult),
                            reads=[d["p0"].b, pre["nbeta"].b], writes=[d["p0"].b])
                        if need_o:
                            P.add("dve", lambda e, v4=v4, kkv=kkv, C=C: e.tensor_tensor(
                                out=v4(d["a"]), in0=v4(d["dtril"]),
                                in1=kkv[:, :, 1:2, :].to_broadcast([C, n, 2, C]), op=ALU.mult),
                                reads=[d["dtril"].b, bkK.b], writes=[d["a"].b])

                        def transpose_hc(src, dst, eng_ev, C=C, W=W, HC=HC):
                            bkT = bank()

                            def tr(e, src=src, bkT=bkT):
                                for hc in range(HC):
                                    r = e.transpose(bkT[0:C, hc * C:(hc + 1) * C], src[0:C, hc * C:(hc + 1) * C], ident[0:C, 0:C])
                                return r
                            P.add("pe", tr, reads=[src.b, ident.b], writes=[bkT.b])
                            P.add(eng_ev, copy_op(eng_ev, dst[0:C, 0:W], bkT[0:C, 0:W]), reads=[bkT.b], writes=[dst.b])
                        transpose_hc(d["p0"], d["pt0"], "act")
                        if need_o:
                            transpose_hc(d["a"], d["at"], "act")
                        P.add("dve", lambda e, v3=v3, HC=HC, C=C: e.tensor_tensor(
                            out=v3(d["rt"]), in0=v3(d["pt0"]),
                            in1=ident[0:C, 0:C].unsqueeze(1).to_broadcast([C, HC, C]), op=ALU.add),
                            reads=[d["pt0"].b, ident.b], writes=[d["rt"].b])
                        nsteps = {64: 5, 4: 1}[C]
                        cur = 0
                        for stp in range(nsteps):
                            Pc, PTc = d["p%d" % cur], d["pt%d" % cur]
                            Pn, PTn = d["p%d" % (1 - cur)], d["pt%d" % (1 - cur)]
                            lastst = (stp == nsteps - 1)
                            bkP = bank()

                            def mmp(e, Pc=Pc, PTc=PTc, bkP=bkP, C=C, HC=HC):
                                for hc in range(HC):
                                    sl = slice(hc * C, (hc + 1) * C)
                                    r = e.matmul(bkP[0:C, sl], lhsT=PTc[0:C, sl], rhs=Pc[0:C, sl], start=True, stop=True)
                                return r
                            P.add("pe", mmp, reads=[Pc.b, PTc.b], writes=[bkP.b])
                            if not lastst:
                                bkQ = bank()

                                def mmq(e, Pc=Pc, PTc=PTc, bkQ=bkQ, C=C, HC=HC):
                                    for hc in range(HC):
                                        sl = slice(hc * C, (hc + 1) * C)
                                        r = e.matmul(bkQ[0:C, sl], lhsT=Pc[0:C, sl], rhs=PTc[0:C, sl], start=True, stop=True)
                                    return r
                                P.add("pe", mmq, reads=[Pc.b, PTc.b], writes=[bkQ.b])
                            P.add("act", copy_op("act", Pn[0:C, 0:W], bkP[0:C, 0:W]), reads=[bkP.b], writes=[Pn.b])
                            if not lastst:
                                P.add("dve", copy_op("dve", PTn[0:C, 0:W], bkQ[0:C, 0:W]), reads=[bkQ.b], writes=[PTn.b])
                            bkR = bank()

                            def mmr(e, Pn=Pn, bkR=bkR, C=C, HC=HC):
                                for hc in range(HC):
                                    sl = slice(hc * C, (hc + 1) * C)
                                    r = e.matmul(bkR[0:C, sl], lhsT=Pn[0:C, sl], rhs=d["rt"][0:C, sl], start=True, stop=True)
                                return r
                            P.add("pe", mmr, reads=[Pn.b, d["rt"].b], writes=[bkR.b])
                            P.add("dve", lambda e, bkR=bkR, W=W, C=C, f2=f2: e.tensor_tensor(
                                out=f2(d["rt"]), in0=f2(d["rt"]), in1=bkR[0:C, 0:W], op=ALU.add),
                                reads=[d["rt"].b, bkR.b], writes=[d["rt"].b])
                            cur = 1 - cur
                        bsl = pre["beta"][0:C, psl, hsl]
                        ngs = pre["nbeg"][0:C, psl, hsl]
                        P.add("pool", lambda e, v4=v4, bc_hc=bc_hc, bsl=bsl: e.tensor_tensor(
                            out=v4(d["tb"]), in0=v4(d["rt"]), in1=bc_hc(bsl), op=ALU.mult),
                            reads=[d["rt"].b, pre["beta"].b], writes=[d["tb"].b])
                        P.add("dve", lambda e, v4=v4, bc_hc=bc_hc, ngs=ngs: e.tensor_tensor(
                            out=v4(d["tg"]), in0=v4(d["rt"]), in1=bc_hc(ngs), op=ALU.mult),
                            reads=[d["rt"].b, pre["nbeg"].b], writes=[d["tg"].b])
                        for q4 in range(0, HC, 4):
                            bkV = bank()

                            def trv(e, bkV=bkV, q4=q4, C=C, st0=st0):
                                for x in range(4):
                                    hc = q4 + x
                                    c, h = hc // 2, hc % 2
                                    r = e.transpose(bkV[0:C, x * 128:(x + 1) * 128],
                                                    QKVZ[:, 2 + h, st0 + c * C:st0 + (c + 1) * C], ident[:])
                                return r
                            P.add("pe", trv, reads=[QKVZ.b, ident.b], writes=[bkV.b])
                            en = ev_eng()
                            P.add(en, copy_op(en, d["vtok"][0:C, q4 * 128:(q4 + 4) * 128], bkV[0:C, 0:512]),
                                  reads=[bkV.b], writes=[d["vtok"].b], join=(q4 > 0))
                        bkV = bank()

                        def trk(e, bkV=bkV, C=C, st0=st0):
                            for c in range(4):
                                r = e.transpose(bkV[0:C, c * 128:(c + 1) * 128],
                                                QKVZ[:, 1, st0 + c * C:st0 + (c + 1) * C], ident[:])
                            return r
                        P.add("pe", trk, reads=[QKVZ.b, ident.b], writes=[bkV.b])
                        en = ev_eng()
                        P.add(en, copy_op(en, d["ktok"][0:C, 0:512], bkV[0:C, 0:512]), reads=[bkV.b], writes=[d["ktok"].b])
                        ess = pre["esuf"][0:C, psl, hsl]
                        P.add("pool", lambda e, ess=ess, C=C: e.tensor_tensor(
                            out=d["kd"][0:C, :].rearrange("p (a h c) -> p a h c", a=n, h=2),
                            in0=d["ktok"][0:C, :].rearrange("p (a c) -> p a c", a=n).unsqueeze(2).to_broadcast([C, n, 2, 128]),
                            in1=ess.unsqueeze(3).to_broadcast([C, n, 2, 128]), op=ALU.mult),
                            reads=[d["ktok"].b, pre["esuf"].b], writes=[d["kd"].b])
                        bkW = bank()

                        def mmw(e, bkW=bkW, C=C, HC=HC):
                            for hc in range(HC):
                                c = hc // 2
                                r = e.matmul(bkW[:, hc * C:(hc + 1) * C], lhsT=d["ktok"][0:C, c * 128:(c + 1) * 128],
                                             rhs=d["tg"][0:C, hc * C:(hc + 1) * C], start=True, stop=True)
                            return r
                        P.add("pe", mmw, reads=[d["ktok"].b, d["tg"].b], writes=[bkW.b])
                        P.add("act", copy_op("act", d["wt"][:, 0:W], bkW[:, 0:W]), reads=[bkW.b], writes=[d["wt"].b])
                        bkO = None
                        for c in range(n):
                            for h in range(2):
                                hc = c * 2 + h
                                head = 2 * g + h
                                sl = slice(hc * C, (hc + 1) * C)
                                vsl = slice(hc * 128, (hc + 1) * 128)
                                if sample:
                                    St = S[hc % 4]
                                    P.add("sp", lambda e, St=St, sq_=c0 + c, head=head: e.dma_start(
                                        out=St[:], in_=sgdn_d[sq_, head]), writes=[St.b], dma=True)
                                else:
                                    St = S[h]
                                    if bi == 0 and c0 == 0 and c == 0:
                                        if first:
                                            P.add("pool", lambda e, St=St: e.memset(St[:], 0.0), writes=[St.b])
                                        else:
                                            P.add("sp", lambda e, St=St, head=head: e.dma_start(out=St[:], in_=sscr_d[head]),
                                                  reads=[bscr[head]], writes=[St.b], dma=True)
                                            P.add("dve", lambda e, St=St: e.tensor_scalar(
                                                out=St[:], in0=St[:], scalar1=f1[:, 0:1], scalar2=None, op0=ALU.mult),
                                                reads=[St.b, f1.b], writes=[St.b])
                                bkVn = bank()
                                vn = d["vn"][hc % 4]

                                def mm1(e, bkVn=bkVn, sl=sl, vsl=vsl, St=St, C=C):
                                    e.matmul(bkVn[0:C, 0:128], lhsT=d["tb"][0:C, sl], rhs=d["vtok"][0:C, vsl], start=True, stop=False)
                                    return e.matmul(bkVn[0:C, 0:128], lhsT=d["wt"][:, sl], rhs=St[:], start=False, stop=True)
                                P.add("pe", mm1, reads=[d["tb"].b, d["vtok"].b, d["wt"].b, St.b], writes=[bkVn.b])
                                P.add("act", copy_op("act", vn[0:C, :], bkVn[0:C, 0:128]), reads=[bkVn.b], writes=[vn.b])
                                if need_o:
                                    if hc % 4 == 0:
                                        bkO = obank()

                                    def mm2(e, bkO=bkO, sl=sl, St=St, vn=vn, x=hc % 4, C=C):
                                        e.matmul(bkO[0:C, x * 128:(x + 1) * 128], lhsT=d["qd"][:, sl], rhs=St[:], start=True, stop=False)
                                        return e.matmul(bkO[0:C, x * 128:(x + 1) * 128], lhsT=d["at"][0:C, sl], rhs=vn[0:C, :],
                                                        start=False, stop=True)
                                    P.add("pe", mm2, reads=[d["qd"].b, d["at"].b, St.b, vn.b], writes=[bkO.b], join=(hc % 4 != 0))
                                    if hc % 4 == 3:
                                        q4 = hc - 3
                                        P.add("dve", copy_op("dve", d["otok"][0:C, q4 * 128:(q4 + 4) * 128], bkO[0:C, 0:512]),
                                              reads=[bkO.b], writes=[d["otok"].b], join=(q4 > 0))
                                bkS = bank()
                                P.add("pe", lambda e, bkS=bkS, vsl=vsl, vn=vn, C=C: e.matmul(
                                    bkS[:, 0:128], lhsT=d["kd"][0:C, vsl], rhs=vn[0:C, :], start=True, stop=True),
                                    reads=[d["kd"].b, vn.b], writes=[bkS.b])
                                gcol = hc * C + C - 1
                                P.add("dve", lambda e, bkS=bkS, St=St, gcol=gcol: e.scalar_tensor_tensor(
                                    out=St[:], in0=St[:], scalar=d["egcb"][:, gcol:gcol + 1], in1=bkS[:, 0:128],
                                    op0=ALU.mult, op1=ALU.add), reads=[St.b, d["egcb"].b, bkS.b], writes=[St.b])
                                if sample:
                                    P.add("pool", lambda e, St=St, sq_=c0 + c, head=head: e.dma_start(
                                        out=gdns_d[sq_, head], in_=St[:]), reads=[St.b], dma=True, out=True)
                                elif bi == nprompt - 1 and c0 + n == nch and c == n - 1:
                                    if last:
                                        P.add("pool", lambda e, St=St, head=head: e.dma_start(out=gdnp_d[head], in_=St[:]),
                                              reads=[St.b], dma=True, out=True)
                                    else:
                                        P.add("pool", lambda e, St=St, head=head: e.dma_start(out=sscr_d[head], in_=St[:]),
                                              reads=[St.b], writes=[bscr[head]], dma=True)
                        if need_o:
                            ot_ = d["otok"][0:C, :]
                            P.add("act", lambda e, ot_=ot_, C=C: e.activation(out=d["osq"][0:C, :], in_=ot_, func=AF.Square),
                                  reads=[d["otok"].b], writes=[d["osq"].b])
                            P.add("dve", lambda e, HC=HC, C=C: e.tensor_reduce(
                                out=d["ors"][0:C, 0:HC], in_=d["osq"][0:C, :].rearrange("p (a c) -> p a c", a=HC),
                                axis=AX.X, op=ALU.add), reads=[d["osq"].b], writes=[d["ors"].b])
                            P.add("act", lambda e, HC=HC, C=C: e.activation(
                                out=d["ors"][0:C, 0:HC], in_=d["ors"][0:C, 0:HC], func=AF.Sqrt, scale=1.0 / 128, bias=epst[0:C, 0:1]),
                                reads=[d["ors"].b, epst.b], writes=[d["ors"].b])
                            P.add("dve", lambda e, HC=HC, C=C: e.reciprocal(out=d["ors"][0:C, 0:HC], in_=d["ors"][0:C, 0:HC]),
                                  reads=[d["ors"].b], writes=[d["ors"].b])
                            P.add("dve", lambda e, HC=HC, C=C, ot_=ot_: e.tensor_tensor(
                                out=ot_.rearrange("p (a c) -> p a c", a=HC), in0=ot_.rearrange("p (a c) -> p a c", a=HC),
                                in1=d["ors"][0:C, 0:HC].unsqueeze(2).to_broadcast([C, HC, 128]), op=ALU.mult),
                                reads=[d["otok"].b, d["ors"].b], writes=[d["otok"].b])
                            bkOT = bank()

                            def tro(e, bkOT=bkOT, C=C, HC=HC):
                                for hc in range(HC):
                                    r = e.transpose(bkOT[:, hc * C:(hc + 1) * C], d["otok"][0:C, hc * 128:(hc + 1) * 128],
                                                    ident[0:C, 0:C])
                                return r
                            P.add("pe", tro, reads=[d["otok"].b, ident.b], writes=[bkOT.b])
                            for h in range(2):
                                P.add("dve", lambda e, h=h, bkOT=bkOT, C=C, st0=st0: e.scalar_tensor_tensor(
                                    out=og[:, h, st0:st0 + n * C].rearrange("p (a c) -> p a c", a=n),
                                    in0=bkOT[:, 0:n * 2 * C].rearrange("p (a h c) -> p a h c", a=n, h=2)[:, :, h, :],
                                    scalar=vecT[:, 576:577],
                                    in1=QKVZ[:, 4 + h, st0:st0 + n * C].rearrange("p (a c) -> p a c", a=n),
                                    op0=ALU.mult, op1=ALU.mult), reads=[bkOT.b, vecT.b, QKVZ.b], writes=[og.b], join=True)
                        if dbg.get('stop') == 'sub' and g == dbg.get('g', 0) and bi == dbg.get('bi', 0) and c0 == dbg.get('c0', 0) and first == dbg.get('first', True):
                            for nm_ in ('beta', 'g', 'esuf', 'nbeg'):
                                dump(pre[nm_], 'pre_' + nm_, [64, totch, 32])
                            dump(QKVZ, 'QKVZ', [128, 6, 512])
                            for nm_ in ('rt', 'tb', 'tg', 'at', 'p0', 'pt0'):
                                dump(d[nm_], nm_, [64, 512])
                            for nm_ in ('egcb', 'qd', 'wt'):
                                dump(d[nm_], nm_, [128, 512])
                            for nm_ in ('vtok', 'kd', 'otok'):
                                dump(d[nm_], nm_, [64, 1024])
                            dump(d['ktok'], 'ktok', [64, 512])
                            dump(og, 'og', [128, 2, 512], BF16)
                            dump(S[0], 'S0', [128, 128])
                            dump(S[1], 'S1', [128, 128])
                            dbg['_halt'] = True
                            return
                    if blk["o_from"] is not None:
                        ot0 = blk["otok0"]
                        lo = blk.get("olo", 0)
                        for h in range(2):
                            head = 2 * g + h
                            P.add("pool", lambda e, h=h, head=head, ot0=ot0, NB=NB, lo=lo: e.dma_start(
                                out=oscr_d[head, :, ot0:ot0 + NB - lo], in_=og[:, h, lo:NB]),
                                reads=[og.b], writes=[boscr[head]], dma=True, join=True)

        with scope() as sc:
            XT = sb([128, KC, NT], stack=sc, name="XTp")
            load_xT(XT, xp_d, NPR, sc)
            norm_mod(XT, NPR, 0, 0, 0, sc)
            dump(XT, 'XT_P', [128, KC, NT])
        P.fence()
        dump(HT, 'HT_P', [128, KC, NT], BF16)
        stop_at('normP')
        blocksP = [dict(C=64, nch=8, tok0=0, nseq=1, T=512, sample=False, otok0=0, qz=bool(dbg.get('p_full')), o_from=(0 if dbg.get('p_full') else None)),
                   dict(C=64, nch=8, tok0=512, nseq=1, T=512, sample=False, otok0=NPR + NSM, qz=True, o_from=4, olo=508)]
        with scope() as sc:
            gdn_pass(sc, blocksP, first=True, last=False)
        P.fence()
        if dbg.get('_halt'):
            raise _Stop()
        with scope() as sc:
            XT = sb([128, KC, NT], stack=sc, name="XTo")
            load_xT(XT, xo_d, NPR + NSM, sc)
            norm_mod(XT, NPR, NSM, 0, 0, sc)
        P.fence()
        blocksO = [dict(C=64, nch=8, tok0=0, nseq=1, T=512, sample=False, otok0=0, qz=True, o_from=0),
                   dict(C=64, nch=8, tok0=512, nseq=1, T=512, sample=False, otok0=512, qz=True, o_from=0),
                   dict(C=4, nch=16, tok0=1024, nseq=16, T=4, sample=True, otok0=1024, qz=True, o_from=0)]
        with scope() as sc:
            gdn_pass(sc, blocksO, first=False, last=True)
        P.fence()
        if 'oscr' in dbg.get('dumps', ()):
            dd_ = nc.dram_tensor('dbg_oscr', [32, 128, NT], BF16, kind='ExternalOutput').ap()
            P.add('sp', lambda e: e.dma_start(out=dd_, in_=oscr_d), reads=boscr, dma=True, out=True)
        stop_at('gdnO')
        if dbg.get('_halt'):
            raise _Stop()

        XT = sb([128, KC, NT], stack=fin, name="XT")
        with scope() as sc:
            load_xT(XT, xo_d, NT, sc)
        tiles = tok_tiles(NT)
        NSA = (NT - NPR) // 4
        ACT_FS = 8
        AT_ = sb([128, ACT_FS, NT], BF16, stack=fin, name="AT")
        gtmp = sb([128, 128], stack=fin, name="gtmp")

        tiles_mm = [(0, 364), (364, 364), (728, 364)]

        def resid_epilogue(l, s):
            gab = (2 + 3 * s) * 16

            def ep(cc, t0, tn, bk):
                pe_ = min(t0 + tn, NPR)
                if t0 < pe_:
                    P.add("dve", lambda e: e.scalar_tensor_tensor(
                        out=XT[:, cc, t0:pe_], in0=bk[:, 0:pe_ - t0], scalar=mod[:, l, gab + cc, 0:1],
                        in1=XT[:, cc, t0:pe_], op0=ALU.mult, op1=ALU.add),
                        reads=[bk.b, mod.b, XT.b], writes=[XT.b])
                if t0 + tn > NPR:
                    s0 = max(t0, NPR)
                    sn = t0 + tn - s0
                    ns = sn // 4
                    g0 = (s0 - NPR) // 4
                    P.add("dve", lambda e: e.tensor_tensor(
                        out=gtmp[:, 0:sn].rearrange("p (s t) -> p s t", t=4),
                        in0=bk[:, s0 - t0:s0 - t0 + sn].rearrange("p (s t) -> p s t", t=4),
                        in1=mod[:, l, gab + cc, 1 + g0:1 + g0 + ns].unsqueeze(2).to_broadcast([128, ns, 4]), op=ALU.mult),
                        reads=[bk.b, mod.b], writes=[gtmp.b])
                    P.add("dve", lambda e: e.tensor_tensor(
                        out=XT[:, cc, s0:s0 + sn], in0=XT[:, cc, s0:s0 + sn], in1=gtmp[:, 0:sn], op=ALU.add),
                        reads=[gtmp.b, XT.b], writes=[XT.b])
            return ep

        at_rhs = lambda k, t0, tn: AT_[:, k, t0:t0 + tn]
        ht_rhs = lambda k, t0, tn: HT[:, k, t0:t0 + tn]

        ep = resid_epilogue(0, 0)
        for hg in range(4):
            for hh in range(8):
                head = hg * 8 + hh
                P.add("sp", lambda e, hh=hh, head=head: e.dma_start(out=AT_[:, hh, :], in_=oscr_d[head]),
                      reads=[boscr[head]], writes=[AT_.b], dma=True, join=(hh > 0))
            linear(w_gout_d[hg * 1024:(hg + 1) * 1024, :], 0, 16, 8, at_rhs, tiles_mm, ep, [AT_.b])

        dump(XT, 'XT_gout', [128, KC, NT])

        def glu_mlp(w_up_d, w_dn_d, hid, l, gate_bc=None):
            nhc = hid // 128
            ep = resid_epilogue(l, 1)
            for f0 in range(0, nhc, ACT_FS):
                fs = min(ACT_FS, nhc - f0)

                def ep_gate(cc, t0, tn, bk):
                    P.add("act", lambda e: e.activation(out=AT_[:, cc, t0:t0 + tn], in_=bk[:, 0:tn], func=AF.Silu),
                          reads=[bk.b], writes=[AT_.b], join=True)

                def ep_up(cc, t0, tn, bk):
                    P.add("dve", lambda e: e.tensor_tensor(out=AT_[:, cc, t0:t0 + tn], in0=AT_[:, cc, t0:t0 + tn],
                                                          in1=bk[:, 0:tn], op=ALU.mult),
                          reads=[bk.b, AT_.b], writes=[AT_.b])
                    if gate_bc is not None:
                        P.add("pool", lambda e: e.tensor_tensor(out=AT_[:, cc, t0:t0 + tn], in0=AT_[:, cc, t0:t0 + tn],
                                                               in1=gate_bc[:, t0:t0 + tn], op=ALU.mult),
                              reads=[gate_bc.b, AT_.b], writes=[AT_.b])
                linear(w_up_d, f0 * 128, fs, KC, ht_rhs, tiles_mm, ep_gate, [HT.b])
                linear(w_up_d, hid + f0 * 128, fs, KC, ht_rhs, tiles_mm, ep_up, [HT.b])
                linear(w_dn_d[f0 * 128:(f0 + fs) * 128, :], 0, 16, fs, at_rhs, tiles_mm, ep, [AT_.b])

        with scope() as sc:
            norm_mod(XT, NPR, NT - NPR, 0, 1, sc)
        glu_mlp(w_fup_d, w_fdn_d, FFN, 0)
        dump(XT, 'XT_ffn', [128, KC, NT])

        with scope() as sc:
            norm_mod(XT, NPR, NT - NPR, 1, 0, sc)
        with scope() as sc:
            F = sb([128, NPR + 2 + NSA * 6], stack=sc, name="Fs")
            Fp = F[:, 0:NPR + 2]
            Fsm = F[:, NPR + 2:NPR + 2 + NSA * 6].rearrange("p (s t) -> p s t", t=6)
            cgt = [sb([128, 512], stack=sc, name="cgt") for _ in range(2)]
            yv = sb([128, NT], stack=sc, name="yv")
            sst = sb([32, 512], stack=sc, name="sst")
            ssT = sb([128, KC, 32], stack=sc, name="ssT")
            sso = sb([128, KC, 34], stack=sc, name="sso")
            sot = sb([34, 512], stack=sc, name="sot")
            for g4 in range(4):
                P.add("sp", lambda e, g4=g4: e.dma_start(out=sst[:], in_=ssconv_d[:, g4 * 512:(g4 + 1) * 512]),
                      writes=[sst.b], dma=True)
                bk = bank()

                def tr(e, g4=g4, bk=bk):
                    for j in range(4):
                        r = e.transpose(bk[:, j * 32:(j + 1) * 32], sst[:, j * 128:(j + 1) * 128], ident[0:32, 0:32])
                    return r
                P.add("pe", tr, reads=[sst.b, ident.b], writes=[bk.b])
                P.add("dve", copy_op("dve", ssT[:, g4 * 4:(g4 + 1) * 4, :], bk[:, 0:128].rearrange("p (a b) -> p a b", a=4)),
                      reads=[bk.b], writes=[ssT.b], join=(g4 > 0))
            P.add("pool", lambda e: e.memset(F[:], 0.0), writes=[F.b])
            ep_res = resid_epilogue(1, 0)
            for half in range(2):
                for c8 in range(8):
                    cc = half * 8 + c8
                    wts = [load_w(w_scin_d[:, o * 2048 + cc * 128:o * 2048 + (cc + 1) * 128], KC, 128) for o in (1, 2)]
                    for (t0, tn) in tiles:
                        cg = cgt[(t0 // 512) % 2]
                        for o in range(2):
                            wt_, wvv = wts[o]
                            bk = bank()

                            def mm(e, wvv=wvv, t0=t0, tn=tn, bk=bk):
                                for k in range(KC):
                                    r = e.matmul(bk[:, 0:tn], lhsT=wvv[:, k, :], rhs=HT[:, k, t0:t0 + tn],
                                                 start=(k == 0), stop=(k == KC - 1))
                                return r
                            P.add("pe", mm, reads=[wt_.b, HT.b], writes=[bk.b])
                            if o == 0:
                                P.add("act", copy_op("act", cg[:, 0:tn], bk[:, 0:tn]), reads=[bk.b], writes=[cg.b])
                            elif t0 < NPR:
                                P.add("dve", lambda e, cg=cg, bk=bk, t0=t0, tn=tn: e.tensor_tensor(
                                    out=Fp[:, 2 + t0:2 + t0 + tn], in0=cg[:, 0:tn], in1=bk[:, 0:tn], op=ALU.mult),
                                    reads=[cg.b, bk.b], writes=[F.b])
                            else:
                                P.add("dve", lambda e, cg=cg, bk=bk, tn=tn: e.tensor_tensor(
                                    out=Fsm[:, :, 2:6], in0=cg[:, 0:tn].rearrange("p (s t) -> p s t", t=4),
                                    in1=bk[:, 0:tn].rearrange("p (s t) -> p s t", t=4), op=ALU.mult),
                                    reads=[cg.b, bk.b], writes=[F.b])
                    P.add("pool", lambda e, cc=cc: e.tensor_copy(
                        out=Fsm[:, 0:NSQ, 0:2], in_=ssT[:, cc, :].rearrange("p (s r) -> p s r", r=2)),
                        reads=[ssT.b], writes=[F.b])
                    P.add("pool", lambda e: e.tensor_scalar(out=Fp[:, 0:2], in0=Fsm[:, NSQ, 4:6], scalar1=f1[:, 0:1],
                                                           scalar2=None, op0=ALU.mult), reads=[F.b, f1.b], writes=[F.b])
                    wcol = 528 + cc
                    en = "dve"
                    yvs = yv[:, NPR:NT].rearrange("p (s t) -> p s t", t=4)
                    P.add(en, lambda e, wcol=wcol: e.tensor_scalar(
                        out=yv[:, 0:NPR], in0=Fp[:, 0:NPR], scalar1=vecT[:, wcol:wcol + 1], scalar2=None, op0=ALU.mult),
                        reads=[F.b, vecT.b], writes=[yv.b])
                    P.add(en, lambda e, wcol=wcol, yvs=yvs: e.tensor_scalar(
                        out=yvs, in0=Fsm[:, :, 0:4], scalar1=vecT[:, wcol:wcol + 1], scalar2=None, op0=ALU.mult),
                        reads=[F.b, vecT.b], writes=[yv.b], join=True)
                    for tp in (1, 2):
                        wc2 = wcol + 16 * tp
                        P.add(en, lambda e, wc2=wc2, tp=tp: e.scalar_tensor_tensor(
                            out=yv[:, 0:NPR], in0=Fp[:, tp:tp + NPR], scalar=vecT[:, wc2:wc2 + 1], in1=yv[:, 0:NPR],
                            op0=ALU.mult, op1=ALU.add), reads=[F.b, vecT.b, yv.b], writes=[yv.b])
                        P.add(en, lambda e, wc2=wc2, tp=tp, yvs=yvs: e.scalar_tensor_tensor(
                            out=yvs, in0=Fsm[:, :, tp:tp + 4], scalar=vecT[:, wc2:wc2 + 1], in1=yvs,
                            op0=ALU.mult, op1=ALU.add), reads=[F.b, vecT.b, yv.b], writes=[yv.b])
                    P.add("pool", lambda e, cc=cc: e.tensor_copy(out=sso[:, cc, 0:2], in_=Fp[:, NPR:NPR + 2]),
                          reads=[F.b], writes=[sso.b], join=True)
                    P.add("pool", lambda e, cc=cc: e.tensor_copy(
                        out=sso[:, cc, 2:34].rearrange("p (s r) -> p s r", r=2), in_=Fsm[:, 0:NSQ, 4:6]),
                        reads=[F.b], writes=[sso.b], join=True)
                    wb_, wbv = load_w(w_scin_d[:, cc * 128:(cc + 1) * 128], KC, 128)
                    for (t0, tn) in tiles:
                        bk = bank()

                        def mm(e, wbv=wbv, t0=t0, tn=tn, bk=bk):
                            for k in range(KC):
                                r = e.matmul(bk[:, 0:tn], lhsT=wbv[:, k, :], rhs=HT[:, k, t0:t0 + tn],
                                             start=(k == 0), stop=(k == KC - 1))
                            return r
                        P.add("pe", mm, reads=[wb_.b, HT.b], writes=[bk.b])
                        P.add("dve", lambda e, c8=c8, bk=bk, t0=t0, tn=tn: e.tensor_tensor(
                            out=AT_[:, c8, t0:t0 + tn], in0=bk[:, 0:tn], in1=yv[:, t0:t0 + tn], op=ALU.mult),
                            reads=[bk.b, yv.b], writes=[AT_.b], join=True)
                linear(w_scout_d[half * 1024:(half + 1) * 1024, :], 0, 16, 8, at_rhs, tiles_mm, ep_res, [AT_.b])
            for g4 in range(4):
                bk = bank()

                def tr(e, g4=g4, bk=bk):
                    for j in range(4):
                        k = g4 * 4 + j
                        r = e.transpose(bk[0:34, j * 128:(j + 1) * 128], sso[:, k, :], ident[:])
                    return r
                P.add("pe", tr, reads=[sso.b, ident.b], writes=[bk.b])
                P.add("dve", copy_op("dve", sot[:, :], bk[0:34, 0:512]), reads=[bk.b], writes=[sot.b])
                P.add("pool", lambda e, g4=g4: e.dma_start(out=sconvp_d[:, g4 * 512:(g4 + 1) * 512], in_=sot[0:2, :]),
                      reads=[sot.b], dma=True, out=True)
                P.add("pool", lambda e, g4=g4: e.dma_start(out=sconvs_d[:, g4 * 512:(g4 + 1) * 512], in_=sot[2:34, :]),
                      reads=[sot.b], dma=True, out=True)

        dump(XT, 'XT_sc', [128, KC, NT])
        gT = sb([8, NT], stack=fin, name="gT")
        gbc = sb([128, NT], stack=fin, name="gbc")
        with scope() as sc:
            wrs = sb([128, KC, NEXP], stack=sc, name="wrs")
            P.add("sp", lambda e: e.dma_start(out=wrs[:], in_=w_rt_d.rearrange("(k p) c -> p k c", p=128)),
                  writes=[wrs.b], dma=True)
            norm_mod(XT, NPR, NT - NPR, 1, 1, sc, router=wrs)
        with scope() as sc:
            lg = sb([128, 9, 8], stack=sc, name="lg")
            m1 = sb([128, 9], stack=sc, name="m1")
            m2 = sb([128, 9], stack=sc, name="m2")
            k1 = sb([128, 9, 8], stack=sc, name="k1")
            k2 = sb([128, 9, 8], stack=sc, name="k2")
            l2 = sb([128, 9, 8], stack=sc, name="l2")
            LT = NT - 1024
            P.add("pool", lambda e: e.memset(lg[:], 0.0), writes=[lg.b])
            P.add("dve", lambda e: e.tensor_tensor(
                out=lg[:, 0:8, :], in0=rbank[:, 0:64].rearrange("p (a b) -> p a b", a=8),
                in1=hvec[:, 64:72].unsqueeze(1).to_broadcast([128, 8, 8]), op=ALU.add),
                reads=[rbank.b, hvec.b, lg.b], writes=[lg.b])
            P.add("dve", lambda e: e.tensor_tensor(out=lg[0:LT, 8, :], in0=rbank[0:LT, 64:72], in1=hvec[0:LT, 64:72], op=ALU.add),
                  reads=[rbank.b, hvec.b, lg.b], writes=[lg.b])
            b98 = lambda ap: ap.unsqueeze(2).to_broadcast([128, 9, 8])
            P.add("dve", lambda e: e.tensor_reduce(out=m1[:], in_=lg[:], axis=AX.X, op=ALU.max), reads=[lg.b], writes=[m1.b])
            P.add("dve", lambda e: e.tensor_tensor(out=k1[:], in0=lg[:], in1=b98(m1[:]), op=ALU.is_equal),
                  reads=[lg.b, m1.b], writes=[k1.b])
            P.add("dve", lambda e: e.scalar_tensor_tensor(out=l2[:], in0=k1[:], scalar=-1e30, in1=lg[:], op0=ALU.mult, op1=ALU.add),
                  reads=[k1.b, lg.b], writes=[l2.b])
            P.add("dve", lambda e: e.tensor_reduce(out=m2[:], in_=l2[:], axis=AX.X, op=ALU.max), reads=[l2.b], writes=[m2.b])
            P.add("dve", lambda e: e.tensor_tensor(out=k2[:], in0=l2[:], in1=b98(m2[:]), op=ALU.is_equal),
                  reads=[l2.b, m2.b], writes=[k2.b])
            P.add("dve", lambda e: e.tensor_tensor(out=m1[:], in0=m1[:], in1=m2[:], op=ALU.subtract),
                  reads=[m1.b, m2.b], writes=[m1.b])
            P.add("act", lambda e: e.activation(out=m1[:], in_=m1[:], func=AF.Exp), reads=[m1.b], writes=[m1.b])
            P.add("dve", lambda e: e.tensor_scalar(out=m1[:], in0=m1[:], scalar1=1.0, scalar2=None, op0=ALU.add),
                  reads=[m1.b], writes=[m1.b])
            P.add("dve", lambda e: e.reciprocal(out=m2[:], in_=m1[:]), reads=[m1.b], writes=[m2.b])
            P.add("dve", lambda e: e.tensor_scalar(out=m1[:], in0=m2[:], scalar1=-1.0, scalar2=1.0, op0=ALU.mult, op1=ALU.add),
                  reads=[m2.b], writes=[m1.b])
            P.add("dve", lambda e: e.tensor_tensor(out=k1[:], in0=k1[:], in1=b98(m1[:]), op=ALU.mult),
                  reads=[k1.b, m1.b], writes=[k1.b])
            P.add("dve", lambda e: e.tensor_tensor(out=k2[:], in0=k2[:], in1=b98(m2[:]), op=ALU.mult),
                  reads=[k2.b, m2.b], writes=[k2.b])
            P.add("dve", lambda e: e.tensor_tensor(out=k1[:], in0=k1[:], in1=k2[:], op=ALU.add),
                  reads=[k1.b, k2.b], writes=[k1.b])
            for t3 in range(3):
                bk = bank()
                nt3 = 4 if t3 < 2 else 1

                def trg(e, bk=bk, t3=t3, nt3=nt3):
                    for x in range(nt3):
                        t = t3 * 4 + x
                        tn = min(128, NT - t * 128)
                        r = e.transpose(bk[0:8, x * 128:x * 128 + tn], k1[0:tn, t, :], ident[0:tn, 0:tn])
                    return r
                P.add("pe", trg, reads=[k1.b, ident.b], writes=[bk.b])
                wd = 512 if t3 < 2 else LT
                P.add("dve", copy_op("dve", gT[:, t3 * 512:t3 * 512 + wd], bk[0:8, 0:wd]), reads=[bk.b], writes=[gT.b],
                      join=(t3 > 0))
        for ex in range(NEXP):
            for (t0, tn) in tiles:
                bk = bank()
                P.add("pe", lambda e, ex=ex, t0=t0, tn=tn, bk=bk: e.matmul(
                    bk[:, 0:tn], lhsT=sel[:, ex, :], rhs=gT[:, t0:t0 + tn], start=True, stop=True),
                    reads=[sel.b, gT.b], writes=[bk.b])
                P.add("act", copy_op("act", gbc[:, t0:t0 + tn], bk[:, 0:tn]), reads=[bk.b], writes=[gbc.b], join=(t0 > 0))
            glu_mlp(w_mup_d[ex], w_mdn_d[ex], EXD, 1, gate_bc=gbc)

        dump(XT, 'XT_moe', [128, KC, NT])
        dump(gT, 'gT', [8, NT])
        with scope() as sc:
            rs = sb([128, NT], stack=sc, name="rsf")
            rstd_bc(XT, NT, sc, rs)
            for k in range(KC):
                P.add("dve", lambda e, k=k: e.scalar_tensor_tensor(
                    out=XT[:, k, :], in0=XT[:, k, :], scalar=vecT[:, 256 + k:257 + k], in1=rs[:, :], op0=ALU.mult, op1=ALU.mult),
                    reads=[XT.b, vecT.b, rs.b], writes=[XT.b])
            y_ = sb([128, D], stack=sc, name="ys")
            for t in range(9):
                r0 = t * 128
                rows = min(128, NT - r0)
                for g4 in range(4):
                    bk = bank()

                    def tr(e, g4=g4, bk=bk, r0=r0, rows=rows):
                        for j in range(4):
                            k = g4 * 4 + j
                            r = e.transpose(bk[0:rows, j * 128:(j + 1) * 128], XT[:, k, r0:r0 + rows], ident[:])
                        return r
                    P.add("pe", tr, reads=[XT.b, ident.b], writes=[bk.b])
                    en = ev_eng()
                    P.add(en, copy_op(en, y_[0:rows, g4 * 512:(g4 + 1) * 512], bk[0:rows, 0:512]), reads=[bk.b], writes=[y_.b],
                          join=(g4 > 0))
                P.add("pool", lambda e, r0=r0, rows=rows: e.dma_start(out=yo_d[r0:r0 + rows, :], in_=y_[0:rows, :]),
                      reads=[y_.b], dma=True, out=True)

    except _Stop:
        pass
    sems = contextlib.ExitStack()
    P.emit(sems)
    sems.close()
    fin.close()
    top.close()
    return nc, P


_CACHE = {}


def _consts():
    ident = np.eye(128, dtype=np.float32)
    masks = np.zeros((64, 4, 64), np.float32)
    t = np.arange(64)[:, None]
    i = np.arange(64)[None, :]
    masks[:, 0] = (t <= i)
    masks[:, 1] = (t > i)
    masks[:, 2] = (t >= i)
    masks[:, 3] = (t > i)
    sel = np.zeros((8, 8, 128), np.float32)
    for e in range(8):
        sel[e, e, :] = 1.0
    return ident, masks, sel


def kernel(x_prompt, x_sample, c_prompt, c_sample, state_gdn, state_gdn_conv, state_sconv,
           w_ada, b_ada, g_norm_mix, g_norm_ffn, g_norm_out, gdn_w_in, gdn_conv_w,
           gdn_a_log, gdn_dt_bias, gdn_g_onorm, gdn_w_out, sc_w_in, sc_conv_w, sc_w_out,
           ffn_w_up, ffn_w_down, moe_w_router, moe_b_router, moe_w_up, moe_w_down):
    f = lambda a: np.ascontiguousarray(np.asarray(a, dtype=np.float32))
    x_prompt, x_sample, c_prompt, c_sample = f(x_prompt), f(x_sample), f(c_prompt), f(c_sample)
    state_gdn, state_gdn_conv, state_sconv = f(state_gdn), f(state_gdn_conv), f(state_sconv)
    ident, masks, sel = _consts()
    vecs = np.zeros((640, 128), np.float32)
    vecs[0:192] = f(b_ada).reshape(192, 128)
    vecs[192:224] = f(g_norm_mix).reshape(32, 128)
    vecs[224:256] = f(g_norm_ffn).reshape(32, 128)
    vecs[256:272] = f(g_norm_out).reshape(16, 128)
    vecs[272:528] = f(gdn_conv_w).reshape(256, 128)
    vecs[528:576] = f(sc_conv_w).reshape(48, 128)
    vecs[576] = f(gdn_g_onorm).reshape(128)
    hvec = np.concatenate([f(gdn_a_log).reshape(-1), f(gdn_dt_bias).reshape(-1), f(moe_b_router).reshape(-1)])[None, :]
    shared = {
        "vecs": vecs, "hvec": np.ascontiguousarray(hvec), "ident": ident, "masks": masks, "sel": sel,
        "w_ada": f(w_ada), "gdn_w_in": f(gdn_w_in)[0], "gdn_w_out": f(gdn_w_out)[0], "sc_w_in": f(sc_w_in)[0],
        "sc_w_out": f(sc_w_out)[0], "ffn_w_up": f(ffn_w_up)[0], "ffn_w_down": f(ffn_w_down)[0],
        "moe_w_router": f(moe_w_router)[0], "moe_w_up": f(moe_w_up)[0], "moe_w_down": f(moe_w_down)[0],
    }
    in_maps = []
    for c in range(8):
        s, m = c // 2, c % 2
        xs_ = x_sample[16 * c:16 * c + 16].reshape(64, D)
        xo = np.concatenate([x_prompt[s, m * 1024:(m + 1) * 1024], xs_, x_prompt[s, 1020:1024]], axis=0)
        xp = x_prompt[s, 0:1024]
        cvec = np.concatenate([c_prompt[s:s + 1], c_sample[16 * c:16 * c + 16], c_prompt[s:s + 1]], axis=0)
        d = dict(shared)
        d.update({
            "xo": np.ascontiguousarray(xo), "xp": np.ascontiguousarray(xp), "cvec": np.ascontiguousarray(cvec),
            "sgdn": np.ascontiguousarray(state_gdn[0, 16 * c:16 * c + 16]),
            "sgconv": np.ascontiguousarray(state_gdn_conv[0, 16 * c:16 * c + 16].reshape(48, 8192)),
            "ssconv": np.ascontiguousarray(state_sconv[0, 16 * c:16 * c + 16].reshape(32, D)),
            "f1": np.full((128, 1), float(m), np.float32),
        })
        in_maps.append(d)
    if _CACHE.get("dbg_hook") is not None:
        return _CACHE["dbg_hook"](in_maps)
    if "nc" not in _CACHE:
        _CACHE["nc"] = build_program()[0]
    nc = _CACHE["nc"]
    res = run_bass_kernel_spmd(nc, in_maps, core_ids=list(range(8)))
    R = res.results
    y_prompt = np.zeros((4, 2048, D), np.float32)
    y_sample = np.zeros((128, 4, D), np.float32)
    gdn_p = np.zeros((1, 4, 32, 128, 128), np.float32)
    gconv_p = np.zeros((1, 4, 3, 8192), np.float32)
    sconv_p = np.zeros((1, 4, 2, D), np.float32)
    gdn_s = np.zeros((1, 128, 32, 128, 128), np.float32)
    gconv_s = np.zeros((1, 128, 3, 8192), np.float32)
    sconv_s = np.zeros((1, 128, 2, D), np.float32)
    for c in range(8):
        s, m = c // 2, c % 2
        r = R[c]
        y_prompt[s, m * 1024:(m + 1) * 1024] = r["yo"][0:1024]
        y_sample[16 * c:16 * c + 16] = r["yo"][1024:1088].reshape(16, 4, D)
        if m == 1:
            gdn_p[0, s] = r["gdn_p"]
            gconv_p[0, s] = r["gconv_p"]
            sconv_p[0, s] = r["sconv_p"]
        gdn_s[0, 16 * c:16 * c + 16] = r["gdn_s"]
        gconv_s[0, 16 * c:16 * c + 16] = r["gconv_s"].reshape(16, 3, 8192)
        sconv_s[0, 16 * c:16 * c + 16] = r["sconv_s"].reshape(16, 2, D)
    return (y_prompt, y_sample, gdn_p, gconv_p, sconv_p, gdn_s, gconv_s, sconv_s)
```

```python
import contextlib
import numpy as np
import concourse.bass as bass
import concourse.mybir as mybir
from concourse.bass_utils import run_bass_kernel_spmd

F32 = mybir.dt.float32
BF16 = mybir.dt.bfloat16
AF = mybir.ActivationFunctionType
ALU = mybir.AluOpType
AX = mybir.AxisListType

SEM_ROT = 12000
D = 2048
KC = 16
NPR = 1024
NSQ = 16
NSM = 64
NHALO = 4
NT = NPR + NSM + NHALO
NMOD = 18
GIN = 12352
FFN = 5632
EXD = 7168
NEXP = 8
EPS = 1e-6


class Buf:
    __slots__ = ("name", "w", "r", "war")

    def __init__(self, name):
        self.name = name
        self.w = None
        self.r = []
        self.war = set()


class Prog:
    ENGS = ("pe", "act", "dve", "pool", "sp")

    def __init__(self, nc):
        self.nc = nc
        self.ins = []
        self.nb = 0
        self.out_dmas = []
        self.last = {}
        self.fence_deps = {}

    def buf(self, name=None):
        self.nb += 1
        return Buf(f"{name or 'b'}{self.nb}")

    def fence(self):
        allast = set(self.last.values())
        for e in self.ENGS:
            self.fence_deps[e] = set(allast)

    def add(self, eng, fn, reads=(), writes=(), dma=False, join=False, out=False):
        i = len(self.ins)
        deps = set()
        for b in reads:
            if b.w:
                deps.update(b.w.values())
        for b in writes:
            if b.w and not join:
                deps.update(b.w.values())
            for r in b.r:
                deps.add(r)
            if join and b.w:
                deps.update(b.war)
        for b in reads:
            b.r.append(i)
        ek = (eng, dma)
        for b in writes:
            if join and b.w:
                b.w[ek] = i
                b.war = b.war | set(b.r)
            else:
                b.w = {ek: i}
                b.war = set(b.r)
            b.r = []
        if eng in self.fence_deps:
            deps |= self.fence_deps.pop(eng)
        deps.discard(i)
        dsem = None
        if dma:
            dsem = writes[0].name if writes else reads[0].name
        self.ins.append(dict(eng=eng, fn=fn, deps=deps, dma=dma, dsem=dsem))
        self.last[eng] = i
        if out:
            self.out_dmas.append(i)
        return i

    def emit(self, stack):
        nc = self.nc
        ins = self.ins
        n = len(ins)
        needed = [False] * n
        for it in ins:
            for d in it["deps"]:
                if ins[d]["eng"] == "pe" and it["eng"] == "pe" and not ins[d]["dma"] and not it["dma"]:
                    continue
                needed[d] = True
        for i, it in enumerate(ins):
            if it["dma"]:
                needed[i] = True
        cnt = [0]

        def newsem(tag):
            cnt[0] += 1
            return stack.enter_context(nc.semaphore(f"s{tag}{cnt[0]}"))

        eng_sem, eng_cnt, dma_sems = {}, {}, {}
        tok = [None] * n
        for i, it in enumerate(ins):
            if not needed[i]:
                continue
            if it["dma"]:
                key = it["dsem"]
                if key not in dma_sems:
                    dma_sems[key] = [newsem("d"), 0]
                ds = dma_sems[key]
                ds[1] += 16
                tok[i] = (ds[0], ds[1], id(ds[0]))
                if ds[1] >= SEM_ROT * 16:
                    del dma_sems[key]
            else:
                e = it["eng"]
                if e not in eng_sem or eng_cnt[e] >= SEM_ROT:
                    eng_sem[e] = newsem(e)
                    eng_cnt[e] = 0
                eng_cnt[e] += 1
                tok[i] = (eng_sem[e], eng_cnt[e], id(eng_sem[e]))
        self.nsem = cnt[0]
        per = {e: [] for e in self.ENGS}
        for i, it in enumerate(ins):
            per[it["eng"]].append(i)
        final_waits = [tok[i] for i in self.out_dmas]

        def run_engine(ename, eh):
            known = {}
            for i in per[ename]:
                it = ins[i]
                waits = {}
                for d in it["deps"]:
                    if tok[d] is None:
                        continue
                    if ename == "pe" and ins[d]["eng"] == "pe" and not ins[d]["dma"] and not it["dma"]:
                        continue
                    s, v, k = tok[d]
                    if k not in waits or waits[k][1] < v:
                        waits[k] = (s, v)
                for k, (s, v) in waits.items():
                    if known.get(k, 0) < v:
                        eh.wait_ge(s, v)
                        known[k] = v
                r = it["fn"](eh)
                if tok[i] is not None:
                    s, v, k = tok[i]
                    r.then_inc(s, 16 if it["dma"] else 1)
            if ename == "sp":
                fw = {}
                for s, v, k in final_waits:
                    if k not in fw or fw[k][1] < v:
                        fw[k] = (s, v)
                for k, (s, v) in fw.items():
                    eh.wait_ge(s, v)

        with nc.Block() as block:
            @block.sync
            def _(e):
                run_engine("sp", e)

            @block.tensor
            def _(e):
                run_engine("pe", e)

            @block.scalar
            def _(e):
                run_engine("act", e)

            @block.vector
            def _(e):
                run_engine("dve", e)

            @block.gpsimd
            def _(e):
                run_engine("pool", e)


class TT:
    def __init__(self, t, b):
        self.t = t
        self.b = b

    def __getitem__(self, k):
        return self.t[k]


class _Stop(Exception):
    pass


def build_program(dbg=None):
    nc = bass.Bass("TRN2", target_bir_lowering=False)
    P = Prog(nc)
    dbg = dbg or {}

    def dump(tt, name, shape, dt=F32):
        if name not in dbg.get('dumps', ()):
            return
        dd = nc.dram_tensor('dbg_' + name, list(shape), dt, kind='ExternalOutput').ap()
        P.add('sp', lambda e: e.dma_start(out=dd, in_=tt[:]), reads=[tt.b], dma=True, out=True)

    @contextlib.contextmanager
    def scope():
        with contextlib.ExitStack() as sc_:
            yield sc_
        P.fence()

    def stop_at(tag):
        if dbg.get('stop') == tag:
            raise _Stop()

    def din(name, shape, dt=F32):
        return nc.dram_tensor(name, list(shape), dt, kind="ExternalInput").ap()

    def dout(name, shape, dt=F32):
        return nc.dram_tensor(name, list(shape), dt, kind="ExternalOutput").ap()

    xo_d = din("xo", [NT, D])
    xp_d = din("xp", [NPR, D])
    cvec_d = din("cvec", [NMOD, D])
    sgdn_d = din("sgdn", [NSQ, 32, 128, 128])
    sgconv_d = din("sgconv", [NSQ * 3, 8192])
    ssconv_d = din("ssconv", [NSQ * 2, D])
    f1_d = din("f1", [128, 1])
    vecs_d = din("vecs", [640, 128])
    hvec_d = din("hvec", [1, 72])
    ident_d = din("ident", [128, 128])
    masks_d = din("masks", [64, 4, 64])
    sel_d = din("sel", [8, 8, 128])
    w_ada_d = din("w_ada", [2, D, 6 * D])
    w_gin_d = din("gdn_w_in", [D, GIN])
    w_gout_d = din("gdn_w_out", [4096, D])
    w_scin_d = din("sc_w_in", [D, 3 * D])
    w_scout_d = din("sc_w_out", [D, D])
    w_fup_d = din("ffn_w_up", [D, 2 * FFN])
    w_fdn_d = din("ffn_w_down", [FFN, D])
    w_rt_d = din("moe_w_router", [D, NEXP])
    w_mup_d = din("moe_w_up", [NEXP, D, 2 * EXD])
    w_mdn_d = din("moe_w_down", [NEXP, EXD, D])

    yo_d = dout("yo", [NT, D])
    gdnp_d = dout("gdn_p", [32, 128, 128])
    gconvp_d = dout("gconv_p", [3, 8192])
    sconvp_d = dout("sconv_p", [2, D])
    gdns_d = dout("gdn_s", [NSQ, 32, 128, 128])
    gconvs_d = dout("gconv_s", [NSQ * 3, 8192])
    sconvs_d = dout("sconv_s", [NSQ * 2, D])

    sscr_d = nc.dram_tensor("sscr", [32, 128, 128], F32).ap()
    oscr_d = nc.dram_tensor("oscr", [32, 128, NT], BF16).ap()
    bscr = [P.buf("sscr") for _ in range(32)]
    boscr = [P.buf("oscr") for _ in range(32)]

    top = contextlib.ExitStack()
    uid = [0]

    def sb(shape, dt=F32, stack=None, name=None):
        uid[0] += 1
        nm = f"{name or 't'}{uid[0]}"
        t = (stack or top).enter_context(nc.sbuf_tensor(nm, list(shape), dt))
        return TT(t, P.buf(nm))

    banks = []
    for i in range(8):
        t = top.enter_context(nc.psum_tensor(f"psb{i}", [128, 512], F32))
        banks.append(TT(t, P.buf(f"psb{i}")))
    brr = [0, 0]

    def bank():
        b = banks[brr[0] % 5]
        brr[0] += 1
        return b

    def obank():
        b = banks[5 + brr[1] % 2]
        brr[1] += 1
        return b
    rbank = banks[7]

    rr = {"ev": 0, "cast": 0}

    def ev_eng():
        rr["ev"] += 1
        return "act" if rr["ev"] % 2 else "dve"

    def copy_op(eng, out, in_):
        if eng == "act":
            return lambda e: e.activation(out=out, in_=in_, func=AF.Copy)
        return lambda e: e.tensor_copy(out=out, in_=in_)

    ident = sb([128, 128], name="ident")
    P.add("sp", lambda e: e.dma_start(out=ident[:], in_=ident_d), writes=[ident.b], dma=True)
    masks = sb([64, 4, 64], name="masks")
    P.add("sp", lambda e: e.dma_start(out=masks[:], in_=masks_d), writes=[masks.b], dma=True)
    sel = sb([8, 8, 128], name="sel")
    P.add("sp", lambda e: e.dma_start(out=sel[:], in_=sel_d), writes=[sel.b], dma=True)
    f1 = sb([128, 1], name="f1")
    P.add("sp", lambda e: e.dma_start(out=f1[:], in_=f1_d), writes=[f1.b], dma=True)
    hvec = sb([128, 72], name="hvec")
    P.add("sp", lambda e: e.dma_start(out=hvec[:], in_=hvec_d.partition_broadcast(128)), writes=[hvec.b], dma=True)
    ones_f = sb([128, 128], name="ones_f")
    P.add("pool", lambda e: e.memset(ones_f[:], 1.0), writes=[ones_f.b])
    ones_b = sb([128, 128], BF16, name="ones_b")
    P.add("pool", lambda e: e.memset(ones_b[:], 1.0), writes=[ones_b.b])
    epst = sb([128, 1], name="eps")
    P.add("pool", lambda e: e.memset(epst[:], EPS), writes=[epst.b])

    vecT = sb([128, 640], name="vecT")
    mod = sb([128, 2, 96, NMOD], name="mod")
    gmod = sb([128, 2, 2, 16, NMOD], name="gmod")
    cT = sb([128, KC, NMOD], BF16, name="cT")
    HT = sb([128, KC, NT], BF16, name="HT")
    nexpa = sb([128, 32], name="nexpa")
    ba_w = sb([128, KC, 64], BF16, name="ba_w")
    tails = sb([128, 4, 16, 3], name="tails")

    NSTG = 2
    stg = [sb([128, 2048], name="stg") for _ in range(NSTG)]
    wbp = [sb([128, 2048], BF16, name="wb") for _ in range(NSTG)]
    wrr = [0]

    def stage_load(src_ap, kc, cw):
        i = wrr[0] % NSTG
        wrr[0] += 1
        s = stg[i]
        sv = s[:, 0:kc * cw].rearrange("p (k c) -> p k c", k=kc)
        P.add("sp", lambda e: e.dma_start(out=sv, in_=src_ap.rearrange("(k p) c -> p k c", p=128)),
              writes=[s.b], dma=True)
        return i, s, sv

    def cast_eng():
        rr["cast"] += 1
        return ("act", "dve", "pool")[rr["cast"] % 3]

    def load_w(src_ap, kc, cw):
        i, s, sv = stage_load(src_ap, kc, cw)
        w = wbp[i]
        wv = w[:, 0:kc * cw].rearrange("p (k c) -> p k c", k=kc)
        ce = cast_eng()
        P.add(ce, copy_op(ce, w[:, 0:kc * cw], s[:, 0:kc * cw]), reads=[s.b], writes=[w.b])
        return w, wv

    fin = contextlib.ExitStack()
    try:
        with scope() as sc:
            vs = sb([128, 5, 128], stack=sc, name="vs")
            P.add("sp", lambda e: e.dma_start(out=vs[:], in_=vecs_d.rearrange("(t p) c -> p t c", p=128)),
                  writes=[vs.b], dma=True)
            for half in range(2):
                bk = bank()
                nt_ = 4 if half == 0 else 1

                def tr(e, half=half, bk=bk, nt_=nt_):
                    for j in range(nt_):
                        r = e.transpose(bk[:, j * 128:(j + 1) * 128], vs[:, half * 4 + j, :], ident[:])
                    return r
                P.add("pe", tr, reads=[vs.b, ident.b], writes=[bk.b])
                P.add("dve", copy_op("dve", vecT[:, half * 512:half * 512 + nt_ * 128], bk[:, 0:nt_ * 128]),
                      reads=[bk.b], writes=[vecT.b], join=(half > 0))
            cs = sb([NMOD, D], stack=sc, name="cs")
            P.add("sp", lambda e: e.dma_start(out=cs[:], in_=cvec_d), writes=[cs.b], dma=True)
            P.add("act", lambda e: e.activation(out=cs[:], in_=cs[:], func=AF.Silu), reads=[cs.b], writes=[cs.b])
            for g4 in range(4):
                bk = bank()

                def tr(e, g4=g4, bk=bk):
                    for j in range(4):
                        k = g4 * 4 + j
                        r = e.transpose(bk[:, j * 32:j * 32 + NMOD], cs[:, k * 128:(k + 1) * 128], ident[0:NMOD, 0:NMOD])
                    return r
                P.add("pe", tr, reads=[cs.b, ident.b], writes=[bk.b])
                P.add("dve", copy_op("dve", cT[:, g4 * 4:(g4 + 1) * 4, :],
                                     bk[:, 0:128].rearrange("p (a b) -> p a b", a=4)[:, :, 0:NMOD]),
                      reads=[bk.b], writes=[cT.b], join=(g4 > 0))
            P.add("act", lambda e: e.activation(out=nexpa[:], in_=hvec[:, 0:32], func=AF.Exp),
                  reads=[hvec.b], writes=[nexpa.b])
            P.add("dve", lambda e: e.tensor_scalar(out=nexpa[:], in0=nexpa[:], scalar1=-1.0, scalar2=None, op0=ALU.mult),
                  reads=[nexpa.b], writes=[nexpa.b])
            _, s_, sv_ = stage_load(w_gin_d[:, 12288:12352], KC, 64)
            P.add("dve", copy_op("dve", ba_w[:], sv_), reads=[s_.b], writes=[ba_w.b])
            for l in range(2):
                for cc in range(96):
                    w, wv = load_w(w_ada_d[l, :, cc * 128:(cc + 1) * 128], KC, 128)
                    bk = bank()

                    def mm(e, wv=wv, bk=bk):
                        for k in range(KC):
                            r = e.matmul(bk[:, 0:NMOD], lhsT=wv[:, k, :], rhs=cT[:, k, :], start=(k == 0), stop=(k == KC - 1))
                        return r
                    P.add("pe", mm, reads=[w.b, cT.b], writes=[bk.b])
                    P.add("dve", lambda e, bk=bk, l=l, cc=cc: e.tensor_scalar(
                        out=mod[:, l, cc, :], in0=bk[:, 0:NMOD],
                        scalar1=vecT[:, l * 96 + cc:l * 96 + cc + 1], scalar2=None, op0=ALU.add),
                        reads=[bk.b, vecT.b], writes=[mod.b], join=True)
            for l in range(2):
                for s in range(2):
                    gcol = 192 + s * 32 + l * 16
                    for k in range(KC):
                        P.add("dve", lambda e, l=l, s=s, k=k, gcol=gcol: e.tensor_scalar(
                            out=gmod[:, l, s, k, :], in0=mod[:, l, (1 + 3 * s) * 16 + k, :],
                            scalar1=1.0, scalar2=vecT[:, gcol + k:gcol + k + 1], op0=ALU.add, op1=ALU.mult),
                            reads=[mod.b, vecT.b], writes=[gmod.b], join=True)
        P.fence()
        dump(vecT, 'vecT', [128, 640])
        dump(mod, 'mod', [128, 2, 96, NMOD])
        dump(gmod, 'gmod', [128, 2, 2, 16, NMOD])
        dump(cT, 'cT', [128, KC, NMOD], BF16)
        stop_at('setup')

        def load_xT(XT, src_d, ntok, stack):
            xs = [sb([128, D], stack=stack, name="xs") for _ in range(2)]
            nt_ = (ntok + 127) // 128
            for t in range(nt_):
                r0 = t * 128
                rows = min(128, ntok - r0)
                s = xs[t % 2]
                P.add("sp", lambda e, s=s, r0=r0, rows=rows: e.dma_start(out=s[0:rows, :], in_=src_d[r0:r0 + rows, :]),
                      writes=[s.b], dma=True)
                for g4 in range(4):
                    bk = bank()

                    def tr(e, s=s, bk=bk, g4=g4, rows=rows):
                        for j in range(4):
                            k = g4 * 4 + j
                            r = e.transpose(bk[:, j * 128:j * 128 + rows], s[0:rows, k * 128:(k + 1) * 128],
                                            ident[0:rows, 0:rows])
                        return r
                    P.add("pe", tr, reads=[s.b, ident.b], writes=[bk.b])
                    en = ev_eng()
                    P.add(en, copy_op(en, XT[:, g4 * 4:(g4 + 1) * 4, r0:r0 + rows],
                                      bk[:, 0:512].rearrange("p (a b) -> p a b", a=4)[:, :, 0:rows]),
                          reads=[bk.b], writes=[XT.b], join=True)

        def tok_tiles(n):
            res = []
            t0 = 0
            while t0 < n:
                res.append((t0, min(512, n - t0)))
                t0 += 512
            return res

        def rstd_bc(XT, ntok, stack, out_rs):
            sq = [sb([128, 512], BF16, stack=stack, name="sq") for _ in range(2)]
            for (t0, tn) in tok_tiles(ntok):
                bk = bank()
                for k in range(KC):
                    s = sq[k % 2]
                    if k % 2 == 0:
                        P.add("act", lambda e, s=s, k=k, t0=t0, tn=tn: e.activation(
                            out=s[:, 0:tn], in_=XT[:, k, t0:t0 + tn], func=AF.Square), reads=[XT.b], writes=[s.b])
                    else:
                        P.add("pool", lambda e, s=s, k=k, t0=t0, tn=tn: e.tensor_tensor(
                            out=s[:, 0:tn], in0=XT[:, k, t0:t0 + tn], in1=XT[:, k, t0:t0 + tn], op=ALU.mult),
                            reads=[XT.b], writes=[s.b])
                    P.add("pe", lambda e, s=s, k=k, bk=bk, tn=tn: e.matmul(
                        bk[:, 0:tn], lhsT=ones_b[:], rhs=s[:, 0:tn], start=(k == 0), stop=(k == KC - 1)),
                        reads=[s.b, ones_b.b], writes=[bk.b], join=(k > 0))
                P.add("act", lambda e, bk=bk, t0=t0, tn=tn: e.activation(
                    out=out_rs[:, t0:t0 + tn], in_=bk[:, 0:tn], func=AF.Sqrt, scale=1.0 / D, bias=epst[:, 0:1]),
                    reads=[bk.b, epst.b], writes=[out_rs.b], join=True)
            P.add("dve", lambda e: e.reciprocal(out=out_rs[:, 0:ntok], in_=out_rs[:, 0:ntok]),
                  reads=[out_rs.b], writes=[out_rs.b])

        def norm_mod(XT, npr, nsm, l, s, stack, router=None):
            ntok = npr + nsm
            rs = sb([128, NT], stack=stack, name="rs")
            rstd_bc(XT, ntok, stack, rs)
            tmp = [sb([128, NT], stack=stack, name="ntmp") for _ in range(2)]
            shb = (0 + 3 * s) * 16
            ns = nsm // 4
            for k in range(KC):
                tm = tmp[k % 2]
                P.add("dve", lambda e, tm=tm, k=k: e.tensor_tensor(
                    out=tm[:, 0:ntok], in0=XT[:, k, 0:ntok], in1=rs[:, 0:ntok], op=ALU.mult),
                    reads=[XT.b, rs.b], writes=[tm.b])
                P.add("pool", lambda e, tm=tm, k=k: e.tensor_scalar(
                    out=tm[:, 0:npr], in0=tm[:, 0:npr],
                    scalar1=gmod[:, l, s, k, 0:1], scalar2=mod[:, l, shb + k, 0:1], op0=ALU.mult, op1=ALU.add),
                    reads=[tm.b, gmod.b, mod.b], writes=[tm.b])
                if nsm:
                    P.add("dve", lambda e, tm=tm, k=k: e.tensor_tensor(
                        out=tm[:, npr:ntok].rearrange("p (s t) -> p s t", t=4),
                        in0=tm[:, npr:ntok].rearrange("p (s t) -> p s t", t=4),
                        in1=gmod[:, l, s, k, 1:1 + ns].unsqueeze(2).to_broadcast([128, ns, 4]), op=ALU.mult),
                        reads=[tm.b, gmod.b], writes=[tm.b])
                    P.add("dve", lambda e, tm=tm, k=k: e.tensor_tensor(
                        out=tm[:, npr:ntok].rearrange("p (s t) -> p s t", t=4),
                        in0=tm[:, npr:ntok].rearrange("p (s t) -> p s t", t=4),
                        in1=mod[:, l, shb + k, 1:1 + ns].unsqueeze(2).to_broadcast([128, ns, 4]), op=ALU.add),
                        reads=[tm.b, mod.b], writes=[tm.b])
                P.add("act", lambda e, tm=tm, k=k: e.activation(out=HT[:, k, 0:ntok], in_=tm[:, 0:ntok], func=AF.Copy),
                      reads=[tm.b], writes=[HT.b], join=True)
                if router is not None:
                    wrt = router

                    def rmm(e, tm=tm, k=k):
                        for t in range(9):
                            tn = min(128, ntok - t * 128)
                            r = e.matmul(rbank[0:tn, t * 8:(t + 1) * 8], lhsT=tm[:, t * 128:t * 128 + tn],
                                         rhs=wrt[:, k, :], start=(k == 0 and t == 0), stop=(k == KC - 1),
                                         skip_group_check=True)
                        return r
                    P.add("pe", rmm, reads=[tm.b, wrt.b], writes=[rbank.b], join=(k > 0))

        def linear(w_d, col0, ncc, kc, rhs_fn, tiles, epilogue, rhs_bufs):
            cw = 256 if kc * 256 <= 2048 else 128
            per = cw // 128
            cc = 0
            while cc < ncc:
                np_ = min(per, ncc - cc)
                w, wv = load_w(w_d[:, col0 + cc * 128: col0 + (cc + np_) * 128], kc, np_ * 128)
                for j in range(np_):
                    for (t0, tn) in tiles:
                        bk = bank()

                        def mm(e, wv=wv, j=j, t0=t0, tn=tn, bk=bk):
                            for k in range(kc):
                                r = e.matmul(bk[:, 0:tn], lhsT=wv[:, k, j * 128:(j + 1) * 128], rhs=rhs_fn(k, t0, tn),
                                             start=(k == 0), stop=(k == kc - 1))
                            return r
                        P.add("pe", mm, reads=[w.b] + rhs_bufs, writes=[bk.b])
                        epilogue(cc + j, t0, tn, bk)
                cc += np_

        def gdn_pass(stack, blocks, first, last):
            wgrp = sb([128, 6, KC, 128], BF16, stack=stack, name="wgrp")
            totch = sum(b["nch"] for b in blocks)
            BA = sb([64, 8, 64], stack=stack, name="BA")
            gtmp_ = sb([64, 16, 32], stack=stack, name="gtmp")
            pre = {nm: sb([64, totch, 32], stack=stack, name=nm) for nm in ("beta", "g", "esuf", "nbeg", "nbeta")}
            pc = 0
            for blk in blocks:
                C, nch = blk["C"], blk["nch"]
                blk["pc0"] = pc
                for c8 in range(0, nch, 8):
                    bk = bank()

                    def mm(e, C=C, c8=c8, bk=bk, blk=blk):
                        for c in range(8):
                            t0 = blk["tok0"] + (c8 + c) * C
                            for k in range(KC):
                                r = e.matmul(bk[0:C, c * 64:(c + 1) * 64], lhsT=HT[:, k, t0:t0 + C], rhs=ba_w[:, k, :],
                                             start=(k == 0), stop=(k == KC - 1))
                        return r
                    P.add("pe", mm, reads=[HT.b, ba_w.b], writes=[bk.b])
                    P.add("dve", copy_op("dve", BA[0:C, :, :], bk[0:C, 0:512].rearrange("p (a b) -> p a b", a=8)),
                          reads=[bk.b], writes=[BA.b])
                    sl = slice(pc + c8, pc + c8 + 8)
                    be, g_, es, nbg, nbe = (pre[k_][0:C, sl, :] for k_ in ("beta", "g", "esuf", "nbeg", "nbeta"))
                    P.add("act", lambda e, be=be, C=C: e.activation(out=be, in_=BA[0:C, :, 0:32], func=AF.Sigmoid),
                          reads=[BA.b], writes=[pre["beta"].b], join=True)
                    P.add("dve", lambda e, g_=g_, C=C: e.tensor_tensor(
                        out=g_, in0=BA[0:C, :, 32:64], in1=hvec[0:C, 32:64].unsqueeze(1).to_broadcast([C, 8, 32]),
                        op=ALU.add), reads=[BA.b, hvec.b], writes=[pre["g"].b], join=True)
                    P.add("act", lambda e, g_=g_: e.activation(out=g_, in_=g_, func=AF.Exp),
                          reads=[pre["g"].b], writes=[pre["g"].b])
                    P.add("act", lambda e, g_=g_: e.activation(out=g_, in_=g_, func=AF.Ln, bias=1.0),
                          reads=[pre["g"].b], writes=[pre["g"].b])
                    P.add("dve", lambda e, g_=g_, C=C: e.tensor_tensor(
                        out=g_, in0=g_, in1=nexpa[0:C, :].unsqueeze(1).to_broadcast([C, 8, 32]), op=ALU.mult),
                        reads=[pre["g"].b, nexpa.b], writes=[pre["g"].b])
                    P.add("dve", lambda e, nbe=nbe, be=be: e.tensor_scalar(out=nbe, in0=be, scalar1=-1.0, scalar2=None, op0=ALU.mult),
                          reads=[pre["beta"].b], writes=[pre["nbeta"].b], join=True)
                    gflat = pre["g"][0:C, :, :].rearrange("p a b -> p (a b)")[:, (pc + c8) * 32:(pc + c8 + 8) * 32]
                    bk1 = bank()
                    P.add("pe", lambda e, bk1=bk1, C=C, gflat=gflat: e.matmul(
                        bk1[0:C, 0:256], lhsT=masks[0:C, 0, 0:C], rhs=gflat, start=True, stop=True),
                        reads=[masks.b, pre["g"].b], writes=[bk1.b])
                    P.add("act", lambda e, bk1=bk1, C=C: e.activation(
                        out=gtmp_[0:C, 0:8, :].rearrange("p a b -> p (a b)"), in_=bk1[0:C, 0:256], func=AF.Exp),
                        reads=[bk1.b], writes=[gtmp_.b])
                    P.add("dve", lambda e, nbg=nbg, nbe=nbe, C=C: e.tensor_tensor(out=nbg, in0=nbe, in1=gtmp_[0:C, 0:8, :], op=ALU.mult),
                          reads=[pre["nbeta"].b, gtmp_.b], writes=[pre["nbeg"].b], join=True)
                    bk2 = bank()
                    P.add("pe", lambda e, bk2=bk2, C=C, gflat=gflat: e.matmul(
                        bk2[0:C, 0:256], lhsT=masks[0:C, 1, 0:C], rhs=gflat, start=True, stop=True),
                        reads=[masks.b, pre["g"].b], writes=[bk2.b])
                    P.add("act", lambda e, bk2=bk2, C=C, es=es: e.activation(
                        out=es, in_=bk2[0:C, 0:256].rearrange("p (a b) -> p a b", a=8), func=AF.Exp),
                        reads=[bk2.b], writes=[pre["esuf"].b], join=True)
                pc += nch

            QKVZ = sb([128, 6, 512], stack=stack, name="QKVZ")
            Fb = sb([128, 515], stack=stack, name="F")
            cacc = sb([128, 512], stack=stack, name="cacc")
            sqt = sb([128, 512], stack=stack, name="sqt")
            S = [sb([128, 128], stack=stack, name="S") for _ in range(4)]
            og = sb([128, 2, 512], BF16, stack=stack, name="og")
            sgc = sb([48, 512], stack=stack, name="sgc")
            sgo = sb([48, 512], stack=stack, name="sgo")
            tl3 = sb([128, 4, 48], stack=stack, name="tl3")
            WM = 512

            def t2(nm):
                return sb([64, WM], stack=stack, name=nm)
            d = dict(rgt=t2("rgt"), rle=t2("rle"), draw=t2("draw"), dtril=t2("dtril"), dstr=t2("dstr"),
                     p0=t2("p0"), pt0=t2("pt0"), rt=t2("rt"), a=t2("a"),
                     egcb=sb([128, WM], stack=stack, name="egcb"), qd=sb([128, WM], BF16, stack=stack, name="qd"),
                     wt=sb([128, WM], BF16, stack=stack, name="wt"),
                     vtok=sb([64, 1024], BF16, stack=stack, name="vtok"), ktok=sb([64, 512], BF16, stack=stack, name="ktok"),
                     kd=sb([64, 1024], BF16, stack=stack, name="kd"), otok=sb([64, 1024], stack=stack, name="otok"),
                     osq=sb([64, 1024], stack=stack, name="osq"),
                     tb=sb([64, WM], BF16, stack=stack, name="tb"), tg=sb([64, WM], BF16, stack=stack, name="tg"),
                     at=sb([64, WM], BF16, stack=stack, name="at"),
                     ors=sb([64, 8], stack=stack, name="ors"),
                     vn=[sb([64, 128], BF16, stack=stack, name="vn") for _ in range(4)])
            Sb = [sb([128, 128], BF16, stack=stack, name="Sb") for _ in range(4)]
            d["p1"] = d["dstr"]
            d["pt1"] = d["dtril"]
            nprompt = len([b for b in blocks if not b["sample"]])

            for g in range(16):
                cols = [g * 128, 2048 + g * 128, 4096 + 2 * g * 128, 4096 + (2 * g + 1) * 128,
                        8192 + 2 * g * 128, 8192 + (2 * g + 1) * 128]
                anyqz = any(b["qz"] for b in blocks)
                for j in range(6):
                    if j in (0, 4, 5) and not anyqz:
                        continue
                    _, s_, sv_ = stage_load(w_gin_d[:, cols[j]:cols[j] + 128], KC, 128)
                    ce = cast_eng()
                    P.add(ce, copy_op(ce, wgrp[:, j, :, :], sv_), reads=[s_.b], writes=[wgrp.b], join=True)

                for bi, blk in enumerate(blocks):
                    C, nch, nseq, T = blk["C"], blk["nch"], blk["nseq"], blk["T"]
                    NB = nseq * T
                    tok0 = blk["tok0"]
                    sample = blk["sample"]
                    pc0 = blk["pc0"]
                    if sample:
                        for j in range(4):
                            P.add("sp", lambda e, j=j, c0=cols[j]: e.dma_start(
                                out=sgc[:, j * 128:(j + 1) * 128], in_=sgconv_d[:, c0:c0 + 128]),
                                writes=[sgc.b], dma=True, join=(j > 0))
                        bk = bank()

                        def tr(e, bk=bk):
                            for j in range(4):
                                r = e.transpose(bk[:, j * 48:(j + 1) * 48], sgc[:, j * 128:(j + 1) * 128], ident[0:48, 0:48])
                            return r
                        P.add("pe", tr, reads=[sgc.b, ident.b], writes=[bk.b])
                        P.add("dve", copy_op("dve", tl3[:], bk[:, 0:192].rearrange("p (a b) -> p a b", a=4)),
                              reads=[bk.b], writes=[tl3.b])
                    for j in range(6):
                        if j in (0, 4, 5) and not blk["qz"]:
                            continue
                        bk = bank()

                        def mm(e, j=j, bk=bk, tok0=tok0, NB=NB):
                            for k in range(KC):
                                r = e.matmul(bk[:, 0:NB], lhsT=wgrp[:, j, k, :], rhs=HT[:, k, tok0:tok0 + NB],
                                             start=(k == 0), stop=(k == KC - 1))
                            return r
                        P.add("pe", mm, reads=[wgrp.b, HT.b], writes=[bk.b])
                        if j >= 4:
                            P.add("act", lambda e, j=j, bk=bk, NB=NB: e.activation(
                                out=QKVZ[:, j, 0:NB], in_=bk[:, 0:NB], func=AF.Silu), reads=[bk.b], writes=[QKVZ.b], join=True)
                            continue
                        F = Fb
                        Fv = F[:, 0:nseq * (T + 3)].rearrange("p (s t) -> p s t", s=nseq)
                        P.add("act", lambda e, Fv=Fv, bk=bk, NB=NB, nseq=nseq: e.activation(
                            out=Fv[:, :, 3:], in_=bk[:, 0:NB].rearrange("p (s t) -> p s t", s=nseq), func=AF.Copy),
                            reads=[bk.b], writes=[F.b])
                        if sample:
                            P.add("pool", lambda e, Fv=Fv, j=j: e.tensor_copy(
                                out=Fv[:, :, 0:3], in_=tl3[:, j, :].rearrange("p (s r) -> p s r", r=3)),
                                reads=[tl3.b], writes=[F.b], join=True)
                        elif first and bi == 0:
                            P.add("pool", lambda e, Fv=Fv: e.memset(Fv[:, 0, 0:3], 0.0), writes=[F.b], join=True)
                        elif (not first) and bi == 0:
                            P.add("pool", lambda e, Fv=Fv, j=j, g=g: e.tensor_scalar(
                                out=Fv[:, 0, 0:3], in0=tails[:, j, g, :], scalar1=f1[:, 0:1], scalar2=None, op0=ALU.mult),
                                reads=[tails.b, f1.b], writes=[F.b], join=True)
                        else:
                            P.add("pool", lambda e, Fv=Fv, j=j, g=g: e.tensor_copy(out=Fv[:, 0, 0:3], in_=tails[:, j, g, :]),
                                  reads=[tails.b], writes=[F.b], join=True)
                        ca = cacc
                        cav = ca[:, 0:NB].rearrange("p (s t) -> p s t", s=nseq)
                        wcol = 272 + cols[j] // 128
                        en = "dve"
                        P.add(en, lambda e, cav=cav, Fv=Fv, wcol=wcol, T=T: e.tensor_scalar(
                            out=cav, in0=Fv[:, :, 0:T], scalar1=vecT[:, wcol:wcol + 1], scalar2=None, op0=ALU.mult),
                            reads=[F.b, vecT.b], writes=[ca.b])
                        for tp in range(1, 4):
                            P.add(en, lambda e, cav=cav, Fv=Fv, wcol=wcol, T=T, tp=tp: e.scalar_tensor_tensor(
                                out=cav, in0=Fv[:, :, tp:tp + T], scalar=vecT[:, wcol + 64 * tp:wcol + 64 * tp + 1],
                                in1=cav, op0=ALU.mult, op1=ALU.add), reads=[F.b, vecT.b, ca.b], writes=[ca.b])
                        if sample:
                            P.add("pool", lambda e, Fv=Fv, j=j: e.tensor_copy(
                                out=tl3[:, j, :].rearrange("p (s r) -> p s r", r=3), in_=Fv[:, :, 4:7]),
                                reads=[F.b], writes=[tl3.b])
                        else:
                            P.add("pool", lambda e, Fv=Fv, j=j, g=g, T=T: e.tensor_copy(
                                out=tails[:, j, g, :], in_=Fv[:, 0, T:T + 3]), reads=[F.b], writes=[tails.b])
                        if j >= 2:
                            P.add("act", lambda e, j=j, ca=ca, NB=NB: e.activation(
                                out=QKVZ[:, j, 0:NB], in_=ca[:, 0:NB], func=AF.Silu), reads=[ca.b], writes=[QKVZ.b], join=True)
                        else:
                            P.add("act", lambda e, ca=ca, NB=NB: e.activation(out=ca[:, 0:NB], in_=ca[:, 0:NB], func=AF.Silu),
                                  reads=[ca.b], writes=[ca.b])
                            P.add("act", lambda e, ca=ca, NB=NB: e.activation(
                                out=sqt[:, 0:NB], in_=ca[:, 0:NB], func=AF.Square), reads=[ca.b], writes=[sqt.b])
                            bk2 = bank()
                            P.add("pe", lambda e, bk2=bk2, NB=NB: e.matmul(
                                bk2[:, 0:NB], lhsT=ones_f[:], rhs=sqt[:, 0:NB], start=True, stop=True),
                                reads=[sqt.b, ones_f.b], writes=[bk2.b])
                            P.add("act", lambda e, bk2=bk2, NB=NB: e.activation(
                                out=sqt[:, 0:NB], in_=bk2[:, 0:NB], func=AF.Sqrt, bias=epst[:, 0:1]),
                                reads=[bk2.b, epst.b], writes=[sqt.b])
                            P.add("dve", lambda e, NB=NB: e.reciprocal(out=sqt[:, 0:NB], in_=sqt[:, 0:NB]),
                                  reads=[sqt.b], writes=[sqt.b])
                            sc_ = (128.0 ** -0.5) if j == 0 else 1.0
                            P.add("dve", lambda e, j=j, ca=ca, NB=NB, sc_=sc_: e.scalar_tensor_tensor(
                                out=QKVZ[:, j, 0:NB], in0=ca[:, 0:NB], scalar=sc_, in1=sqt[:, 0:NB],
                                op0=ALU.mult, op1=ALU.mult), reads=[ca.b, sqt.b], writes=[QKVZ.b], join=True)
                    if sample or (last and bi == nprompt - 1):
                        nr = 48 if sample else 3
                        bk = bank()

                        def tr(e, bk=bk, nr=nr, sample=sample, g=g):
                            for j in range(4):
                                src = tl3[:, j, :] if sample else tails[:, j, g, :]
                                r = e.transpose(bk[0:nr, j * 128:(j + 1) * 128], src, ident[:])
                            return r
                        P.add("pe", tr, reads=[tl3.b if sample else tails.b, ident.b], writes=[bk.b])
                        P.add("dve", copy_op("dve", sgo[0:nr, :], bk[0:nr, :]), reads=[bk.b], writes=[sgo.b])
                        dd = gconvs_d if sample else gconvp_d
                        for j in range(4):
                            P.add("pool", lambda e, j=j, nr=nr, dd=dd, c0=cols[j]: e.dma_start(
                                out=dd[:, c0:c0 + 128], in_=sgo[0:nr, j * 128:(j + 1) * 128]),
                                reads=[sgo.b], dma=True, out=True)

                    n = 4
                    for c0 in range(0, nch, n):
                        need_o = blk["o_from"] is not None and c0 >= blk["o_from"]
                        W = n * 2 * C
                        HC = n * 2
                        st0 = c0 * C
                        Mle = masks[0:C, 0, 0:C]
                        Mgt = masks[0:C, 1, 0:C]
                        Mtril = masks[0:C, 2, 0:C]
                        Mstr = masks[0:C, 3, 0:C]
                        psl = slice(pc0 + c0, pc0 + c0 + n)
                        hsl = slice(2 * g, 2 * g + 2)
                        gsl = pre["g"][0:C, psl, hsl]

                        def v4(t, C=C, W=W):
                            return t[0:C, 0:W].rearrange("p (a h c) -> p a h c", a=n, h=2)

                        def v3(t, C=C, W=W, HC=HC):
                            return t[0:C, 0:W].rearrange("p (a c) -> p a c", a=HC)

                        def f2(t, C=C, W=W):
                            return t[0:C, 0:W]

                        def bc_hc(ap2, C=C):
                            return ap2.unsqueeze(3).to_broadcast([C, n, 2, C])

                        def bc_m(m, C=C):
                            return m.unsqueeze(1).unsqueeze(1).to_broadcast([C, n, 2, C])
                        P.add("dve", lambda e, v4=v4, bc_hc=bc_hc, bc_m=bc_m, gsl=gsl, Mgt=Mgt: e.tensor_tensor(
                            out=v4(d["rgt"]), in0=bc_m(Mgt), in1=bc_hc(gsl), op=ALU.mult),
                            reads=[masks.b, pre["g"].b], writes=[d["rgt"].b])
                        P.add("pool", lambda e, v4=v4, bc_hc=bc_hc, bc_m=bc_m, gsl=gsl, Mle=Mle: e.tensor_tensor(
                            out=v4(d["rle"]), in0=bc_m(Mle), in1=bc_hc(gsl), op=ALU.mult),
                            reads=[masks.b, pre["g"].b], writes=[d["rle"].b])
                        bkG = bank()
                        P.add("pe", lambda e, bkG=bkG, Mle=Mle, W=W, C=C, f2=f2: e.matmul(
                            bkG[0:C, 0:W], lhsT=Mle, rhs=f2(d["rgt"]), start=True, stop=True),
                            reads=[masks.b, d["rgt"].b], writes=[bkG.b])
                        P.add("act", lambda e, bkG=bkG, W=W, C=C, f2=f2: e.activation(
                            out=f2(d["draw"]), in_=bkG[0:C, 0:W], func=AF.Exp), reads=[bkG.b], writes=[d["draw"].b])
                        P.add("pool", lambda e, v4=v4, bc_m=bc_m, Mtril=Mtril: e.tensor_tensor(
                            out=v4(d["dtril"]), in0=v4(d["draw"]), in1=bc_m(Mtril), op=ALU.mult),
                            reads=[d["draw"].b, masks.b], writes=[d["dtril"].b])
                        P.add("dve", lambda e, v4=v4, bc_m=bc_m, Mstr=Mstr: e.tensor_tensor(
                            out=v4(d["dstr"]), in0=v4(d["draw"]), in1=bc_m(Mstr), op=ALU.mult),
                            reads=[d["draw"].b, masks.b], writes=[d["dstr"].b])
                        bkE = bank()
                        P.add("pe", lambda e, bkE=bkE, W=W, C=C, f2=f2: e.matmul(
                            bkE[:, 0:W], lhsT=ones_f[0:C, :], rhs=f2(d["rle"]), start=True, stop=True),
                            reads=[ones_f.b, d["rle"].b], writes=[bkE.b])
                        P.add("act", lambda e, bkE=bkE, W=W: e.activation(
                            out=d["egcb"][:, 0:W], in_=bkE[:, 0:W], func=AF.Exp), reads=[bkE.b], writes=[d["egcb"].b])
                        qsl = QKVZ[:, 0, st0:st0 + n * C].rearrange("p (a c) -> p a c", a=n)
                        ksl = QKVZ[:, 1, st0:st0 + n * C].rearrange("p (a c) -> p a c", a=n)
                        if need_o:
                            P.add("dve", lambda e, qsl=qsl, W=W, C=C: e.tensor_tensor(
                                out=d["qd"][:, 0:W].rearrange("p (a h c) -> p a h c", a=n, h=2),
                                in0=qsl.unsqueeze(2).to_broadcast([128, n, 2, C]),
                                in1=d["egcb"][:, 0:W].rearrange("p (a h c) -> p a h c", a=n, h=2), op=ALU.mult),
                                reads=[QKVZ.b, d["egcb"].b], writes=[d["qd"].b])
                        bkK = bank()

                        def mmk(e, bkK=bkK, qsl=qsl, ksl=ksl, C=C, need_o=need_o):
                            for c in range(n):
                                r = e.matmul(bkK[0:C, (2 * c) * C:(2 * c + 1) * C], lhsT=ksl[:, c, :], rhs=ksl[:, c, :],
                                             start=True, stop=True)
                                if need_o:
                                    r = e.matmul(bkK[0:C, (2 * c + 1) * C:(2 * c + 2) * C], lhsT=qsl[:, c, :], rhs=ksl[:, c, :],
                                                 start=True, stop=True)
                            return r
                        P.add("pe", mmk, reads=[QKVZ.b], writes=[bkK.b])
                        kkv = bkK[0:C, 0:W].rearrange("p (a h c) -> p a h c", a=n, h=2)
                        P.add("dve", lambda e, v4=v4, kkv=kkv, C=C: e.tensor_tensor(
                            out=v4(d["p0"]), in0=v4(d["dstr"]),
                            in1=kkv[:, :, 0:1, :].to_broadcast([C, n, 2, C]), op=ALU.mult),
                            reads=[d["dstr"].b, bkK.b], writes=[d["p0"].b])
                        nbs = pre["nbeta"][0:C, psl, hsl]
                        P.add("dve", lambda e, v4=v4, bc_hc=bc_hc, nbs=nbs: e.tensor_tensor(
                            out=v4(d["p0"]), in0=v4(d["p0"]), in1=bc_hc(nbs), op=ALU.mult),
                            reads=[d["p0"].b, pre["nbeta"].b], writes=[d["p0"].b])
                        if need_o:
                            P.add("dve", lambda e, v4=v4, kkv=kkv, C=C: e.tensor_tensor(
                                out=v4(d["a"]), in0=v4(d["dtril"]),
                                in1=kkv[:, :, 1:2, :].to_broadcast([C, n, 2, C]), op=ALU.mult),
                                reads=[d["dtril"].b, bkK.b], writes=[d["a"].b])

                        def transpose_hc(src, dst, eng_ev, C=C, W=W, HC=HC):
                            bkT = bank()

                            def tr(e, src=src, bkT=bkT):
                                for hc in range(HC):
                                    r = e.transpose(bkT[0:C, hc * C:(hc + 1) * C], src[0:C, hc * C:(hc + 1) * C], ident[0:C, 0:C])
                                return r
                            P.add("pe", tr, reads=[src.b, ident.b], writes=[bkT.b])
                            P.add(eng_ev, copy_op(eng_ev, dst[0:C, 0:W], bkT[0:C, 0:W]), reads=[bkT.b], writes=[dst.b])
                        transpose_hc(d["p0"], d["pt0"], "act")
                        if need_o:
                            transpose_hc(d["a"], d["at"], "act")
                        P.add("dve", lambda e, v3=v3, HC=HC, C=C: e.tensor_tensor(
                            out=v3(d["rt"]), in0=v3(d["pt0"]),
                            in1=ident[0:C, 0:C].unsqueeze(1).to_broadcast([C, HC, C]), op=ALU.add),
                            reads=[d["pt0"].b, ident.b], writes=[d["rt"].b])
                        nsteps = {64: 5, 4: 1}[C]
                        cur = 0
                        for stp in range(nsteps):
                            Pc, PTc = d["p%d" % cur], d["pt%d" % cur]
                            Pn, PTn = d["p%d" % (1 - cur)], d["pt%d" % (1 - cur)]
                            lastst = (stp == nsteps - 1)
                            bkP = bank()

                            def mmp(e, Pc=Pc, PTc=PTc, bkP=bkP, C=C, HC=HC):
                                for hc in range(HC):
                                    sl = slice(hc * C, (hc + 1) * C)
                                    r = e.matmul(bkP[0:C, sl], lhsT=PTc[0:C, sl], rhs=Pc[0:C, sl], start=True, stop=True)
                                return r
                            P.add("pe", mmp, reads=[Pc.b, PTc.b], writes=[bkP.b])
                            if not lastst:
                                bkQ = bank()

                                def mmq(e, Pc=Pc, PTc=PTc, bkQ=bkQ, C=C, HC=HC):
                                    for hc in range(HC):
                                        sl = slice(hc * C, (hc + 1) * C)
                                        r = e.matmul(bkQ[0:C, sl], lhsT=Pc[0:C, sl], rhs=PTc[0:C, sl], start=True, stop=True)
                                    return r
                                P.add("pe", mmq, reads=[Pc.b, PTc.b], writes=[bkQ.b])
                            P.add("act", copy_op("act", Pn[0:C, 0:W], bkP[0:C, 0:W]), reads=[bkP.b], writes=[Pn.b])
                            if not lastst:
                                P.add("dve", copy_op("dve", PTn[0:C, 0:W], bkQ[0:C, 0:W]), reads=[bkQ.b], writes=[PTn.b])
                            bkR = bank()

                            def mmr(e, Pn=Pn, bkR=bkR, C=C, HC=HC):
                                for hc in range(HC):
                                    sl = slice(hc * C, (hc + 1) * C)
                                    r = e.matmul(bkR[0:C, sl], lhsT=Pn[0:C, sl], rhs=d["rt"][0:C, sl], start=True, stop=True)
                                return r
                            P.add("pe", mmr, reads=[Pn.b, d["rt"].b], writes=[bkR.b])
                            P.add("dve", lambda e, bkR=bkR, W=W, C=C, f2=f2: e.tensor_tensor(
                                out=f2(d["rt"]), in0=f2(d["rt"]), in1=bkR[0:C, 0:W], op=ALU.add),
                                reads=[d["rt"].b, bkR.b], writes=[d["rt"].b])
                            cur = 1 - cur
                        bsl = pre["beta"][0:C, psl, hsl]
                        ngs = pre["nbeg"][0:C, psl, hsl]
                        P.add("pool", lambda e, v4=v4, bc_hc=bc_hc, bsl=bsl: e.tensor_tensor(
                            out=v4(d["tb"]), in0=v4(d["rt"]), in1=bc_hc(bsl), op=ALU.mult),
                            reads=[d["rt"].b, pre["beta"].b], writes=[d["tb"].b])
                        P.add("dve", lambda e, v4=v4, bc_hc=bc_hc, ngs=ngs: e.tensor_tensor(
                            out=v4(d["tg"]), in0=v4(d["rt"]), in1=bc_hc(ngs), op=ALU.mult),
                            reads=[d["rt"].b, pre["nbeg"].b], writes=[d["tg"].b])
                        for q4 in range(0, HC, 4):
                            bkV = bank()

                            def trv(e, bkV=bkV, q4=q4, C=C, st0=st0):
                                for x in range(4):
                                    hc = q4 + x
                                    c, h = hc // 2, hc % 2
                                    r = e.transpose(bkV[0:C, x * 128:(x + 1) * 128],
                                                    QKVZ[:, 2 + h, st0 + c * C:st0 + (c + 1) * C], ident[:])
                                return r
                            P.add("pe", trv, reads=[QKVZ.b, ident.b], writes=[bkV.b])
                            en = ev_eng()
                            P.add(en, copy_op(en, d["vtok"][0:C, q4 * 128:(q4 + 4) * 128], bkV[0:C, 0:512]),
                                  reads=[bkV.b], writes=[d["vtok"].b], join=(q4 > 0))
                        bkV = bank()

                        def trk(e, bkV=bkV, C=C, st0=st0):
                            for c in range(4):
                                r = e.transpose(bkV[0:C, c * 128:(c + 1) * 128],
                                                QKVZ[:, 1, st0 + c * C:st0 + (c + 1) * C], ident[:])
                            return r
                        P.add("pe", trk, reads=[QKVZ.b, ident.b], writes=[bkV.b])
                        en = ev_eng()
                        P.add(en, copy_op(en, d["ktok"][0:C, 0:512], bkV[0:C, 0:512]), reads=[bkV.b], writes=[d["ktok"].b])
                        ess = pre["esuf"][0:C, psl, hsl]
                        P.add("pool", lambda e, ess=ess, C=C: e.tensor_tensor(
                            out=d["kd"][0:C, :].rearrange("p (a h c) -> p a h c", a=n, h=2),
                            in0=d["ktok"][0:C, :].rearrange("p (a c) -> p a c", a=n).unsqueeze(2).to_broadcast([C, n, 2, 128]),
                            in1=ess.unsqueeze(3).to_broadcast([C, n, 2, 128]), op=ALU.mult),
                            reads=[d["ktok"].b, pre["esuf"].b], writes=[d["kd"].b])
                        bkW = bank()

                        def mmw(e, bkW=bkW, C=C, HC=HC):
                            for hc in range(HC):
                                c = hc // 2
                                r = e.matmul(bkW[:, hc * C:(hc + 1) * C], lhsT=d["ktok"][0:C, c * 128:(c + 1) * 128],
                                             rhs=d["tg"][0:C, hc * C:(hc + 1) * C], start=True, stop=True)
                            return r
                        P.add("pe", mmw, reads=[d["ktok"].b, d["tg"].b], writes=[bkW.b])
                        P.add("act", copy_op("act", d["wt"][:, 0:W], bkW[:, 0:W]), reads=[bkW.b], writes=[d["wt"].b])
                        bkO = None
                        for c in range(n):
                            for h in range(2):
                                hc = c * 2 + h
                                head = 2 * g + h
                                sl = slice(hc * C, (hc + 1) * C)
                                vsl = slice(hc * 128, (hc + 1) * 128)
                                if sample:
                                    St = S[hc % 4]
                                    Sbt = Sb[hc % 4]
                                    P.add("sp", lambda e, St=St, sq_=c0 + c, head=head: e.dma_start(
                                        out=St[:], in_=sgdn_d[sq_, head]), writes=[St.b], dma=True)
                                    P.add("act", copy_op("act", Sbt[:], St[:]), reads=[St.b], writes=[Sbt.b])
                                else:
                                    St = S[h]
                                    Sbt = Sb[h]
                                    if bi == 0 and c0 == 0 and c == 0:
                                        if first:
                                            P.add("pool", lambda e, St=St: e.memset(St[:], 0.0), writes=[St.b])
                                        else:
                                            P.add("sp", lambda e, St=St, head=head: e.dma_start(out=St[:], in_=sscr_d[head]),
                                                  reads=[bscr[head]], writes=[St.b], dma=True)
                                            P.add("dve", lambda e, St=St: e.tensor_scalar(
                                                out=St[:], in0=St[:], scalar1=f1[:, 0:1], scalar2=None, op0=ALU.mult),
                                                reads=[St.b, f1.b], writes=[St.b])
                                    if bi == 0 and c0 == 0 and c == 0:
                                        P.add("act", copy_op("act", Sbt[:], St[:]), reads=[St.b], writes=[Sbt.b])
                                bkVn = bank()
                                vn = d["vn"][hc % 4]

                                def mm1(e, bkVn=bkVn, sl=sl, vsl=vsl, Sbt=Sbt, C=C):
                                    e.matmul(bkVn[0:C, 0:128], lhsT=d["tb"][0:C, sl], rhs=d["vtok"][0:C, vsl], start=True, stop=False)
                                    return e.matmul(bkVn[0:C, 0:128], lhsT=d["wt"][:, sl], rhs=Sbt[:], start=False, stop=True)
                                P.add("pe", mm1, reads=[d["tb"].b, d["vtok"].b, d["wt"].b, Sbt.b], writes=[bkVn.b])
                                P.add("act", copy_op("act", vn[0:C, :], bkVn[0:C, 0:128]), reads=[bkVn.b], writes=[vn.b])
                                if need_o:
                                    if hc % 4 == 0:
                                        bkO = obank()

                                    def mm2(e, bkO=bkO, sl=sl, Sbt=Sbt, vn=vn, x=hc % 4, C=C):
                                        e.matmul(bkO[0:C, x * 128:(x + 1) * 128], lhsT=d["qd"][:, sl], rhs=Sbt[:], start=True, stop=False)
                                        return e.matmul(bkO[0:C, x * 128:(x + 1) * 128], lhsT=d["at"][0:C, sl], rhs=vn[0:C, :],
                                                        start=False, stop=True)
                                    P.add("pe", mm2, reads=[d["qd"].b, d["at"].b, Sbt.b, vn.b], writes=[bkO.b], join=(hc % 4 != 0))
                                    if hc % 4 == 3:
                                        q4 = hc - 3
                                        P.add("dve", copy_op("dve", d["otok"][0:C, q4 * 128:(q4 + 4) * 128], bkO[0:C, 0:512]),
                                              reads=[bkO.b], writes=[d["otok"].b], join=(q4 > 0))
                                bkS = bank()
                                P.add("pe", lambda e, bkS=bkS, vsl=vsl, vn=vn, C=C: e.matmul(
                                    bkS[:, 0:128], lhsT=d["kd"][0:C, vsl], rhs=vn[0:C, :], start=True, stop=True),
                                    reads=[d["kd"].b, vn.b], writes=[bkS.b])
                                gcol = hc * C + C - 1
                                P.add("dve", lambda e, bkS=bkS, St=St, gcol=gcol: e.scalar_tensor_tensor(
                                    out=St[:], in0=St[:], scalar=d["egcb"][:, gcol:gcol + 1], in1=bkS[:, 0:128],
                                    op0=ALU.mult, op1=ALU.add), reads=[St.b, d["egcb"].b, bkS.b], writes=[St.b])
                                P.add("act", copy_op("act", Sbt[:], St[:]), reads=[St.b], writes=[Sbt.b])
                                if sample:
                                    P.add("pool", lambda e, St=St, sq_=c0 + c, head=head: e.dma_start(
                                        out=gdns_d[sq_, head], in_=St[:]), reads=[St.b], dma=True, out=True)
                                elif bi == nprompt - 1 and c0 + n == nch and c == n - 1:
                                    if last:
                                        P.add("pool", lambda e, St=St, head=head: e.dma_start(out=gdnp_d[head], in_=St[:]),
                                              reads=[St.b], dma=True, out=True)
                                    else:
                                        P.add("pool", lambda e, St=St, head=head: e.dma_start(out=sscr_d[head], in_=St[:]),
                                              reads=[St.b], writes=[bscr[head]], dma=True)
                        if need_o:
                            ot_ = d["otok"][0:C, :]
                            P.add("act", lambda e, ot_=ot_, C=C: e.activation(out=d["osq"][0:C, :], in_=ot_, func=AF.Square),
                                  reads=[d["otok"].b], writes=[d["osq"].b])
                            P.add("dve", lambda e, HC=HC, C=C: e.tensor_reduce(
                                out=d["ors"][0:C, 0:HC], in_=d["osq"][0:C, :].rearrange("p (a c) -> p a c", a=HC),
                                axis=AX.X, op=ALU.add), reads=[d["osq"].b], writes=[d["ors"].b])
                            P.add("act", lambda e, HC=HC, C=C: e.activation(
                                out=d["ors"][0:C, 0:HC], in_=d["ors"][0:C, 0:HC], func=AF.Sqrt, scale=1.0 / 128, bias=epst[0:C, 0:1]),
                                reads=[d["ors"].b, epst.b], writes=[d["ors"].b])
                            P.add("dve", lambda e, HC=HC, C=C: e.reciprocal(out=d["ors"][0:C, 0:HC], in_=d["ors"][0:C, 0:HC]),
                                  reads=[d["ors"].b], writes=[d["ors"].b])
                            P.add("dve", lambda e, HC=HC, C=C, ot_=ot_: e.tensor_tensor(
                                out=ot_.rearrange("p (a c) -> p a c", a=HC), in0=ot_.rearrange("p (a c) -> p a c", a=HC),
                                in1=d["ors"][0:C, 0:HC].unsqueeze(2).to_broadcast([C, HC, 128]), op=ALU.mult),
                                reads=[d["otok"].b, d["ors"].b], writes=[d["otok"].b])
                            bkOT = bank()

                            def tro(e, bkOT=bkOT, C=C, HC=HC):
                                for hc in range(HC):
                                    r = e.transpose(bkOT[:, hc * C:(hc + 1) * C], d["otok"][0:C, hc * 128:(hc + 1) * 128],
                                                    ident[0:C, 0:C])
                                return r
                            P.add("pe", tro, reads=[d["otok"].b, ident.b], writes=[bkOT.b])
                            for h in range(2):
                                P.add("dve", lambda e, h=h, bkOT=bkOT, C=C, st0=st0: e.scalar_tensor_tensor(
                                    out=og[:, h, st0:st0 + n * C].rearrange("p (a c) -> p a c", a=n),
                                    in0=bkOT[:, 0:n * 2 * C].rearrange("p (a h c) -> p a h c", a=n, h=2)[:, :, h, :],
                                    scalar=vecT[:, 576:577],
                                    in1=QKVZ[:, 4 + h, st0:st0 + n * C].rearrange("p (a c) -> p a c", a=n),
                                    op0=ALU.mult, op1=ALU.mult), reads=[bkOT.b, vecT.b, QKVZ.b], writes=[og.b], join=True)
                        if dbg.get('stop') == 'sub' and g == dbg.get('g', 0) and bi == dbg.get('bi', 0) and c0 == dbg.get('c0', 0) and first == dbg.get('first', True):
                            for nm_ in ('beta', 'g', 'esuf', 'nbeg'):
                                dump(pre[nm_], 'pre_' + nm_, [64, totch, 32])
                            dump(QKVZ, 'QKVZ', [128, 6, 512])
                            for nm_ in ('rt', 'tb', 'tg', 'at', 'p0', 'pt0'):
                                dump(d[nm_], nm_, [64, 512])
                            for nm_ in ('egcb', 'qd', 'wt'):
                                dump(d[nm_], nm_, [128, 512])
                            for nm_ in ('vtok', 'kd', 'otok'):
                                dump(d[nm_], nm_, [64, 1024])
                            dump(d['ktok'], 'ktok', [64, 512])
                            dump(og, 'og', [128, 2, 512], BF16)
                            dump(S[0], 'S0', [128, 128])
                            dump(S[1], 'S1', [128, 128])
                            dbg['_halt'] = True
                            return
                    if blk["o_from"] is not None:
                        ot0 = blk["otok0"]
                        lo = blk.get("olo", 0)
                        for h in range(2):
                            head = 2 * g + h
                            P.add("pool", lambda e, h=h, head=head, ot0=ot0, NB=NB, lo=lo: e.dma_start(
                                out=oscr_d[head, :, ot0:ot0 + NB - lo], in_=og[:, h, lo:NB]),
                                reads=[og.b], writes=[boscr[head]], dma=True, join=True)

        with scope() as sc:
            XT = sb([128, KC, NT], stack=sc, name="XTp")
            load_xT(XT, xp_d, NPR, sc)
            norm_mod(XT, NPR, 0, 0, 0, sc)
            dump(XT, 'XT_P', [128, KC, NT])
        P.fence()
        dump(HT, 'HT_P', [128, KC, NT], BF16)
        stop_at('normP')
        blocksP = [dict(C=64, nch=8, tok0=0, nseq=1, T=512, sample=False, otok0=0, qz=bool(dbg.get('p_full')), o_from=(0 if dbg.get('p_full') else None)),
                   dict(C=64, nch=8, tok0=512, nseq=1, T=512, sample=False, otok0=NPR + NSM, qz=True, o_from=4, olo=508)]
        with scope() as sc:
            gdn_pass(sc, blocksP, first=True, last=False)
        P.fence()
        if dbg.get('_halt'):
            raise _Stop()
        with scope() as sc:
            XT = sb([128, KC, NT], stack=sc, name="XTo")
            load_xT(XT, xo_d, NPR + NSM, sc)
            norm_mod(XT, NPR, NSM, 0, 0, sc)
        P.fence()
        blocksO = [dict(C=64, nch=8, tok0=0, nseq=1, T=512, sample=False, otok0=0, qz=True, o_from=0),
                   dict(C=64, nch=8, tok0=512, nseq=1, T=512, sample=False, otok0=512, qz=True, o_from=0),
                   dict(C=4, nch=16, tok0=1024, nseq=16, T=4, sample=True, otok0=1024, qz=True, o_from=0)]
        with scope() as sc:
            gdn_pass(sc, blocksO, first=False, last=True)
        P.fence()
        if 'oscr' in dbg.get('dumps', ()):
            dd_ = nc.dram_tensor('dbg_oscr', [32, 128, NT], BF16, kind='ExternalOutput').ap()
            P.add('sp', lambda e: e.dma_start(out=dd_, in_=oscr_d), reads=boscr, dma=True, out=True)
        stop_at('gdnO')
        if dbg.get('_halt'):
            raise _Stop()

        XT = sb([128, KC, NT], stack=fin, name="XT")
        with scope() as sc:
            load_xT(XT, xo_d, NT, sc)
        tiles = tok_tiles(NT)
        NSA = (NT - NPR) // 4
        ACT_FS = 8
        AT_ = sb([128, ACT_FS, NT], BF16, stack=fin, name="AT")
        gtmp = sb([128, 128], stack=fin, name="gtmp")

        tiles_mm = [(0, 364), (364, 364), (728, 364)]

        def resid_epilogue(l, s):
            gab = (2 + 3 * s) * 16

            def ep(cc, t0, tn, bk):
                pe_ = min(t0 + tn, NPR)
                if t0 < pe_:
                    P.add("dve", lambda e: e.scalar_tensor_tensor(
                        out=XT[:, cc, t0:pe_], in0=bk[:, 0:pe_ - t0], scalar=mod[:, l, gab + cc, 0:1],
                        in1=XT[:, cc, t0:pe_], op0=ALU.mult, op1=ALU.add),
                        reads=[bk.b, mod.b, XT.b], writes=[XT.b])
                if t0 + tn > NPR:
                    s0 = max(t0, NPR)
                    sn = t0 + tn - s0
                    ns = sn // 4
                    g0 = (s0 - NPR) // 4
                    P.add("dve", lambda e: e.tensor_tensor(
                        out=gtmp[:, 0:sn].rearrange("p (s t) -> p s t", t=4),
                        in0=bk[:, s0 - t0:s0 - t0 + sn].rearrange("p (s t) -> p s t", t=4),
                        in1=mod[:, l, gab + cc, 1 + g0:1 + g0 + ns].unsqueeze(2).to_broadcast([128, ns, 4]), op=ALU.mult),
                        reads=[bk.b, mod.b], writes=[gtmp.b])
                    P.add("dve", lambda e: e.tensor_tensor(
                        out=XT[:, cc, s0:s0 + sn], in0=XT[:, cc, s0:s0 + sn], in1=gtmp[:, 0:sn], op=ALU.add),
                        reads=[gtmp.b, XT.b], writes=[XT.b])
            return ep

        at_rhs = lambda k, t0, tn: AT_[:, k, t0:t0 + tn]
        ht_rhs = lambda k, t0, tn: HT[:, k, t0:t0 + tn]

        ep = resid_epilogue(0, 0)
        for hg in range(4):
            for hh in range(8):
                head = hg * 8 + hh
                P.add("sp", lambda e, hh=hh, head=head: e.dma_start(out=AT_[:, hh, :], in_=oscr_d[head]),
                      reads=[boscr[head]], writes=[AT_.b], dma=True, join=(hh > 0))
            linear(w_gout_d[hg * 1024:(hg + 1) * 1024, :], 0, 16, 8, at_rhs, tiles_mm, ep, [AT_.b])

        dump(XT, 'XT_gout', [128, KC, NT])

        def glu_mlp(w_up_d, w_dn_d, hid, l, gate_bc=None):
            nhc = hid // 128
            ep = resid_epilogue(l, 1)
            for f0 in range(0, nhc, ACT_FS):
                fs = min(ACT_FS, nhc - f0)

                def ep_gate(cc, t0, tn, bk):
                    P.add("act", lambda e: e.activation(out=AT_[:, cc, t0:t0 + tn], in_=bk[:, 0:tn], func=AF.Silu),
                          reads=[bk.b], writes=[AT_.b], join=True)

                def ep_up(cc, t0, tn, bk):
                    P.add("dve", lambda e: e.tensor_tensor(out=AT_[:, cc, t0:t0 + tn], in0=AT_[:, cc, t0:t0 + tn],
                                                          in1=bk[:, 0:tn], op=ALU.mult),
                          reads=[bk.b, AT_.b], writes=[AT_.b])
                    if gate_bc is not None:
                        P.add("pool", lambda e: e.tensor_tensor(out=AT_[:, cc, t0:t0 + tn], in0=AT_[:, cc, t0:t0 + tn],
                                                               in1=gate_bc[:, t0:t0 + tn], op=ALU.mult),
                              reads=[gate_bc.b, AT_.b], writes=[AT_.b])
                linear(w_up_d, f0 * 128, fs, KC, ht_rhs, tiles_mm, ep_gate, [HT.b])
                linear(w_up_d, hid + f0 * 128, fs, KC, ht_rhs, tiles_mm, ep_up, [HT.b])
                linear(w_dn_d[f0 * 128:(f0 + fs) * 128, :], 0, 16, fs, at_rhs, tiles_mm, ep, [AT_.b])

        with scope() as sc:
            norm_mod(XT, NPR, NT - NPR, 0, 1, sc)
        glu_mlp(w_fup_d, w_fdn_d, FFN, 0)
        dump(XT, 'XT_ffn', [128, KC, NT])

        with scope() as sc:
            norm_mod(XT, NPR, NT - NPR, 1, 0, sc)
        with scope() as sc:
            F = sb([128, NPR + 2 + NSA * 6], stack=sc, name="Fs")
            Fp = F[:, 0:NPR + 2]
            Fsm = F[:, NPR + 2:NPR + 2 + NSA * 6].rearrange("p (s t) -> p s t", t=6)
            cgt = [sb([128, 512], stack=sc, name="cgt") for _ in range(2)]
            yv = sb([128, NT], stack=sc, name="yv")
            sst = sb([32, 512], stack=sc, name="sst")
            ssT = sb([128, KC, 32], stack=sc, name="ssT")
            sso = sb([128, KC, 34], stack=sc, name="sso")
            sot = sb([34, 512], stack=sc, name="sot")
            for g4 in range(4):
                P.add("sp", lambda e, g4=g4: e.dma_start(out=sst[:], in_=ssconv_d[:, g4 * 512:(g4 + 1) * 512]),
                      writes=[sst.b], dma=True)
                bk = bank()

                def tr(e, g4=g4, bk=bk):
                    for j in range(4):
                        r = e.transpose(bk[:, j * 32:(j + 1) * 32], sst[:, j * 128:(j + 1) * 128], ident[0:32, 0:32])
                    return r
                P.add("pe", tr, reads=[sst.b, ident.b], writes=[bk.b])
                P.add("dve", copy_op("dve", ssT[:, g4 * 4:(g4 + 1) * 4, :], bk[:, 0:128].rearrange("p (a b) -> p a b", a=4)),
                      reads=[bk.b], writes=[ssT.b], join=(g4 > 0))
            P.add("pool", lambda e: e.memset(F[:], 0.0), writes=[F.b])
            ep_res = resid_epilogue(1, 0)
            for half in range(2):
                for c8 in range(8):
                    cc = half * 8 + c8
                    wts = [load_w(w_scin_d[:, o * 2048 + cc * 128:o * 2048 + (cc + 1) * 128], KC, 128) for o in (1, 2)]
                    for (t0, tn) in tiles:
                        cg = cgt[(t0 // 512) % 2]
                        for o in range(2):
                            wt_, wvv = wts[o]
                            bk = bank()

                            def mm(e, wvv=wvv, t0=t0, tn=tn, bk=bk):
                                for k in range(KC):
                                    r = e.matmul(bk[:, 0:tn], lhsT=wvv[:, k, :], rhs=HT[:, k, t0:t0 + tn],
                                                 start=(k == 0), stop=(k == KC - 1))
                                return r
                            P.add("pe", mm, reads=[wt_.b, HT.b], writes=[bk.b])
                            if o == 0:
                                P.add("act", copy_op("act", cg[:, 0:tn], bk[:, 0:tn]), reads=[bk.b], writes=[cg.b])
                            elif t0 < NPR:
                                P.add("dve", lambda e, cg=cg, bk=bk, t0=t0, tn=tn: e.tensor_tensor(
                                    out=Fp[:, 2 + t0:2 + t0 + tn], in0=cg[:, 0:tn], in1=bk[:, 0:tn], op=ALU.mult),
                                    reads=[cg.b, bk.b], writes=[F.b])
                            else:
                                P.add("dve", lambda e, cg=cg, bk=bk, tn=tn: e.tensor_tensor(
                                    out=Fsm[:, :, 2:6], in0=cg[:, 0:tn].rearrange("p (s t) -> p s t", t=4),
                                    in1=bk[:, 0:tn].rearrange("p (s t) -> p s t", t=4), op=ALU.mult),
                                    reads=[cg.b, bk.b], writes=[F.b])
                    P.add("pool", lambda e, cc=cc: e.tensor_copy(
                        out=Fsm[:, 0:NSQ, 0:2], in_=ssT[:, cc, :].rearrange("p (s r) -> p s r", r=2)),
                        reads=[ssT.b], writes=[F.b])
                    P.add("pool", lambda e: e.tensor_scalar(out=Fp[:, 0:2], in0=Fsm[:, NSQ, 4:6], scalar1=f1[:, 0:1],
                                                           scalar2=None, op0=ALU.mult), reads=[F.b, f1.b], writes=[F.b])
                    wcol = 528 + cc
                    en = "dve"
                    yvs = yv[:, NPR:NT].rearrange("p (s t) -> p s t", t=4)
                    P.add(en, lambda e, wcol=wcol: e.tensor_scalar(
                        out=yv[:, 0:NPR], in0=Fp[:, 0:NPR], scalar1=vecT[:, wcol:wcol + 1], scalar2=None, op0=ALU.mult),
                        reads=[F.b, vecT.b], writes=[yv.b])
                    P.add(en, lambda e, wcol=wcol, yvs=yvs: e.tensor_scalar(
                        out=yvs, in0=Fsm[:, :, 0:4], scalar1=vecT[:, wcol:wcol + 1], scalar2=None, op0=ALU.mult),
                        reads=[F.b, vecT.b], writes=[yv.b], join=True)
                    for tp in (1, 2):
                        wc2 = wcol + 16 * tp
                        P.add(en, lambda e, wc2=wc2, tp=tp: e.scalar_tensor_tensor(
                            out=yv[:, 0:NPR], in0=Fp[:, tp:tp + NPR], scalar=vecT[:, wc2:wc2 + 1], in1=yv[:, 0:NPR],
                            op0=ALU.mult, op1=ALU.add), reads=[F.b, vecT.b, yv.b], writes=[yv.b])
                        P.add(en, lambda e, wc2=wc2, tp=tp, yvs=yvs: e.scalar_tensor_tensor(
                            out=yvs, in0=Fsm[:, :, tp:tp + 4], scalar=vecT[:, wc2:wc2 + 1], in1=yvs,
                            op0=ALU.mult, op1=ALU.add), reads=[F.b, vecT.b, yv.b], writes=[yv.b])
                    P.add("pool", lambda e, cc=cc: e.tensor_copy(out=sso[:, cc, 0:2], in_=Fp[:, NPR:NPR + 2]),
                          reads=[F.b], writes=[sso.b], join=True)
                    P.add("pool", lambda e, cc=cc: e.tensor_copy(
                        out=sso[:, cc, 2:34].rearrange("p (s r) -> p s r", r=2), in_=Fsm[:, 0:NSQ, 4:6]),
                        reads=[F.b], writes=[sso.b], join=True)
                    wb_, wbv = load_w(w_scin_d[:, cc * 128:(cc + 1) * 128], KC, 128)
                    for (t0, tn) in tiles:
                        bk = bank()

                        def mm(e, wbv=wbv, t0=t0, tn=tn, bk=bk):
                            for k in range(KC):
                                r = e.matmul(bk[:, 0:tn], lhsT=wbv[:, k, :], rhs=HT[:, k, t0:t0 + tn],
                                             start=(k == 0), stop=(k == KC - 1))
                            return r
                        P.add("pe", mm, reads=[wb_.b, HT.b], writes=[bk.b])
                        P.add("dve", lambda e, c8=c8, bk=bk, t0=t0, tn=tn: e.tensor_tensor(
                            out=AT_[:, c8, t0:t0 + tn], in0=bk[:, 0:tn], in1=yv[:, t0:t0 + tn], op=ALU.mult),
                            reads=[bk.b, yv.b], writes=[AT_.b], join=True)
                linear(w_scout_d[half * 1024:(half + 1) * 1024, :], 0, 16, 8, at_rhs, tiles_mm, ep_res, [AT_.b])
            for g4 in range(4):
                bk = bank()

                def tr(e, g4=g4, bk=bk):
                    for j in range(4):
                        k = g4 * 4 + j
                        r = e.transpose(bk[0:34, j * 128:(j + 1) * 128], sso[:, k, :], ident[:])
                    return r
                P.add("pe", tr, reads=[sso.b, ident.b], writes=[bk.b])
                P.add("dve", copy_op("dve", sot[:, :], bk[0:34, 0:512]), reads=[bk.b], writes=[sot.b])
                P.add("pool", lambda e, g4=g4: e.dma_start(out=sconvp_d[:, g4 * 512:(g4 + 1) * 512], in_=sot[0:2, :]),
                      reads=[sot.b], dma=True, out=True)
                P.add("pool", lambda e, g4=g4: e.dma_start(out=sconvs_d[:, g4 * 512:(g4 + 1) * 512], in_=sot[2:34, :]),
                      reads=[sot.b], dma=True, out=True)

        dump(XT, 'XT_sc', [128, KC, NT])
        gT = sb([8, NT], stack=fin, name="gT")
        gbc = sb([128, NT], stack=fin, name="gbc")
        with scope() as sc:
            wrs = sb([128, KC, NEXP], stack=sc, name="wrs")
            P.add("sp", lambda e: e.dma_start(out=wrs[:], in_=w_rt_d.rearrange("(k p) c -> p k c", p=128)),
                  writes=[wrs.b], dma=True)
            norm_mod(XT, NPR, NT - NPR, 1, 1, sc, router=wrs)
        with scope() as sc:
            lg = sb([128, 9, 8], stack=sc, name="lg")
            m1 = sb([128, 9], stack=sc, name="m1")
            m2 = sb([128, 9], stack=sc, name="m2")
            k1 = sb([128, 9, 8], stack=sc, name="k1")
            k2 = sb([128, 9, 8], stack=sc, name="k2")
            l2 = sb([128, 9, 8], stack=sc, name="l2")
            LT = NT - 1024
            P.add("pool", lambda e: e.memset(lg[:], 0.0), writes=[lg.b])
            P.add("dve", lambda e: e.tensor_tensor(
                out=lg[:, 0:8, :], in0=rbank[:, 0:64].rearrange("p (a b) -> p a b", a=8),
                in1=hvec[:, 64:72].unsqueeze(1).to_broadcast([128, 8, 8]), op=ALU.add),
                reads=[rbank.b, hvec.b, lg.b], writes=[lg.b])
            P.add("dve", lambda e: e.tensor_tensor(out=lg[0:LT, 8, :], in0=rbank[0:LT, 64:72], in1=hvec[0:LT, 64:72], op=ALU.add),
                  reads=[rbank.b, hvec.b, lg.b], writes=[lg.b])
            b98 = lambda ap: ap.unsqueeze(2).to_broadcast([128, 9, 8])
            P.add("dve", lambda e: e.tensor_reduce(out=m1[:], in_=lg[:], axis=AX.X, op=ALU.max), reads=[lg.b], writes=[m1.b])
            P.add("dve", lambda e: e.tensor_tensor(out=k1[:], in0=lg[:], in1=b98(m1[:]), op=ALU.is_equal),
                  reads=[lg.b, m1.b], writes=[k1.b])
            P.add("dve", lambda e: e.scalar_tensor_tensor(out=l2[:], in0=k1[:], scalar=-1e30, in1=lg[:], op0=ALU.mult, op1=ALU.add),
                  reads=[k1.b, lg.b], writes=[l2.b])
            P.add("dve", lambda e: e.tensor_reduce(out=m2[:], in_=l2[:], axis=AX.X, op=ALU.max), reads=[l2.b], writes=[m2.b])
            P.add("dve", lambda e: e.tensor_tensor(out=k2[:], in0=l2[:], in1=b98(m2[:]), op=ALU.is_equal),
                  reads=[l2.b, m2.b], writes=[k2.b])
            P.add("dve", lambda e: e.tensor_tensor(out=m1[:], in0=m1[:], in1=m2[:], op=ALU.subtract),
                  reads=[m1.b, m2.b], writes=[m1.b])
            P.add("act", lambda e: e.activation(out=m1[:], in_=m1[:], func=AF.Exp), reads=[m1.b], writes=[m1.b])
            P.add("dve", lambda e: e.tensor_scalar(out=m1[:], in0=m1[:], scalar1=1.0, scalar2=None, op0=ALU.add),
                  reads=[m1.b], writes=[m1.b])
            P.add("dve", lambda e: e.reciprocal(out=m2[:], in_=m1[:]), reads=[m1.b], writes=[m2.b])
            P.add("dve", lambda e: e.tensor_scalar(out=m1[:], in0=m2[:], scalar1=-1.0, scalar2=1.0, op0=ALU.mult, op1=ALU.add),
                  reads=[m2.b], writes=[m1.b])
            P.add("dve", lambda e: e.tensor_tensor(out=k1[:], in0=k1[:], in1=b98(m1[:]), op=ALU.mult),
                  reads=[k1.b, m1.b], writes=[k1.b])
            P.add("dve", lambda e: e.tensor_tensor(out=k2[:], in0=k2[:], in1=b98(m2[:]), op=ALU.mult),
                  reads=[k2.b, m2.b], writes=[k2.b])
            P.add("dve", lambda e: e.tensor_tensor(out=k1[:], in0=k1[:], in1=k2[:], op=ALU.add),
                  reads=[k1.b, k2.b], writes=[k1.b])
            for t3 in range(3):
                bk = bank()
                nt3 = 4 if t3 < 2 else 1

                def trg(e, bk=bk, t3=t3, nt3=nt3):
                    for x in range(nt3):
                        t = t3 * 4 + x
                        tn = min(128, NT - t * 128)
                        r = e.transpose(bk[0:8, x * 128:x * 128 + tn], k1[0:tn, t, :], ident[0:tn, 0:tn])
                    return r
                P.add("pe", trg, reads=[k1.b, ident.b], writes=[bk.b])
                wd = 512 if t3 < 2 else LT
                P.add("dve", copy_op("dve", gT[:, t3 * 512:t3 * 512 + wd], bk[0:8, 0:wd]), reads=[bk.b], writes=[gT.b],
                      join=(t3 > 0))
        for ex in range(NEXP):
            for (t0, tn) in tiles:
                bk = bank()
                P.add("pe", lambda e, ex=ex, t0=t0, tn=tn, bk=bk: e.matmul(
                    bk[:, 0:tn], lhsT=sel[:, ex, :], rhs=gT[:, t0:t0 + tn], start=True, stop=True),
                    reads=[sel.b, gT.b], writes=[bk.b])
                P.add("act", copy_op("act", gbc[:, t0:t0 + tn], bk[:, 0:tn]), reads=[bk.b], writes=[gbc.b], join=(t0 > 0))
            glu_mlp(w_mup_d[ex], w_mdn_d[ex], EXD, 1, gate_bc=gbc)

        dump(XT, 'XT_moe', [128, KC, NT])
        dump(gT, 'gT', [8, NT])
        with scope() as sc:
            rs = sb([128, NT], stack=sc, name="rsf")
            rstd_bc(XT, NT, sc, rs)
            for k in range(KC):
                P.add("dve", lambda e, k=k: e.scalar_tensor_tensor(
                    out=XT[:, k, :], in0=XT[:, k, :], scalar=vecT[:, 256 + k:257 + k], in1=rs[:, :], op0=ALU.mult, op1=ALU.mult),
                    reads=[XT.b, vecT.b, rs.b], writes=[XT.b])
            y_ = sb([128, D], stack=sc, name="ys")
            for t in range(9):
                r0 = t * 128
                rows = min(128, NT - r0)
                for g4 in range(4):
                    bk = bank()

                    def tr(e, g4=g4, bk=bk, r0=r0, rows=rows):
                        for j in range(4):
                            k = g4 * 4 + j
                            r = e.transpose(bk[0:rows, j * 128:(j + 1) * 128], XT[:, k, r0:r0 + rows], ident[:])
                        return r
                    P.add("pe", tr, reads=[XT.b, ident.b], writes=[bk.b])
                    en = ev_eng()
                    P.add(en, copy_op(en, y_[0:rows, g4 * 512:(g4 + 1) * 512], bk[0:rows, 0:512]), reads=[bk.b], writes=[y_.b],
                          join=(g4 > 0))
                P.add("pool", lambda e, r0=r0, rows=rows: e.dma_start(out=yo_d[r0:r0 + rows, :], in_=y_[0:rows, :]),
                      reads=[y_.b], dma=True, out=True)

    except _Stop:
        pass
    sems = contextlib.ExitStack()
    P.emit(sems)
    sems.close()
    fin.close()
    top.close()
    return nc, P


_CACHE = {}


def _consts():
    ident = np.eye(128, dtype=np.float32)
    masks = np.zeros((64, 4, 64), np.float32)
    t = np.arange(64)[:, None]
    i = np.arange(64)[None, :]
    masks[:, 0] = (t <= i)
    masks[:, 1] = (t > i)
    masks[:, 2] = (t >= i)
    masks[:, 3] = (t > i)
    sel = np.zeros((8, 8, 128), np.float32)
    for e in range(8):
        sel[e, e, :] = 1.0
    return ident, masks, sel


def kernel(x_prompt, x_sample, c_prompt, c_sample, state_gdn, state_gdn_conv, state_sconv,
           w_ada, b_ada, g_norm_mix, g_norm_ffn, g_norm_out, gdn_w_in, gdn_conv_w,
           gdn_a_log, gdn_dt_bias, gdn_g_onorm, gdn_w_out, sc_w_in, sc_conv_w, sc_w_out,
           ffn_w_up, ffn_w_down, moe_w_router, moe_b_router, moe_w_up, moe_w_down):
    f = lambda a: np.ascontiguousarray(np.asarray(a, dtype=np.float32))
    x_prompt, x_sample, c_prompt, c_sample = f(x_prompt), f(x_sample), f(c_prompt), f(c_sample)
    state_gdn, state_gdn_conv, state_sconv = f(state_gdn), f(state_gdn_conv), f(state_sconv)
    ident, masks, sel = _consts()
    vecs = np.zeros((640, 128), np.float32)
    vecs[0:192] = f(b_ada).reshape(192, 128)
    vecs[192:224] = f(g_norm_mix).reshape(32, 128)
    vecs[224:256] = f(g_norm_ffn).reshape(32, 128)
    vecs[256:272] = f(g_norm_out).reshape(16, 128)
    vecs[272:528] = f(gdn_conv_w).reshape(256, 128)
    vecs[528:576] = f(sc_conv_w).reshape(48, 128)
    vecs[576] = f(gdn_g_onorm).reshape(128)
    hvec = np.concatenate([f(gdn_a_log).reshape(-1), f(gdn_dt_bias).reshape(-1), f(moe_b_router).reshape(-1)])[None, :]
    shared = {
        "vecs": vecs, "hvec": np.ascontiguousarray(hvec), "ident": ident, "masks": masks, "sel": sel,
        "w_ada": f(w_ada), "gdn_w_in": f(gdn_w_in)[0], "gdn_w_out": f(gdn_w_out)[0], "sc_w_in": f(sc_w_in)[0],
        "sc_w_out": f(sc_w_out)[0], "ffn_w_up": f(ffn_w_up)[0], "ffn_w_down": f(ffn_w_down)[0],
        "moe_w_router": f(moe_w_router)[0], "moe_w_up": f(moe_w_up)[0], "moe_w_down": f(moe_w_down)[0],
    }
    in_maps = []
    for c in range(8):
        s, m = c // 2, c % 2
        xs_ = x_sample[16 * c:16 * c + 16].reshape(64, D)
        xo = np.concatenate([x_prompt[s, m * 1024:(m + 1) * 1024], xs_, x_prompt[s, 1020:1024]], axis=0)
        xp = x_prompt[s, 0:1024]
        cvec = np.concatenate([c_prompt[s:s + 1], c_sample[16 * c:16 * c + 16], c_prompt[s:s + 1]], axis=0)
        d = dict(shared)
        d.update({
            "xo": np.ascontiguousarray(xo), "xp": np.ascontiguousarray(xp), "cvec": np.ascontiguousarray(cvec),
            "sgdn": np.ascontiguousarray(state_gdn[0, 16 * c:16 * c + 16]),
            "sgconv": np.ascontiguousarray(state_gdn_conv[0, 16 * c:16 * c + 16].reshape(48, 8192)),
            "ssconv": np.ascontiguousarray(state_sconv[0, 16 * c:16 * c + 16].reshape(32, D)),
            "f1": np.full((128, 1), float(m), np.float32),
        })
        in_maps.append(d)
    if _CACHE.get("dbg_hook") is not None:
        return _CACHE["dbg_hook"](in_maps)
    if "nc" not in _CACHE:
        _CACHE["nc"] = build_program()[0]
    nc = _CACHE["nc"]
    res = run_bass_kernel_spmd(nc, in_maps, core_ids=list(range(8)))
    R = res.results
    y_prompt = np.zeros((4, 2048, D), np.float32)
    y_sample = np.zeros((128, 4, D), np.float32)
    gdn_p = np.zeros((1, 4, 32, 128, 128), np.float32)
    gconv_p = np.zeros((1, 4, 3, 8192), np.float32)
    sconv_p = np.zeros((1, 4, 2, D), np.float32)
    gdn_s = np.zeros((1, 128, 32, 128, 128), np.float32)
    gconv_s = np.zeros((1, 128, 3, 8192), np.float32)
    sconv_s = np.zeros((1, 128, 2, D), np.float32)
    for c in range(8):
        s, m = c // 2, c % 2
        r = R[c]
        y_prompt[s, m * 1024:(m + 1) * 1024] = r["yo"][0:1024]
        y_sample[16 * c:16 * c + 16] = r["yo"][1024:1088].reshape(16, 4, D)
        if m == 1:
            gdn_p[0, s] = r["gdn_p"]
            gconv_p[0, s] = r["gconv_p"]
            sconv_p[0, s] = r["sconv_p"]
        gdn_s[0, 16 * c:16 * c + 16] = r["gdn_s"]
        gconv_s[0, 16 * c:16 * c + 16] = r["gconv_s"].reshape(16, 3, 8192)
        sconv_s[0, 16 * c:16 * c + 16] = r["sconv_s"].reshape(16, 2, D)
    return (y_prompt, y_sample, gdn_p, gconv_p, sconv_p, gdn_s, gconv_s, sconv_s)
```
